# Optimizing a Trainium2 kernel written in Bass

```python
import math
import jax, jax.numpy as jnp
from jax import lax
import numpy as np

D_MODEL = 2048
BATCH = 2
SEQ = 16384
DEPTH = 1

CHUNK = 64
Q_BLOCK = 128
EPS = 1e-6
D_SSM = D_MODEL // 2
SSM_GROUP = 16
N_SSM_GROUPS = D_SSM // SSM_GROUP
SSM_STATE = 64
DT_MIN = 0.001
DT_MAX = 0.1
N_MLA_HEADS = 8
QK_NOPE = 128
QK_ROPE = 64
V_HEAD = 128
D_MLA = N_MLA_HEADS * V_HEAD
Q_LORA = 512
KV_LORA = 256
ROPE_BASE = 10000.0
D_IN = 2 * D_SSM + Q_LORA + KV_LORA + QK_ROPE
D_MIX = D_SSM + D_MLA
N_EXPERTS = 32
TOP_K = 4
D_FF = D_MODEL
SWIGLU_LIMIT = 7.0
SWIGLU_ALPHA = 1.702
MOE_BLOCK = 256
N_MOD = 6

kernel_name = 'hybrid_s5_mla_moe_adaln_block'


def rms_norm(x, g):
    xf = x.astype(jnp.float32)
    y = xf * lax.rsqrt(jnp.mean(xf * xf, axis=-1, keepdims=True) + EPS)
    return (y * g.astype(jnp.float32)).astype(x.dtype)


def rope(x, cos, sin):
    half = x.shape[-1] // 2
    x1, x2 = x[..., :half], x[..., half:]
    c, s = cos.astype(x.dtype), sin.astype(x.dtype)
    return jnp.concatenate([x1 * c - x2 * s, x2 * c + x1 * s], axis=-1)


def s5_mixer(u, A_re, A_im, B_re, B_im, C_re, C_im, D, log_dt):
    Bsz, L, _ = u.shape
    f32 = jnp.float32
    G, P, H = N_SSM_GROUPS, SSM_STATE, SSM_GROUP
    A = lax.complex(A_re.astype(f32), A_im.astype(f32))
    dtA = A * jnp.exp(log_dt.astype(f32))[:, None]
    A_bar = jnp.exp(dtA)
    Bc = lax.complex(B_re.astype(f32), B_im.astype(f32))
    B_bar = ((A_bar - 1.0) / A)[..., None] * Bc
    Cc = lax.complex(C_re.astype(f32), C_im.astype(f32))
    powers = jnp.exp(jnp.arange(1, CHUNK + 1, dtype=f32)[:, None, None] * dtA[None])
    a_chunk = jnp.broadcast_to(A_bar, (Bsz, CHUNK, G, P))

    def combine(left, right):
        a_l, b_l = left
        a_r, b_r = right
        return a_r * a_l, a_r * b_l + b_r

    def step(state, u_c):
        bu = jnp.einsum('gph,blgh->blgp', B_bar, u_c.astype(jnp.complex64))
        _, local = lax.associative_scan(combine, (a_chunk, bu), axis=1)
        states = local + powers[None] * state[:, None]
        y = jnp.einsum('ghp,blgp->blgh', Cc, states).real
        return states[:, -1], y

    u_chunks = jnp.moveaxis(u.astype(f32).reshape(Bsz, L // CHUNK, CHUNK, G, H), 1, 0)
    state0 = jnp.zeros((Bsz, G, P), jnp.complex64)
    _, ys = lax.scan(step, state0, u_chunks)
    y = jnp.moveaxis(ys, 0, 1).reshape(Bsz, L, D_SSM)
    return y + D.astype(f32) * u.astype(f32)


def mla_mixer(q_lat, kv_lat, k_rope, cos, sin, q_lat_g, kv_lat_g, w_uq, w_ukv,
              q_nope_g, q_rope_g, k_nope_g, k_rope_g):
    Bsz, L, _ = q_lat.shape
    H = N_MLA_HEADS
    q = (rms_norm(q_lat, q_lat_g) @ w_uq).reshape(Bsz, L, H, QK_NOPE + QK_ROPE)
    kv = (rms_norm(kv_lat, kv_lat_g) @ w_ukv).reshape(Bsz, L, H, QK_NOPE + V_HEAD)
    q_nope = rms_norm(q[..., :QK_NOPE], q_nope_g)
    q_rp = rope(rms_norm(q[..., QK_NOPE:], q_rope_g), cos[:, :, None], sin[:, :, None])
    k_nope = rms_norm(kv[..., :QK_NOPE], k_nope_g)
    v = kv[..., QK_NOPE:]
    k_rp = rope(rms_norm(k_rope, k_rope_g), cos, sin)
    scale = (QK_NOPE + QK_ROPE) ** -0.5
    n_blk = L // Q_BLOCK
    qn_b = jnp.swapaxes(q_nope.reshape(Bsz, n_blk, Q_BLOCK, H, QK_NOPE), 0, 1)
    qr_b = jnp.swapaxes(q_rp.reshape(Bsz, n_blk, Q_BLOCK, H, QK_ROPE), 0, 1)
    key_chunk = jnp.arange(L) // CHUNK

    def attend(args):
        blk, qn, qr = args
        s = (jnp.einsum('bqhd,bkhd->bhqk', qn, k_nope, preferred_element_type=jnp.float32)
             + jnp.einsum('bqhr,bkr->bhqk', qr, k_rp, preferred_element_type=jnp.float32)) * scale
        q_chunk = (blk * Q_BLOCK + jnp.arange(Q_BLOCK)) // CHUNK
        mask = key_chunk[None, :] <= q_chunk[:, None]
        s = jnp.where(mask[None, None], s, -jnp.inf)
        p = jax.nn.softmax(s, axis=-1)
        return jnp.einsum('bhqk,bkhd->bqhd', p.astype(v.dtype), v)

    out = lax.map(attend, (jnp.arange(n_blk), qn_b, qr_b))
    return jnp.swapaxes(out, 0, 1).reshape(Bsz, L, D_MLA)


def moe_ffn(h, w_router, b_router, w_gate_up, b_gate_up, w_down, b_down):
    Bsz, L, D = h.shape
    T = Bsz * L
    E, K, M = N_EXPERTS, TOP_K, MOE_BLOCK
    xt = h.reshape(T, D)
    logits = (xt @ w_router).astype(jnp.float32) + b_router.astype(jnp.float32)
    top_val, top_idx = lax.top_k(logits, K)
    gates = jax.nn.softmax(top_val, axis=-1)
    flat_e = top_idx.reshape(-1)
    flat_tok = jnp.arange(T * K, dtype=jnp.int32) // K
    flat_gate = gates.reshape(-1)
    order = jnp.argsort(flat_e)
    sorted_e = flat_e[order]
    counts = jnp.bincount(flat_e, length=E)
    starts = jnp.cumsum(counts) - counts
    padded = ((counts + M - 1) // M) * M
    pad_ends = jnp.cumsum(padded)
    pad_starts = pad_ends - padded
    dest = pad_starts[sorted_e] + (jnp.arange(T * K) - starts[sorted_e])
    n_blocks = -(-(T * K) // M) + E
    n_rows = n_blocks * M
    row_tok = jnp.zeros((n_rows,), jnp.int32).at[dest].set(flat_tok[order])
    row_gate = jnp.zeros((n_rows,), jnp.float32).at[dest].set(flat_gate[order])
    block_e = jnp.minimum(jnp.searchsorted(pad_ends, jnp.arange(n_blocks) * M, side='right'), E - 1)

    def expert_block(y, args):
        e, tok, g = args
        xb = xt[tok]
        gu = xb @ w_gate_up[e] + b_gate_up[e]
        gate = jnp.minimum(gu[:, 0::2], SWIGLU_LIMIT)
        up = jnp.clip(gu[:, 1::2], -SWIGLU_LIMIT, SWIGLU_LIMIT)
        act = (up + 1.0) * (gate * jax.nn.sigmoid(SWIGLU_ALPHA * gate))
        yb = (act @ w_down[e] + b_down[e]) * g[:, None].astype(xt.dtype)
        return y.at[tok].add(yb), None

    y0 = jnp.zeros((T, D), xt.dtype)
    y, _ = lax.scan(expert_block, y0,
                    (block_e, row_tok.reshape(n_blocks, M), row_gate.reshape(n_blocks, M)))
    return y.reshape(Bsz, L, D)


def setup_inputs(seed: int = 0) -> dict:
    key = jax.random.key(seed)
    ks = jax.random.split(key, 40)
    f32 = jnp.float32
    nrm = lambda k, shape, s: jax.random.normal(k, shape, f32) * s
    gain = lambda k, shape: 1.0 + 0.02 * jax.random.normal(k, shape, f32)
    G, P, H = N_SSM_GROUPS, SSM_STATE, SSM_GROUP
    offsets = jax.random.randint(ks[2], (BATCH, 1), 0, 1024, dtype=jnp.int32) * CHUNK
    positions = offsets + jnp.arange(SEQ, dtype=jnp.int32)[None, :]
    A_im = math.pi * jnp.arange(P, dtype=f32)[None, None, :] + nrm(ks[7], (DEPTH, G, P), 0.01)
    log_dt = jax.random.uniform(ks[14], (DEPTH, G), f32, math.log(DT_MIN), math.log(DT_MAX))
    return {
        'x': nrm(ks[0], (BATCH, SEQ, D_MODEL), 1.0),
        'c': nrm(ks[1], (BATCH, D_MODEL), 1.0),
        'positions': positions,
        'w_ada': nrm(ks[3], (DEPTH, D_MODEL, N_MOD * D_MODEL), 0.5 * D_MODEL ** -0.5),
        'b_ada': nrm(ks[4], (DEPTH, N_MOD * D_MODEL), 0.01),
        'norm_mix_g': gain(ks[5], (DEPTH, D_MODEL)),
        'w_in': nrm(ks[6], (DEPTH, D_MODEL, D_IN), D_MODEL ** -0.5),
        'ssm_A_re': -0.5 + nrm(ks[8], (DEPTH, G, P), 0.01),
        'ssm_A_im': A_im,
        'ssm_B_re': nrm(ks[9], (DEPTH, G, P, H), (2 * H) ** -0.5),
        'ssm_B_im': nrm(ks[10], (DEPTH, G, P, H), (2 * H) ** -0.5),
        'ssm_C_re': nrm(ks[11], (DEPTH, G, H, P), (2 * P) ** -0.5),
        'ssm_C_im': nrm(ks[12], (DEPTH, G, H, P), (2 * P) ** -0.5),
        'ssm_D': nrm(ks[13], (DEPTH, D_SSM), 1.0),
        'ssm_log_dt': log_dt,
        'q_lat_g': gain(ks[15], (DEPTH, Q_LORA)),
        'kv_lat_g': gain(ks[16], (DEPTH, KV_LORA)),
        'w_uq': nrm(ks[17], (DEPTH, Q_LORA, N_MLA_HEADS * (QK_NOPE + QK_ROPE)), Q_LORA ** -0.5),
        'w_ukv': nrm(ks[18], (DEPTH, KV_LORA, N_MLA_HEADS * (QK_NOPE + V_HEAD)), KV_LORA ** -0.5),
        'q_nope_g': gain(ks[19], (DEPTH, QK_NOPE)),
        'q_rope_g': gain(ks[20], (DEPTH, QK_ROPE)),
        'k_nope_g': gain(ks[21], (DEPTH, QK_NOPE)),
        'k_rope_g': gain(ks[22], (DEPTH, QK_ROPE)),
        'out_ssm_g': gain(ks[23], (DEPTH, D_SSM)),
        'out_mla_g': gain(ks[24], (DEPTH, D_MLA)),
        'w_out': nrm(ks[25], (DEPTH, D_MIX, D_MODEL), D_MIX ** -0.5),
        'norm_ffn_g': gain(ks[26], (DEPTH, D_MODEL)),
        'w_router': nrm(ks[27], (DEPTH, D_MODEL, N_EXPERTS), D_MODEL ** -0.5),
        'b_router': nrm(ks[28], (DEPTH, N_EXPERTS), 0.01),
        'w_gate_up': nrm(ks[29], (DEPTH, N_EXPERTS, D_MODEL, 2 * D_FF), D_MODEL ** -0.5),
        'b_gate_up': nrm(ks[30], (DEPTH, N_EXPERTS, 2 * D_FF), 0.01),
        'w_down': nrm(ks[31], (DEPTH, N_EXPERTS, D_FF, D_MODEL), D_FF ** -0.5),
        'b_down': nrm(ks[32], (DEPTH, N_EXPERTS, D_MODEL), 0.01),
    }


def reference(x, c, positions, w_ada, b_ada, norm_mix_g, w_in, ssm_A_re, ssm_A_im, ssm_B_re,
              ssm_B_im, ssm_C_re, ssm_C_im, ssm_D, ssm_log_dt, q_lat_g, kv_lat_g, w_uq, w_ukv,
              q_nope_g, q_rope_g, k_nope_g, k_rope_g, out_ssm_g, out_mla_g, w_out, norm_ffn_g,
              w_router, b_router, w_gate_up, b_gate_up, w_down, b_down):
    inv_freq = ROPE_BASE ** (-jnp.arange(0, QK_ROPE, 2, dtype=jnp.float32) / QK_ROPE)
    ang = positions.astype(jnp.float32)[..., None] * inv_freq
    cos, sin = jnp.cos(ang), jnp.sin(ang)
    c_act = jax.nn.silu(c)
    s1, s2, s3 = 2 * D_SSM, 2 * D_SSM + Q_LORA, 2 * D_SSM + Q_LORA + KV_LORA
    for l in range(DEPTH):
        mod = c_act @ w_ada[l] + b_ada[l]
        sh1, sc1, g1, sh2, sc2, g2 = [m[:, None, :] for m in jnp.split(mod, N_MOD, axis=-1)]
        h = rms_norm(x, norm_mix_g[l]) * (1.0 + sc1) + sh1
        z = h @ w_in[l]
        u, g_ssm = z[..., :D_SSM], z[..., D_SSM:s1]
        q_lat, kv_lat, k_rope = z[..., s1:s2], z[..., s2:s3], z[..., s3:]
        y_s5 = s5_mixer(u, ssm_A_re[l], ssm_A_im[l], ssm_B_re[l], ssm_B_im[l],
                        ssm_C_re[l], ssm_C_im[l], ssm_D[l], ssm_log_dt[l])
        y_s5 = (jax.nn.gelu(y_s5) * jax.nn.sigmoid(g_ssm.astype(jnp.float32))).astype(x.dtype)
        y_mla = mla_mixer(q_lat, kv_lat, k_rope, cos, sin, q_lat_g[l], kv_lat_g[l], w_uq[l],
                          w_ukv[l], q_nope_g[l], q_rope_g[l], k_nope_g[l], k_rope_g[l])
        mix = jnp.concatenate([rms_norm(y_s5, out_ssm_g[l]), rms_norm(y_mla, out_mla_g[l])], axis=-1)
        x = x + g1 * (mix @ w_out[l])
        h2 = rms_norm(x, norm_ffn_g[l]) * (1.0 + sc2) + sh2
        x = x + g2 * moe_ffn(h2, w_router[l], b_router[l], w_gate_up[l], b_gate_up[l],
                             w_down[l], b_down[l])
    return x
```

```python
import math
import numpy as np
import ml_dtypes
from contextlib import ExitStack
import concourse.bass as bass
import concourse.mybir as mybir
from concourse.bass_utils import run_bass_kernel_spmd

F32 = mybir.dt.float32
BF16 = mybir.dt.bfloat16
I32 = mybir.dt.int32
U32 = mybir.dt.uint32
AF = mybir.ActivationFunctionType
ALU = mybir.AluOpType
AX = mybir.AxisListType

ENGS = ["pe", "act", "dve", "pool", "sp"]
D = 2048
NT = 16384
NOWN = 4096
EPS = 1e-6
NEG = -30000.0


class Buf:
    __slots__ = ("name", "last_w", "reads", "excl")

    def __init__(self, name, excl=False):
        self.name = name
        self.last_w = None
        self.reads = []
        self.excl = excl


class Ev:
    __slots__ = ("key", "val", "clock")

    def __init__(self, key, val, clock):
        self.key = key
        self.val = val
        self.clock = clock


class Sched:
    def __init__(self, nc, stack, n_dma_sems=48):
        self.nc = nc
        self.ops = {e: [] for e in ENGS}
        self.cnt = {e: 0 for e in ENGS}
        self.known = {e: {} for e in ENGS}
        self.sems = {}
        for e in ENGS:
            self.sems[e] = stack.enter_context(nc.semaphore("s_" + e))
        self.dma_sems = []
        for i in range(n_dma_sems):
            self.sems[("d", i)] = stack.enter_context(nc.semaphore("d_%d" % i))
            self.dma_sems.append({"key": ("d", i), "val": 0, "last": None})
        self.dma_rr = 0
        self.dma_rr_p = 0
        self.stopped = False

    def _need(self, eng, ev):
        if ev is None:
            return
        kn = self.known[eng]
        if kn.get(ev.key, 0) >= ev.val:
            return
        if ev.key == eng and eng == "pe":
            kn[ev.key] = ev.val
            return
        sem = self.sems[ev.key]
        val = ev.val
        self.ops[eng].append(lambda e, sem=sem, val=val: e.wait_ge(sem, val))
        for k, v in ev.clock.items():
            if kn.get(k, 0) < v:
                kn[k] = v
        kn[ev.key] = max(kn.get(ev.key, 0), ev.val)

    def _deps(self, eng, reads, writes):
        for b in reads:
            self._need(eng, b.last_w)
            if b.excl:
                for r in b.reads:
                    self._need(eng, r)
        for b in writes:
            self._need(eng, b.last_w)
            for r in b.reads:
                self._need(eng, r)

    def _commit(self, ev, reads, writes):
        for b in reads:
            b.reads.append(ev)
        for b in writes:
            b.last_w = ev
            b.reads = []

    def op(self, eng, fn, reads=(), writes=()):
        if self.stopped:
            return None
        self._deps(eng, reads, writes)
        self.cnt[eng] += 1
        idx = self.cnt[eng]
        sem = self.sems[eng]
        self.ops[eng].append(lambda e, fn=fn, sem=sem: fn(e).then_inc(sem, 1))
        clock = dict(self.known[eng])
        clock[eng] = idx
        ev = Ev(eng, idx, clock)
        self._commit(ev, reads, writes)
        return ev

    def dma(self, eng, fn, reads=(), writes=()):
        if self.stopped:
            return None
        self._deps(eng, reads, writes)
        pool_ = self.dma_sems[:8] if eng == "pool" else self.dma_sems[8:]
        rr = self.dma_rr_p if eng == "pool" else self.dma_rr
        n = len(pool_)
        pick = None
        for t in range(n):
            s_ = pool_[(rr + t) % n]
            if s_["last"] is None or self.known[eng].get(s_["key"], 0) >= s_["val"]:
                pick = s_
                rr = (rr + t + 1) % n
                break
        if pick is None:
            pick = pool_[rr % n]
            rr = (rr + 1) % n
            self._need(eng, pick["last"])
        if eng == "pool":
            self.dma_rr_p = rr
        else:
            self.dma_rr = rr
        pick["val"] += 16
        sem = self.sems[pick["key"]]
        self.ops[eng].append(lambda e, fn=fn, sem=sem: fn(e).then_inc(sem, 16))
        clock = dict(self.known[eng])
        clock[pick["key"]] = pick["val"]
        ev = Ev(pick["key"], pick["val"], clock)
        pick["last"] = ev
        self._commit(ev, reads, writes)
        return ev

    def barrier(self):
        evs = []
        for e in ENGS:
            if self.cnt[e] > 0:
                clock = dict(self.known[e])
                clock[e] = self.cnt[e]
                evs.append(Ev(e, self.cnt[e], clock))
        for s_ in self.dma_sems:
            if s_["last"] is not None:
                evs.append(s_["last"])
        for e in ENGS:
            for ev in evs:
                kn = self.known[e]
                if kn.get(ev.key, 0) >= ev.val:
                    continue
                sem = self.sems[ev.key]
                val = ev.val
                self.ops[e].append(lambda en, sem=sem, val=val: en.wait_ge(sem, val))
                for k, v in ev.clock.items():
                    if kn.get(k, 0) < v:
                        kn[k] = v
                kn[ev.key] = max(kn.get(ev.key, 0), ev.val)

    def emit(self):
        nc = self.nc
        with nc.Block() as block:
            @block.tensor
            def _(e):
                for t in self.ops["pe"]:
                    t(e)

            @block.scalar
            def _(e):
                for t in self.ops["act"]:
                    t(e)

            @block.vector
            def _(e):
                for t in self.ops["dve"]:
                    t(e)

            @block.gpsimd
            def _(e):
                for t in self.ops["pool"]:
                    t(e)

            @block.sync
            def _(e):
                for t in self.ops["sp"]:
                    t(e)


class TT:
    __slots__ = ("t", "b")

    def __init__(self, t, name, excl=False):
        self.t = t
        self.b = Buf(name, excl)

    def __getitem__(self, k):
        return self.t[k]


class Ring:
    def __init__(self, items):
        self.items = items
        self.i = 0

    def get(self):
        it = self.items[self.i % len(self.items)]
        self.i += 1
        return it


INV_FREQ = [float(np.float32(10000.0) ** np.float32(-(2.0 * i) / 64.0)) for i in range(32)]
TWO_PI = 2.0 * math.pi
CW1 = 6.28125
CW2 = TWO_PI - 6.28125


def DMA(e, **kw):
    return e.dma_start(allow_slow_non_contiguous=True, **kw)


class Cut(Exception):
    pass


def build(stage=99, nblk=16, nown=4, cut=0, NE=32):
    NT = nblk * 1024
    NOWN = nown * 1024
    nc = bass.Bass("TRN2", target_bir_lowering=False)
    din = lambda n, sh, dt=F32: nc.dram_tensor(n, sh, dt, kind="ExternalInput").ap()
    xp = din("xp", [NT, D])
    pos_in = din("pos", [128, 128])
    valid_in = din("valid", [128, 16])
    kbias_in = din("kbias", [128, 128])
    c_in = din("c", [D])
    w_ada = din("w_ada", [D, 6 * D])
    b_ada = din("b_ada", [6 * D])
    g_mix = din("norm_mix_g", [D])
    w_in = din("w_in", [D, 2880])
    q_lat_g = din("q_lat_g", [512])
    kv_lat_g = din("kv_lat_g", [256])
    w_uq = din("w_uq", [512, 1536])
    w_ukv = din("w_ukv", [256, 2048])
    q_nope_g = din("q_nope_g", [128])
    q_rope_g = din("q_rope_g", [64])
    k_nope_g = din("k_nope_g", [128])
    k_rope_g = din("k_rope_g", [64])
    ssm_A_re = din("ssm_A_re", [64, 64])
    ssm_A_im = din("ssm_A_im", [64, 64])
    ssm_B_re = din("ssm_B_re", [64, 64, 16])
    ssm_B_im = din("ssm_B_im", [64, 64, 16])
    ssm_C_re = din("ssm_C_re", [64, 16, 64])
    ssm_C_im = din("ssm_C_im", [64, 16, 64])
    ssm_D = din("ssm_D", [1024])
    ssm_log_dt = din("ssm_log_dt", [64])
    out_ssm_g = din("out_ssm_g", [1024])
    out_mla_g = din("out_mla_g", [1024])
    w_out = din("w_out", [2048, 2048])
    norm_ffn_g = din("norm_ffn_g", [2048])
    w_router = din("w_router", [2048, NE])
    b_router = din("b_router", [NE])
    w_gate_up = din("w_gate_up", [NE, 2048, 4096])
    b_gate_up = din("b_gate_up", [NE, 4096])
    w_down = din("w_down", [NE, 2048, 2048])
    b_down = din("b_down", [NE, 2048])
    out = nc.dram_tensor("out", [NOWN, D], F32, kind="ExternalOutput").ap()
    x1_d = nc.dram_tensor("x1_d", [NOWN, D], F32).ap()
    NCH = NT // 8
    Ud = nc.dram_tensor("Ud", [64, 128, NCH], BF16).ap()
    Gd = nc.dram_tensor("Gd", [NOWN // 1024, 128, 8, 1024], BF16).ap()
    ys5_d = nc.dram_tensor("ys5_d", [NOWN, 1024], BF16).ap()

    kT_d = nc.dram_tensor("kT_d", [8, 128, NT], BF16).ap()
    krT_d = nc.dram_tensor("krT_d", [64, NT], BF16).ap()
    V_d = nc.dram_tensor("V_d", [NT, 8, 128], BF16).ap()
    qTn_d = nc.dram_tensor("qTn_d", [8, 128, NOWN], BF16).ap()
    qTr_d = nc.dram_tensor("qTr_d", [4, 128, NOWN], BF16).ap()
    ymla_d = nc.dram_tensor("ymla_d", [NOWN, 1024], BF16).ap()
    dbg = {}
    if stage == 0:
        dbg["mod"] = nc.dram_tensor("dbg_mod", [128, 32], F32, kind="ExternalOutput").ap()
    if stage == 1:
        dbg["kT"] = nc.dram_tensor("dbg_kT", [8, 128, NT], BF16, kind="ExternalOutput").ap()
        dbg["krT"] = nc.dram_tensor("dbg_krT", [64, NT], BF16, kind="ExternalOutput").ap()
        dbg["V"] = nc.dram_tensor("dbg_V", [NT, 8, 128], BF16, kind="ExternalOutput").ap()
        dbg["mod"] = nc.dram_tensor("dbg_mod", [128, 32], F32, kind="ExternalOutput").ap()
        dbg["qTn"] = nc.dram_tensor("dbg_qTn", [8, 128, NOWN], BF16, kind="ExternalOutput").ap()
        dbg["qTr"] = nc.dram_tensor("dbg_qTr", [4, 128, NOWN], BF16, kind="ExternalOutput").ap()
    if stage == 4:
        dbg["x1"] = nc.dram_tensor("dbg_x1", [NOWN, D], F32, kind="ExternalOutput").ap()
    if stage == 3:
        dbg["ys5"] = nc.dram_tensor("dbg_ys5", [NOWN, 1024], BF16, kind="ExternalOutput").ap()
    if stage == 2:
        dbg["ymla"] = nc.dram_tensor("dbg_ymla", [NOWN, 1024], BF16, kind="ExternalOutput").ap()

    with ExitStack() as root:
        s = Sched(nc, root)

        def sbt(st, name, shape, dt):
            return TT(st.enter_context(nc.sbuf_tensor("sb_" + name, shape, dt)), name)

        def pst(st, name, shape, dt):
            return TT(st.enter_context(nc.psum_tensor("ps_" + name, shape, dt)), name, True)

        ident_i = sbt(root, "ident_i", [128, 128], I32)
        ident_f = sbt(root, "ident_f", [128, 128], F32)
        ident = sbt(root, "ident", [128, 128], BF16)
        ones_r = sbt(root, "ones_r", [1, 128], BF16)
        s.op("pool", lambda e: e.iota(ident_i[:], [[1, 128]], base=0, channel_multiplier=-1), writes=[ident_i.b])
        s.op("dve", lambda e: e.tensor_scalar(out=ident_f[:], in0=ident_i[:], scalar1=0, scalar2=None, op0=ALU.is_equal),
             reads=[ident_i.b], writes=[ident_f.b])
        s.op("dve", lambda e: e.tensor_copy(ident[:], ident_f[:]), reads=[ident_f.b], writes=[ident.b])
        s.op("dve", lambda e: e.memset(ones_r[:], 1.0), writes=[ones_r.b])

        sh1c = sbt(root, "sh1c", [128, 16], F32)
        a1c = sbt(root, "a1c", [128, 16], F32)
        sh1c16 = sbt(root, "sh1c16", [128, 16], BF16)

        with ExitStack() as p0:
            cT = sbt(p0, "cT", [128, 16], F32)
            cact = sbt(p0, "cact", [128, 16], BF16)
            s.dma("sp", lambda e: DMA(e, out=cT[:], in_=c_in.rearrange("(k p) -> p k", p=128)), writes=[cT.b])
            s.op("act", lambda e: e.activation(out=cact[:], in_=cT[:], func=AF.Silu), reads=[cT.b], writes=[cact.b])
            wa = Ring([sbt(p0, "wa%d" % i, [128, 16, 512], BF16) for i in range(2)])
            pacc = Ring([pst(p0, "pacc%d" % i, [128, 512], F32) for i in range(2)])
            modc = sbt(p0, "modc", [128, 32], F32)
            badc = sbt(p0, "badc", [128, 32], F32)
            gmc = sbt(p0, "gmc", [128, 16], F32)
            s.dma("sp", lambda e: DMA(e, out=badc[:], in_=b_ada[0:4096].rearrange("(k p) -> p k", p=128)), writes=[badc.b])
            s.dma("sp", lambda e: DMA(e, out=gmc[:], in_=g_mix.rearrange("(k p) -> p k", p=128)), writes=[gmc.b])
            for j in range(8):
                w = wa.get()
                s.dma("pool", lambda e, w=w, j=j: DMA(e, out=w[:], in_=w_ada[:, j * 512:(j + 1) * 512].rearrange("(k p) n -> p k n", p=128)),
                      writes=[w.b])
                pa = pacc.get()
                for sub in range(4):
                    for k in range(16):
                        s.op("pe", lambda e, w=w, pa=pa, sub=sub, k=k: e.matmul(pa[:, sub:sub + 1], w[:, k, sub * 128:(sub + 1) * 128], cact[:, k:k + 1],
                                                                               start=(k == 0), stop=(k == 15)),
                             reads=[w.b, cact.b], writes=[pa.b])
                s.op("dve", lambda e, pa=pa, j=j: e.tensor_tensor(out=modc[:, j * 4:(j + 1) * 4], in0=pa[:, 0:4], in1=badc[:, j * 4:(j + 1) * 4], op=ALU.add),
                     reads=[pa.b, badc.b], writes=[modc.b])
            s.op("dve", lambda e: e.tensor_copy(sh1c[:], modc[:, 0:16]), reads=[modc.b], writes=[sh1c.b])
            s.op("dve", lambda e: e.tensor_copy(sh1c16[:], modc[:, 0:16]), reads=[modc.b], writes=[sh1c16.b])
            s.op("dve", lambda e: e.scalar_tensor_tensor(out=a1c[:], in0=modc[:, 16:32], scalar=1.0, in1=gmc[:], op0=ALU.add, op1=ALU.mult),
                 reads=[modc.b, gmc.b], writes=[a1c.b])
            if stage <= 1:
                s.dma("sp", lambda e: DMA(e, out=dbg["mod"], in_=modc[:]), reads=[modc.b])
            s.barrier()
        if stage == 0:
            s.barrier()
            s.emit()
            return nc

        def cp(n):
            if cut == n:
                s.stopped = True
        try:
          with ExitStack() as p1:
              NCA = 320
              wA = sbt(p1, "wA", [128, 16, NCA], BF16)
              biasA = sbt(p1, "biasA", [1, NCA], BF16)
              wukv = sbt(p1, "wukv", [128, 2, 2048], BF16)
              kvg = sbt(p1, "kvg", [128, 2], F32)
              gk = sbt(p1, "gk", [128, 1], F32)
              gq_ = sbt(p1, "gq_", [128, 1], F32)
              gkr = sbt(p1, "gkr", [128, 64], F32)
              posT = sbt(p1, "posT", [128, 128], F32)
              cosT = sbt(p1, "cosT", [128, 128, 32], BF16)
              sinT = sbt(p1, "sinT", [128, 128, 32], BF16)
              kbias = sbt(p1, "kbias", [128, 128], F32)
              s.dma("pool", lambda e: DMA(e, out=wA[:], in_=w_in[:, 2560:2880].rearrange("(k p) n -> p k n", p=128)), writes=[wA.b])
              s.dma("pool", lambda e: DMA(e, out=wukv[:], in_=w_ukv.rearrange("(k p) n -> p k n", p=128)), writes=[wukv.b])
              s.dma("sp", lambda e: DMA(e, out=kvg[:], in_=kv_lat_g.rearrange("(k p) -> p k", p=128)), writes=[kvg.b])
              s.dma("sp", lambda e: DMA(e, out=gk[:], in_=k_nope_g.rearrange("(p o) -> p o", o=1)), writes=[gk.b])
              s.dma("sp", lambda e: DMA(e, out=gq_[:], in_=q_nope_g.rearrange("(p o) -> p o", o=1)), writes=[gq_.b])
              s.dma("sp", lambda e: DMA(e, out=gkr[:], in_=k_rope_g.partition_broadcast(128)), writes=[gkr.b])
              s.op("dve", lambda e: e.tensor_tensor(out=gk[:], in0=gk[:], in1=gq_[:], op=ALU.mult), reads=[gk.b, gq_.b], writes=[gk.b])
              pb = pst(p1, "pb", [1, 512], F32)
              for k in range(16):
                  s.op("pe", lambda e, k=k: e.matmul(pb[:, 0:NCA], sh1c16[:, k:k + 1], wA[:, k, :], start=(k == 0), stop=(k == 15)),
                       reads=[sh1c16.b, wA.b], writes=[pb.b])
              s.op("dve", lambda e: e.tensor_copy(biasA[:], pb[:, 0:NCA]), reads=[pb.b], writes=[biasA.b])
              for k in range(16):
                  s.op("dve", lambda e, k=k: e.tensor_scalar(out=wA[:, k, :], in0=wA[:, k, :], scalar1=a1c[:, k:k + 1], scalar2=None, op0=ALU.mult),
                       reads=[wA.b, a1c.b], writes=[wA.b])
              for k in range(2):
                  s.op("dve", lambda e, k=k: e.tensor_scalar(out=wukv[:, k, :], in0=wukv[:, k, :], scalar1=kvg[:, k:k + 1], scalar2=None, op0=ALU.mult),
                       reads=[wukv.b, kvg.b], writes=[wukv.b])
              cp(1)
              ptile = sbt(p1, "ptile", [128, 128], F32)
              ppos = pst(p1, "ppos", [128, 512], F32)
              s.dma("sp", lambda e: DMA(e, out=ptile[:], in_=pos_in), writes=[ptile.b])
              s.dma("sp", lambda e: DMA(e, out=kbias[:], in_=kbias_in), writes=[kbias.b])
              s.op("pe", lambda e: e.transpose(ppos[:, 0:128], ptile[:], ident_f[:]), reads=[ptile.b, ident_f.b], writes=[ppos.b])
              s.op("dve", lambda e: e.tensor_copy(posT[:], ppos[:, 0:128]), reads=[ppos.b], writes=[posT.b])
              ptrig = ExitStack()
              ang = sbt(ptrig, "ang", [128, 128, 32], F32)
              kf = sbt(ptrig, "kf", [128, 128, 32], F32)
              ki = sbt(ptrig, "ki", [128, 128, 32], I32)
              for i in range(32):
                  s.op("dve", lambda e, i=i: e.tensor_scalar(out=ang[:, :, i], in0=posT[:], scalar1=INV_FREQ[i], scalar2=None, op0=ALU.mult),
                       reads=[posT.b], writes=[ang.b])

              def wrap(t):
                  s.op("dve", lambda e: e.tensor_scalar(out=kf[:], in0=t[:], scalar1=math.pi, scalar2=-TWO_PI, op0=ALU.is_gt, op1=ALU.mult),
                       reads=[t.b], writes=[kf.b])
                  s.op("dve", lambda e: e.tensor_tensor(out=t[:], in0=t[:], in1=kf[:], op=ALU.add), reads=[t.b, kf.b], writes=[t.b])
                  s.op("dve", lambda e: e.tensor_scalar(out=kf[:], in0=t[:], scalar1=-math.pi, scalar2=TWO_PI, op0=ALU.is_lt, op1=ALU.mult),
                       reads=[t.b], writes=[kf.b])
                  s.op("dve", lambda e: e.tensor_tensor(out=t[:], in0=t[:], in1=kf[:], op=ALU.add), reads=[t.b, kf.b], writes=[t.b])

              s.op("dve", lambda e: e.tensor_scalar(out=kf[:], in0=ang[:], scalar1=1.0 / TWO_PI, scalar2=None, op0=ALU.mult), reads=[ang.b], writes=[kf.b])
              s.op("dve", lambda e: e.tensor_copy(ki[:], kf[:]), reads=[kf.b], writes=[ki.b])
              s.op("dve", lambda e: e.tensor_copy(kf[:], ki[:]), reads=[ki.b], writes=[kf.b])
              s.op("dve", lambda e: e.scalar_tensor_tensor(out=ang[:], in0=kf[:], scalar=-CW1, in1=ang[:], op0=ALU.mult, op1=ALU.add),
                   reads=[kf.b, ang.b], writes=[ang.b])
              s.op("dve", lambda e: e.scalar_tensor_tensor(out=ang[:], in0=kf[:], scalar=-CW2, in1=ang[:], op0=ALU.mult, op1=ALU.add),
                   reads=[kf.b, ang.b], writes=[ang.b])
              wrap(ang)
              s.op("act", lambda e: e.activation(out=sinT[:], in_=ang[:], func=AF.Sin), reads=[ang.b], writes=[sinT.b])
              s.op("dve", lambda e: e.tensor_scalar(out=ang[:], in0=ang[:], scalar1=math.pi / 2, scalar2=None, op0=ALU.add), reads=[ang.b], writes=[ang.b])
              wrap(ang)
              s.op("act", lambda e: e.activation(out=cosT[:], in_=ang[:], func=AF.Sin), reads=[ang.b], writes=[cosT.b])

              s.barrier()
              ptrig.close()
              cp(2)
              wQ = sbt(p1, "wQ", [128, 16, 512], BF16)
              biasQ = sbt(p1, "biasQ", [1, 512], BF16)
              wuq = sbt(p1, "wuq", [128, 4, 1536], BF16)
              qlg = sbt(p1, "qlg", [128, 4], F32)
              gqr = sbt(p1, "gqr", [128, 64], F32)
              s.dma("pool", lambda e: DMA(e, out=wQ[:], in_=w_in[:, 2048:2560].rearrange("(k p) n -> p k n", p=128)), writes=[wQ.b])
              s.dma("pool", lambda e: DMA(e, out=wuq[:], in_=w_uq.rearrange("(k p) n -> p k n", p=128)), writes=[wuq.b])
              s.dma("sp", lambda e: DMA(e, out=qlg[:], in_=q_lat_g.rearrange("(k p) -> p k", p=128)), writes=[qlg.b])
              s.dma("sp", lambda e: DMA(e, out=gqr[:], in_=q_rope_g.partition_broadcast(128)), writes=[gqr.b])
              for k in range(16):
                  s.op("pe", lambda e, k=k: e.matmul(pb[:, 0:512], sh1c16[:, k:k + 1], wQ[:, k, :], start=(k == 0), stop=(k == 15)),
                       reads=[sh1c16.b, wQ.b], writes=[pb.b])
              s.op("dve", lambda e: e.tensor_copy(biasQ[:], pb[:, 0:512]), reads=[pb.b], writes=[biasQ.b])
              for k in range(16):
                  s.op("dve", lambda e, k=k: e.tensor_scalar(out=wQ[:, k, :], in0=wQ[:, k, :], scalar1=a1c[:, k:k + 1], scalar2=None, op0=ALU.mult),
                       reads=[wQ.b, a1c.b], writes=[wQ.b])
              for k in range(4):
                  s.op("dve", lambda e, k=k: e.tensor_scalar(out=wuq[:, k, :], in0=wuq[:, k, :], scalar1=qlg[:, k:k + 1], scalar2=None, op0=ALU.mult),
                       reads=[wuq.b, qlg.b], writes=[wuq.b])
              qn16 = Ring([sbt(p1, "qn16%d" % i, [128, 512], BF16) for i in range(2)])
              qnT = Ring([sbt(p1, "qnT%d" % i, [128, 4, 128], BF16) for i in range(2)])
              sqq = sbt(p1, "sqq", [128, 8, 192], F32)
              ssq = Ring([sbt(p1, "ssq%d" % i, [128, 16], F32) for i in range(2)])
              qno16 = Ring([sbt(p1, "qno%d" % i, [128, 8, 128], BF16) for i in range(2)])
              qrA = sbt(p1, "qrA", [128, 8, 64], F32)
              qrB = sbt(p1, "qrB", [128, 8, 64], F32)
              qrC = sbt(p1, "qrC", [128, 8, 64], F32)
              qr16 = Ring([sbt(p1, "qr16%d" % i, [128, 8, 64], BF16) for i in range(2)])
              qnstR = Ring([sbt(p1, "qnst%d" % i, [128, 8, 128], BF16) for i in range(2)])
              qrstR = Ring([sbt(p1, "qrst%d" % i, [128, 4, 128], BF16) for i in range(2)])
              xpool = Ring([sbt(p1, "xt%d" % i, [128, D], F32) for i in range(2)])
              junk = sbt(p1, "junk", [128, D], BF16)
              xh = Ring([sbt(p1, "xh%d" % i, [128, D], BF16) for i in range(2)])
              xhT = sbt(p1, "xhT", [128, 16, 1024], BF16)
              ptr = Ring([pst(p1, "ptr%d" % i, [128, 1024], BF16) for i in range(2)])
              pacc = Ring([pst(p1, "pa%d" % i, [128, 512], F32) for i in range(4)])
              ssr = Ring([sbt(p1, "ss%d" % i, [128, 4], F32) for i in range(4)])
              kvn = Ring([sbt(p1, "kvn%d" % i, [128, 256], BF16) for i in range(2)])
              kvnT = Ring([sbt(p1, "kvnT%d" % i, [128, 2, 128], BF16) for i in range(2)])
              kr = Ring([sbt(p1, "kr%d" % i, [128, 64], F32) for i in range(2)])
              kr2 = Ring([sbt(p1, "krb%d" % i, [128, 64], F32) for i in range(2)])
              kr16 = Ring([sbt(p1, "krc%d" % i, [128, 128], BF16) for i in range(2)])
              for t_ in kr16.items:
                  s.op("dve", lambda e, t_=t_: e.memset(t_[:], 0.0), writes=[t_.b])
              sq = Ring([sbt(p1, "sq%d" % i, [128, 8, 128], F32) for i in range(2)])
              kn16 = Ring([sbt(p1, "kn16%d" % i, [128, 8, 128], BF16) for i in range(2)])
              v16 = Ring([sbt(p1, "v16%d" % i, [128, 8, 128], BF16) for i in range(2)])
              kstR = Ring([sbt(p1, "kst%d" % i, [128, 8, 128], BF16) for i in range(2)])
              krst = sbt(p1, "krst", [128, 1024], BF16)
              ev_alt = [0]

              def evac(out_ap, in_ap, reads, writes):
                  ev_alt[0] += 1
                  if ev_alt[0] % 2:
                      s.op("dve", lambda e: e.tensor_copy(out_ap, in_ap), reads=reads, writes=writes)
                  else:
                      s.op("act", lambda e: e.activation(out=out_ap, in_=in_ap, func=AF.Copy), reads=reads, writes=writes)

              def rstd_from_ss(ss, col, n):
                  s.op("dve", lambda e: e.tensor_scalar(out=ss[:, col:col + 1], in0=ss[:, col:col + 1], scalar1=1.0 / n, scalar2=EPS, op0=ALU.mult, op1=ALU.add),
                       reads=[ss.b], writes=[ss.b])
                  s.op("act", lambda e: e.activation(out=ss[:, col:col + 1], in_=ss[:, col:col + 1], func=AF.Sqrt), reads=[ss.b], writes=[ss.b])
                  s.op("dve", lambda e: e.reciprocal(ss[:, col:col + 1], ss[:, col:col + 1]), reads=[ss.b], writes=[ss.b])

              def make_xhT(blk):
                  for tt in range(8):
                      r0 = blk * 1024 + tt * 128
                      xt = xpool.get()
                      s.dma("sp", lambda e, xt=xt, r0=r0: DMA(e, out=xt[:], in_=xp[r0:r0 + 128, :]), writes=[xt.b])
                      ss = ssr.get()
                      s.op("act", lambda e, xt=xt, ss=ss: e.activation(out=junk[:], in_=xt[:], func=AF.Square, accum_out=ss[:, 0:1]),
                           reads=[xt.b], writes=[junk.b, ss.b])
                      rstd_from_ss(ss, 0, D)
                      xb = xh.get()
                      s.op("dve", lambda e, xt=xt, ss=ss, xb=xb: e.tensor_scalar(out=xb[:], in0=xt[:], scalar1=ss[:, 0:1], scalar2=None, op0=ALU.mult),
                           reads=[xt.b, ss.b], writes=[xb.b])
                      for half in range(2):
                          pt = ptr.get()
                          for k8 in range(8):
                              k = half * 8 + k8
                              s.op("pe", lambda e, pt=pt, xb=xb, k8=k8, k=k: e.transpose(pt[:, k8 * 128:(k8 + 1) * 128], xb[:, k * 128:(k + 1) * 128], ident[:]),
                                   reads=[xb.b, ident.b], writes=[pt.b])
                          evac(xhT[:, half * 8:(half + 1) * 8, tt * 128:(tt + 1) * 128], pt[:].rearrange("p (a b) -> p a b", a=8), [pt.b], [xhT.b])

              for blk in range(nblk):
                  make_xhT(blk)
                  cp(3)
                  for tt in range(8):
                      tile_i = blk * 8 + tt
                      r0 = tile_i * 128
                      pa = pacc.get()
                      for k in range(16):
                          s.op("pe", lambda e, pa=pa, k=k, tt=tt: e.matmul(pa[:, 0:NCA], xhT[:, k, tt * 128:(tt + 1) * 128], wA[:, k, :], start=(k == 0), stop=False),
                               reads=[xhT.b, wA.b], writes=[pa.b])
                      s.op("pe", lambda e, pa=pa: e.matmul(pa[:, 0:NCA], ones_r[:], biasA[:], start=False, stop=True),
                           reads=[ones_r.b, biasA.b], writes=[pa.b])
                      ss = ssr.get()
                      s.op("act", lambda e, pa=pa, ss=ss: e.activation(out=junk[:, 0:256], in_=pa[:, 0:256], func=AF.Square, accum_out=ss[:, 0:1]),
                           reads=[pa.b], writes=[junk.b, ss.b])
                      s.op("act", lambda e, pa=pa, ss=ss: e.activation(out=junk[:, 256:320], in_=pa[:, 256:320], func=AF.Square, accum_out=ss[:, 1:2]),
                           reads=[pa.b], writes=[junk.b, ss.b])
                      rstd_from_ss(ss, 0, 256)
                      rstd_from_ss(ss, 1, 64)
                      kn_ = kvn.get()
                      s.op("dve", lambda e, pa=pa, ss=ss, kn_=kn_: e.tensor_scalar(out=kn_[:], in0=pa[:, 0:256], scalar1=ss[:, 0:1], scalar2=None, op0=ALU.mult),
                           reads=[pa.b, ss.b], writes=[kn_.b])
                      cp(4)
                      k1 = kr.get()
                      s.op("dve", lambda e, pa=pa, ss=ss, k1=k1: e.scalar_tensor_tensor(out=k1[:], in0=pa[:, 256:320], scalar=ss[:, 1:2], in1=gkr[:], op0=ALU.mult, op1=ALU.mult),
                           reads=[pa.b, ss.b, gkr.b], writes=[k1.b])
                      k2 = kr2.get()
                      k3 = kr16.get()
                      cs = cosT[:, tile_i, :]
                      sn = sinT[:, tile_i, :]
                      s.op("pool", lambda e, k1=k1, k2=k2, cs=cs: e.tensor_tensor(out=k2[:, 0:32], in0=k1[:, 0:32], in1=cs, op=ALU.mult), reads=[k1.b, cosT.b], writes=[k2.b])
                      s.op("pool", lambda e, k1=k1, k2=k2, cs=cs: e.tensor_tensor(out=k2[:, 32:64], in0=k1[:, 32:64], in1=cs, op=ALU.mult), reads=[k1.b, cosT.b], writes=[k2.b])
                      tmpa = kr.get()
                      s.op("pool", lambda e, k1=k1, tmpa=tmpa, sn=sn: e.tensor_tensor(out=tmpa[:, 0:32], in0=k1[:, 32:64], in1=sn, op=ALU.mult), reads=[k1.b, sinT.b], writes=[tmpa.b])
                      s.op("pool", lambda e, k1=k1, tmpa=tmpa, sn=sn: e.tensor_tensor(out=tmpa[:, 32:64], in0=k1[:, 0:32], in1=sn, op=ALU.mult), reads=[k1.b, sinT.b], writes=[tmpa.b])
                      s.op("pool", lambda e, k2=k2, k3=k3, tmpa=tmpa: e.tensor_tensor(out=k3[:, 0:32], in0=k2[:, 0:32], in1=tmpa[:, 0:32], op=ALU.subtract), reads=[k2.b, tmpa.b], writes=[k3.b])
                      s.op("pool", lambda e, k2=k2, k3=k3, tmpa=tmpa: e.tensor_tensor(out=k3[:, 32:64], in0=k2[:, 32:64], in1=tmpa[:, 32:64], op=ALU.add), reads=[k2.b, tmpa.b], writes=[k3.b])
                      cp(5)
                      pt = ptr.get()
                      for k in range(2):
                          s.op("pe", lambda e, pt=pt, kn_=kn_, k=k: e.transpose(pt[:, k * 128:(k + 1) * 128], kn_[:, k * 128:(k + 1) * 128], ident[:]),
                               reads=[kn_.b, ident.b], writes=[pt.b])
                      s.op("pe", lambda e, pt=pt, k3=k3: e.transpose(pt[:, 256:384], k3[:, :], ident[:]), reads=[k3.b, ident.b], writes=[pt.b])
                      cp(8)
                      kT_ = kvnT.get()
                      evac(kT_[:], pt[:, 0:256].rearrange("p (a b) -> p a b", a=2), [pt.b], [kT_.b])
                      evac(krst[:, tt * 128:(tt + 1) * 128], pt[:, 256:384], [pt.b], [krst.b])
                      cp(6)
                      sq_ = sq.get()
                      k16 = kn16.get()
                      v_ = v16.get()
                      pus = []
                      for cg in range(4):
                          pu = pacc.get()
                          pus.append(pu)
                          for k in range(2):
                              s.op("pe", lambda e, pu=pu, kT_=kT_, k=k, cg=cg: e.matmul(pu[:], kT_[:, k, :], wukv[:, k, cg * 512:(cg + 1) * 512], start=(k == 0), stop=(k == 1)),
                                   reads=[kT_.b, wukv.b], writes=[pu.b])
                          puv = pu[:].rearrange("p (h d) -> p h d", h=2)
                          s.op("act", lambda e, puv=puv, sq_=sq_, cg=cg: e.activation(out=sq_[:, 2 * cg:2 * cg + 2, :], in_=puv[:, :, 0:128], func=AF.Square),
                               reads=[pu.b], writes=[sq_.b])
                          s.op("act", lambda e, puv=puv, v_=v_, cg=cg: e.activation(out=v_[:, 2 * cg:2 * cg + 2, :], in_=puv[:, :, 128:256], func=AF.Copy),
                               reads=[pu.b], writes=[v_.b])
                      ss2 = ssr.get()
                      ss8 = sq.get()
                      s.op("dve", lambda e, sq_=sq_, ss8=ss8: e.tensor_reduce(out=ss8[:, :, 0], in_=sq_[:], axis=AX.X, op=ALU.add), reads=[sq_.b], writes=[ss8.b])
                      s.op("dve", lambda e, ss8=ss8: e.tensor_scalar(out=ss8[:, :, 0], in0=ss8[:, :, 0], scalar1=1.0 / 128, scalar2=EPS, op0=ALU.mult, op1=ALU.add),
                           reads=[ss8.b], writes=[ss8.b])
                      s.op("act", lambda e, ss8=ss8: e.activation(out=ss8[:, :, 0], in_=ss8[:, :, 0], func=AF.Sqrt), reads=[ss8.b], writes=[ss8.b])
                      s.op("dve", lambda e, ss8=ss8: e.reciprocal(ss8[:, :, 0], ss8[:, :, 0]), reads=[ss8.b], writes=[ss8.b])
                      for cg in range(4):
                          pu = pus[cg]
                          puv = pu[:].rearrange("p (h d) -> p h d", h=2)
                          s.op("dve", lambda e, puv=puv, k16=k16, ss8=ss8, cg=cg: e.tensor_tensor(out=k16[:, 2 * cg:2 * cg + 2, :], in0=puv[:, :, 0:128],
                                                                                                    in1=ss8[:, 2 * cg:2 * cg + 2, 0:1].to_broadcast([128, 2, 128]), op=ALU.mult),
                               reads=[pu.b, ss8.b], writes=[k16.b])
                      cp(7)
                      s.dma("sp", lambda e, v_=v_, r0=r0: DMA(e, out=V_d[r0:r0 + 128, :, :], in_=v_[:]), reads=[v_.b])
                      pt2 = ptr.get()
                      for h in range(8):
                          s.op("pe", lambda e, pt2=pt2, k16=k16, h=h: e.transpose(pt2[:, h * 128:(h + 1) * 128], k16[:, h, :], ident[:]),
                               reads=[k16.b, ident.b], writes=[pt2.b])
                      kst = kstR.get()
                      s.op("dve", lambda e, pt2=pt2, kst=kst: e.tensor_scalar(out=kst[:], in0=pt2[:].rearrange("p (a b) -> p a b", a=8),
                                                                           scalar1=gk[:, 0:1], scalar2=None, op0=ALU.mult),
                           reads=[pt2.b, gk.b], writes=[kst.b])
                      s.dma("sp", lambda e, kst=kst, r0=r0: DMA(e, out=kT_d[:, :, r0:r0 + 128].rearrange("h d t -> d h t"), in_=kst[:]), reads=[kst.b])

                      if blk >= nblk - nown:
                          pq = pacc.get()
                          for k in range(16):
                              s.op("pe", lambda e, pq=pq, k=k, tt=tt: e.matmul(pq[:], xhT[:, k, tt * 128:(tt + 1) * 128], wQ[:, k, :], start=(k == 0), stop=False),
                                   reads=[xhT.b, wQ.b], writes=[pq.b])
                          s.op("pe", lambda e, pq=pq: e.matmul(pq[:], ones_r[:], biasQ[:], start=False, stop=True), reads=[ones_r.b, biasQ.b], writes=[pq.b])
                          ssA = ssr.get()
                          s.op("act", lambda e, pq=pq, ssA=ssA: e.activation(out=junk[:, 0:512], in_=pq[:], func=AF.Square, accum_out=ssA[:, 0:1]),
                               reads=[pq.b], writes=[junk.b, ssA.b])
                          rstd_from_ss(ssA, 0, 512)
                          qn_ = qn16.get()
                          s.op("dve", lambda e, pq=pq, ssA=ssA, qn_=qn_: e.tensor_scalar(out=qn_[:], in0=pq[:], scalar1=ssA[:, 0:1], scalar2=None, op0=ALU.mult),
                               reads=[pq.b, ssA.b], writes=[qn_.b])
                          ptq = ptr.get()
                          for k in range(4):
                              s.op("pe", lambda e, ptq=ptq, qn_=qn_, k=k: e.transpose(ptq[:, k * 128:(k + 1) * 128], qn_[:, k * 128:(k + 1) * 128], ident[:]),
                                   reads=[qn_.b, ident.b], writes=[ptq.b])
                          qT_ = qnT.get()
                          evac(qT_[:], ptq[:, 0:512].rearrange("p (a b) -> p a b", a=4), [ptq.b], [qT_.b])
                          pqs = []
                          for cg in range(4):
                              pu = pacc.get()
                              pqs.append(pu)
                              for k in range(4):
                                  s.op("pe", lambda e, pu=pu, qT_=qT_, k=k, cg=cg: e.matmul(pu[:, 0:384], qT_[:, k, :], wuq[:, k, cg * 384:(cg + 1) * 384], start=(k == 0), stop=(k == 3)),
                                       reads=[qT_.b, wuq.b], writes=[pu.b])
                              s.op("act", lambda e, pu=pu, cg=cg: e.activation(out=sqq[:, 2 * cg:2 * cg + 2, :], in_=pu[:, 0:384].rearrange("p (h d) -> p h d", h=2), func=AF.Square),
                                   reads=[pu.b], writes=[sqq.b])
                          s8 = ssq.get()
                          s.op("dve", lambda e, s8=s8: e.tensor_reduce(out=s8[:, 0:8], in_=sqq[:, :, 0:128], axis=AX.X, op=ALU.add), reads=[sqq.b], writes=[s8.b])
                          s.op("dve", lambda e, s8=s8: e.tensor_reduce(out=s8[:, 8:16], in_=sqq[:, :, 128:192], axis=AX.X, op=ALU.add), reads=[sqq.b], writes=[s8.b])
                          s.op("dve", lambda e, s8=s8: e.tensor_scalar(out=s8[:, 0:8], in0=s8[:, 0:8], scalar1=1.0 / 128, scalar2=EPS, op0=ALU.mult, op1=ALU.add), reads=[s8.b], writes=[s8.b])
                          s.op("dve", lambda e, s8=s8: e.tensor_scalar(out=s8[:, 8:16], in0=s8[:, 8:16], scalar1=1.0 / 64, scalar2=EPS, op0=ALU.mult, op1=ALU.add), reads=[s8.b], writes=[s8.b])
                          s.op("act", lambda e, s8=s8: e.activation(out=s8[:], in_=s8[:], func=AF.Sqrt), reads=[s8.b], writes=[s8.b])
                          s.op("dve", lambda e, s8=s8: e.reciprocal(s8[:], s8[:]), reads=[s8.b], writes=[s8.b])
                          qno = qno16.get()
                          for cg in range(4):
                              pu = pqs[cg]
                              puv = pu[:, 0:384].rearrange("p (h d) -> p h d", h=2)
                              s.op("dve", lambda e, puv=puv, qno=qno, s8=s8, cg=cg: e.tensor_tensor(out=qno[:, 2 * cg:2 * cg + 2, :], in0=puv[:, :, 0:128],
                                                                                                    in1=s8[:, 2 * cg:2 * cg + 2].unsqueeze(2).to_broadcast([128, 2, 128]), op=ALU.mult),
                                   reads=[pu.b, s8.b], writes=[qno.b])
                              s.op("dve", lambda e, puv=puv, s8=s8, cg=cg: e.tensor_tensor(out=qrA[:, 2 * cg:2 * cg + 2, :], in0=puv[:, :, 128:192],
                                                                                           in1=s8[:, 8 + 2 * cg:10 + 2 * cg].unsqueeze(2).to_broadcast([128, 2, 64]), op=ALU.mult),
                                   reads=[pu.b, s8.b], writes=[qrA.b])
                          s.op("pool", lambda e: e.tensor_tensor(out=qrA[:], in0=qrA[:], in1=gqr[:].unsqueeze(1).to_broadcast([128, 8, 64]), op=ALU.mult),
                               reads=[qrA.b, gqr.b], writes=[qrA.b])
                          csb = cosT[:, tile_i, :].unsqueeze(1).to_broadcast([128, 8, 32])
                          snb = sinT[:, tile_i, :].unsqueeze(1).to_broadcast([128, 8, 32])
                          s.op("pool", lambda e, csb=csb: e.tensor_tensor(out=qrB[:, :, 0:32], in0=qrA[:, :, 0:32], in1=csb, op=ALU.mult), reads=[qrA.b, cosT.b], writes=[qrB.b])
                          s.op("pool", lambda e, csb=csb: e.tensor_tensor(out=qrB[:, :, 32:64], in0=qrA[:, :, 32:64], in1=csb, op=ALU.mult), reads=[qrA.b, cosT.b], writes=[qrB.b])
                          s.op("pool", lambda e, snb=snb: e.tensor_tensor(out=qrC[:, :, 0:32], in0=qrA[:, :, 32:64], in1=snb, op=ALU.mult), reads=[qrA.b, sinT.b], writes=[qrC.b])
                          s.op("pool", lambda e, snb=snb: e.tensor_tensor(out=qrC[:, :, 32:64], in0=qrA[:, :, 0:32], in1=snb, op=ALU.mult), reads=[qrA.b, sinT.b], writes=[qrC.b])
                          q16 = qr16.get()
                          s.op("pool", lambda e, q16=q16: e.tensor_tensor(out=q16[:, :, 0:32], in0=qrB[:, :, 0:32], in1=qrC[:, :, 0:32], op=ALU.subtract), reads=[qrB.b, qrC.b], writes=[q16.b])
                          s.op("pool", lambda e, q16=q16: e.tensor_tensor(out=q16[:, :, 32:64], in0=qrB[:, :, 32:64], in1=qrC[:, :, 32:64], op=ALU.add), reads=[qrB.b, qrC.b], writes=[q16.b])
                          pt3 = ptr.get()
                          for h in range(8):
                              s.op("pe", lambda e, pt3=pt3, qno=qno, h=h: e.transpose(pt3[:, h * 128:(h + 1) * 128], qno[:, h, :], ident[:]),
                                   reads=[qno.b, ident.b], writes=[pt3.b])
                          qnst = qnstR.get()
                          evac(qnst[:], pt3[:].rearrange("p (a b) -> p a b", a=8), [pt3.b], [qnst.b])
                          q0 = (blk - (nblk - nown)) * 1024 + tt * 128
                          s.dma("sp", lambda e, q0=q0, qnst=qnst: DMA(e, out=qTn_d[:, :, q0:q0 + 128].rearrange("h d t -> d h t"), in_=qnst[:]), reads=[qnst.b])
                          pt4 = ptr.get()
                          for hp in range(4):
                              s.op("pe", lambda e, pt4=pt4, q16=q16, hp=hp: e.transpose(pt4[:, hp * 128:(hp + 1) * 128], q16[:, 2 * hp:2 * hp + 2, :].rearrange("p h d -> p (h d)"), ident[:]),
                                   reads=[q16.b, ident.b], writes=[pt4.b])
                          qrst = qrstR.get()
                          evac(qrst[:], pt4[:, 0:512].rearrange("p (a b) -> p a b", a=4), [pt4.b], [qrst.b])
                          s.dma("sp", lambda e, q0=q0, qrst=qrst: DMA(e, out=qTr_d[:, :, q0:q0 + 128].rearrange("h d t -> d h t"), in_=qrst[:]), reads=[qrst.b])
                  c0 = blk * 1024
                  s.dma("sp", lambda e, c0=c0: DMA(e, out=krT_d[:, c0:c0 + 1024], in_=krst[0:64, :]), reads=[krst.b])
              s.barrier()
              if stage == 1:
                  s.dma("sp", lambda e: DMA(e, out=dbg["kT"], in_=kT_d))
                  s.dma("sp", lambda e: DMA(e, out=dbg["krT"], in_=krT_d))
                  s.dma("sp", lambda e: DMA(e, out=dbg["V"], in_=V_d))
                  s.dma("sp", lambda e: DMA(e, out=dbg["qTn"], in_=qTn_d))
                  s.dma("sp", lambda e: DMA(e, out=dbg["qTr"], in_=qTr_d))
                  s.barrier()
        except Cut:
            pass

        if stage >= 2:
          with ExitStack() as p2:
            NKT = NT // 128
            base0 = (NT - NOWN) // 128
            SCALE = 192.0 ** -0.5
            kbias2 = sbt(p2, "kbias2", [128, 128], F32)
            krT2 = sbt(p2, "krT2", [128, NT], BF16)
            kTh = sbt(p2, "kTh", [128, NT], BF16)
            Vh = sbt(p2, "Vh", [128, NKT, 130], BF16)
            s.dma("sp", lambda e: DMA(e, out=kbias2[:], in_=kbias_in), writes=[kbias2.b])
            s.dma("sp", lambda e: DMA(e, out=krT2[0:64, :], in_=krT_d), writes=[krT2.b])
            s.dma("sp", lambda e: DMA(e, out=krT2[64:128, :], in_=krT_d), writes=[krT2.b])
            s.op("dve", lambda e: e.memset(Vh[:, :, 128:130], 1.0), writes=[Vh.b])
            qnR = Ring([sbt(p2, "qn_t%d" % i, [128, 512], BF16) for i in range(2)])
            qrR = Ring([sbt(p2, "qr_t%d" % i, [128, 512], BF16) for i in range(2)])
            pTR = Ring([sbt(p2, "pT%d" % i, [128, 512], BF16) for i in range(3)])
            pSR = Ring([pst(p2, "pS%d" % i, [128, 512], F32) for i in range(2)])
            pO = [pst(p2, "pO%d" % i, [128, 512], F32) for i in range(4)]
            ostR = Ring([sbt(p2, "ost%d" % i, [128, 128], BF16) for i in range(2)])
            rlR = Ring([sbt(p2, "rl%d" % i, [128, 1], F32) for i in range(2)])
            for h in range(8):
                hb = (h % 2) * 64
                s.dma("sp", lambda e, h=h: DMA(e, out=kTh[:], in_=kT_d[h]), writes=[kTh.b])
                for n0 in range(0, NKT, 16):
                    n1 = min(NKT, n0 + 16)
                    s.dma("sp", lambda e, h=h, n0=n0, n1=n1: DMA(e, out=Vh[:, n0:n1, 0:128], in_=V_d[n0 * 128:n1 * 128, h, :].rearrange("(n p) d -> p n d", p=128)),
                          writes=[Vh.b])
                for qg in range(NOWN // 512):
                    qn = qnR.get()
                    qr = qrR.get()
                    s.dma("sp", lambda e, h=h, qg=qg, qn=qn: DMA(e, out=qn[:], in_=qTn_d[h, :, qg * 512:(qg + 1) * 512]), writes=[qn.b])
                    s.dma("sp", lambda e, h=h, qg=qg, qr=qr: DMA(e, out=qr[:], in_=qTr_d[h // 2, :, qg * 512:(qg + 1) * 512]), writes=[qr.b])
                    dbase = base0 + 4 * qg
                    nk = dbase + 4
                    for kt in range(nk):
                        i = kt - dbase
                        c0 = 128 * i if i > 0 else 0
                        ps = pSR.get()
                        s.op("pe", lambda e, ps=ps, kt=kt, qn=qn, c0=c0: e.matmul(ps[:, c0:512], kTh[:, kt * 128:(kt + 1) * 128], qn[:, c0:512], start=True, stop=False),
                             reads=[kTh.b, qn.b], writes=[ps.b])
                        s.op("pe", lambda e, ps=ps, kt=kt, qr=qr, c0=c0, hb=hb: e.matmul(ps[:, c0:512], krT2[hb:hb + 64, kt * 128:(kt + 1) * 128], qr[hb:hb + 64, c0:512], start=False, stop=True),
                             reads=[krT2.b, qr.b], writes=[ps.b])
                        p = pTR.get()
                        s.op("act", lambda e, ps=ps, p=p, kt=kt, c0=c0: e.activation(out=p[:, c0:512], in_=ps[:, c0:512], func=AF.Exp, bias=kbias2[:, kt:kt + 1], scale=SCALE),
                             reads=[ps.b, kbias2.b], writes=[p.b])
                        if i >= 0:
                            s.op("dve", lambda e, p=p, c0=c0: e.memset(p[64:128, c0:c0 + 64], 0.0), writes=[p.b])
                        for qt in range(max(i, 0), 4):
                            s.op("pe", lambda e, p=p, qt=qt, kt=kt, dbase=dbase: e.matmul(pO[qt][:, 0:129], p[:, qt * 128:(qt + 1) * 128], Vh[:, kt, 0:129],
                                                                                           start=(kt == 0), stop=(kt == dbase + qt)),
                                 reads=[p.b, Vh.b], writes=[pO[qt].b])
                    for qt in range(4):
                        rl = rlR.get()
                        s.op("dve", lambda e, rl=rl, qt=qt: e.reciprocal(rl[:], pO[qt][:, 128:129]), reads=[pO[qt].b], writes=[rl.b])
                        o = ostR.get()
                        s.op("dve", lambda e, rl=rl, qt=qt, o=o: e.tensor_scalar(out=o[:], in0=pO[qt][:, 0:128], scalar1=rl[:, 0:1], scalar2=None, op0=ALU.mult),
                             reads=[pO[qt].b, rl.b], writes=[o.b])
                        t0_ = qg * 512 + qt * 128
                        s.dma("sp", lambda e, o=o, t0_=t0_, h=h: DMA(e, out=ymla_d[t0_:t0_ + 128, h * 128:(h + 1) * 128], in_=o[:]), reads=[o.b])
            s.barrier()
            if stage == 2:
                s.dma("sp", lambda e: DMA(e, out=dbg["ymla"], in_=ymla_d))
                s.barrier()

        if stage >= 3:
          with ExitStack() as p3:
            wU = sbt(p3, "wU", [128, 16, 1024], BF16)
            wG = sbt(p3, "wG", [128, 16, 1024], BF16)
            biasU = sbt(p3, "biasU", [1, 1024], BF16)
            biasG = sbt(p3, "biasG", [1, 1024], BF16)
            validS = sbt(p3, "validS", [128, 16], F32)
            s.dma("sp", lambda e: DMA(e, out=validS[:], in_=valid_in), writes=[validS.b])
            s.dma("pool", lambda e: DMA(e, out=wU[:], in_=w_in[:, 0:1024].rearrange("(k p) n -> p k n", p=128)), writes=[wU.b])
            s.dma("pool", lambda e: DMA(e, out=wG[:], in_=w_in[:, 1024:2048].rearrange("(k p) n -> p k n", p=128)), writes=[wG.b])
            pb3 = pst(p3, "pb3", [1, 512], F32)
            for (w_, b_) in ((wU, biasU), (wG, biasG)):
                for cg in range(2):
                    for k in range(16):
                        s.op("pe", lambda e, k=k, w_=w_, cg=cg: e.matmul(pb3[:, :], sh1c16[:, k:k + 1], w_[:, k, cg * 512:(cg + 1) * 512], start=(k == 0), stop=(k == 15)),
                             reads=[sh1c16.b, w_.b], writes=[pb3.b])
                    s.op("dve", lambda e, b_=b_, cg=cg: e.tensor_copy(b_[:, cg * 512:(cg + 1) * 512], pb3[:, :]), reads=[pb3.b], writes=[b_.b])
                for k in range(16):
                    s.op("dve", lambda e, k=k, w_=w_: e.tensor_scalar(out=w_[:, k, :], in0=w_[:, k, :], scalar1=a1c[:, k:k + 1], scalar2=None, op0=ALU.mult),
                         reads=[w_.b, a1c.b], writes=[w_.b])
            xpool_3 = Ring([sbt(p3, "xt3%d" % i, [128, D], F32) for i in range(2)])
            junk_3 = sbt(p3, "junk3", [128, D], BF16)
            xh_3 = Ring([sbt(p3, "xh3%d" % i, [128, D], BF16) for i in range(2)])
            xhT_3 = sbt(p3, "xhT3", [128, 16, 1024], BF16)
            ptr_3 = Ring([pst(p3, "ptr3%d" % i, [128, 1024], BF16) for i in range(2)])
            pacc_3 = Ring([pst(p3, "pa3%d" % i, [128, 512], F32) for i in range(4)])
            ssr_3 = Ring([sbt(p3, "ss3%d" % i, [128, 4], F32) for i in range(4)])
            Utok = sbt(p3, "Utok", [128, 64, 8, 16], BF16)
            Gblk = sbt(p3, "Gblk", [128, 8, 1024], BF16)
            UTs = Ring([sbt(p3, "UTs%d" % i, [128, 8, 128], BF16) for i in range(2)])
            ev3 = [0]

            def evac3(out_ap, in_ap, reads, writes):
                ev3[0] += 1
                if ev3[0] % 2:
                    s.op("dve", lambda e: e.tensor_copy(out_ap, in_ap), reads=reads, writes=writes)
                else:
                    s.op("act", lambda e: e.activation(out=out_ap, in_=in_ap, func=AF.Copy), reads=reads, writes=writes)

            for blk in range(nblk):
                for tt in range(8):
                    r0 = blk * 1024 + tt * 128
                    xt = xpool_3.get()
                    s.dma("sp", lambda e, xt=xt, r0=r0: DMA(e, out=xt[:], in_=xp[r0:r0 + 128, :]), writes=[xt.b])
                    ss = ssr_3.get()
                    s.op("act", lambda e, xt=xt, ss=ss: e.activation(out=junk_3[:], in_=xt[:], func=AF.Square, accum_out=ss[:, 0:1]), reads=[xt.b], writes=[junk_3.b, ss.b])
                    s.op("dve", lambda e, ss=ss: e.tensor_scalar(out=ss[:, 0:1], in0=ss[:, 0:1], scalar1=1.0 / D, scalar2=EPS, op0=ALU.mult, op1=ALU.add), reads=[ss.b], writes=[ss.b])
                    s.op("act", lambda e, ss=ss: e.activation(out=ss[:, 0:1], in_=ss[:, 0:1], func=AF.Sqrt), reads=[ss.b], writes=[ss.b])
                    s.op("dve", lambda e, ss=ss: e.reciprocal(ss[:, 0:1], ss[:, 0:1]), reads=[ss.b], writes=[ss.b])
                    xb = xh_3.get()
                    s.op("dve", lambda e, xt=xt, ss=ss, xb=xb: e.tensor_scalar(out=xb[:], in0=xt[:], scalar1=ss[:, 0:1], scalar2=None, op0=ALU.mult), reads=[xt.b, ss.b], writes=[xb.b])
                    for half in range(2):
                        pt = ptr_3.get()
                        for k8 in range(8):
                            k = half * 8 + k8
                            s.op("pe", lambda e, pt=pt, xb=xb, k8=k8, k=k: e.transpose(pt[:, k8 * 128:(k8 + 1) * 128], xb[:, k * 128:(k + 1) * 128], ident[:]),
                                 reads=[xb.b, ident.b], writes=[pt.b])
                        evac3(xhT_3[:, half * 8:(half + 1) * 8, tt * 128:(tt + 1) * 128], pt[:].rearrange("p (a b) -> p a b", a=8), [pt.b], [xhT_3.b])
                own = blk >= nblk - nown
                for tau in range(8):
                    for cg in range(2):
                        pa = pacc_3.get()
                        for k in range(16):
                            s.op("pe", lambda e, pa=pa, k=k, tau=tau, cg=cg: e.matmul(pa[:], xhT_3[:, k, tau::8], wU[:, k, cg * 512:(cg + 1) * 512], start=(k == 0), stop=False),
                                 reads=[xhT_3.b, wU.b], writes=[pa.b])
                        s.op("pe", lambda e, pa=pa, cg=cg: e.matmul(pa[:], ones_r[:], biasU[:, cg * 512:(cg + 1) * 512], start=False, stop=True), reads=[ones_r.b, biasU.b], writes=[pa.b])
                        s.op("dve", lambda e, pa=pa, tau=tau, cg=cg, blk=blk: e.tensor_scalar(out=Utok[:, cg * 32:(cg + 1) * 32, tau, :], in0=pa[:].rearrange("p (g i) -> p g i", i=16),
                                                                                         scalar1=validS[:, blk:blk + 1], scalar2=None, op0=ALU.mult),
                             reads=[pa.b, validS.b], writes=[Utok.b])
                        if own:
                            pg = pacc_3.get()
                            for k in range(16):
                                s.op("pe", lambda e, pg=pg, k=k, tau=tau, cg=cg: e.matmul(pg[:], xhT_3[:, k, tau::8], wG[:, k, cg * 512:(cg + 1) * 512], start=(k == 0), stop=False),
                                     reads=[xhT_3.b, wG.b], writes=[pg.b])
                            s.op("pe", lambda e, pg=pg, cg=cg: e.matmul(pg[:], ones_r[:], biasG[:, cg * 512:(cg + 1) * 512], start=False, stop=True), reads=[ones_r.b, biasG.b], writes=[pg.b])
                            s.op("act", lambda e, pg=pg, tau=tau, cg=cg: e.activation(out=Gblk[:, tau, cg * 512:(cg + 1) * 512], in_=pg[:], func=AF.Sigmoid), reads=[pg.b], writes=[Gblk.b])
                if own:
                    ob = blk - (nblk - nown)
                    s.dma("sp", lambda e, ob=ob: DMA(e, out=Gd[ob], in_=Gblk[:]), reads=[Gblk.b])
                for g8 in range(8):
                    pt = ptr_3.get()
                    for gg in range(8):
                        g = g8 * 8 + gg
                        s.op("pe", lambda e, pt=pt, gg=gg, g=g: e.transpose(pt[:, gg * 128:(gg + 1) * 128], Utok[:, g, :, :].rearrange("p t i -> p (t i)"), ident[:]),
                             reads=[Utok.b, ident.b], writes=[pt.b])
                    ut = UTs.get()
                    evac3(ut[:], pt[:].rearrange("p (a b) -> p a b", a=8), [pt.b], [ut.b])
                    s.dma("sp", lambda e, ut=ut, g8=g8, blk=blk: DMA(e, out=Ud[g8 * 8:(g8 + 1) * 8, :, blk * 128:(blk + 1) * 128].rearrange("g s c -> s g c"), in_=ut[:]), reads=[ut.b])
            s.barrier()

        if stage >= 3:
          with ExitStack() as p4:
            G = 64
            NOB = NOWN // 1024
            LV = max(1, int(math.ceil(math.log2(NCH))))

            def T4(name, shape, dt=F32):
                return sbt(p4, name, shape, dt)

            def V(fn, reads, writes):
                s.op("dve", fn, reads=[r.b for r in reads], writes=[w.b for w in writes])

            Ar = T4("Ar", [128, G]); Ai = T4("Ai", [128, G]); dtt = T4("dtt", [128, G])
            for half in range(2):
                s.dma("sp", lambda e, half=half: DMA(e, out=Ar[half * 64:(half + 1) * 64, :], in_=ssm_A_re.rearrange("g p -> p g")), writes=[Ar.b])
                s.dma("sp", lambda e, half=half: DMA(e, out=Ai[half * 64:(half + 1) * 64, :], in_=ssm_A_im.rearrange("g p -> p g")), writes=[Ai.b])
            s.dma("sp", lambda e: DMA(e, out=dtt[:], in_=ssm_log_dt.partition_broadcast(128)), writes=[dtt.b])
            s.op("act", lambda e: e.activation(out=dtt[:], in_=dtt[:], func=AF.Exp), reads=[dtt.b], writes=[dtt.b])
            dAr = T4("dAr", [128, G]); dAi = T4("dAi", [128, G])
            V(lambda e: e.tensor_tensor(out=dAr[:], in0=Ar[:], in1=dtt[:], op=ALU.mult), [Ar, dtt], [dAr])
            V(lambda e: e.tensor_tensor(out=dAi[:], in0=Ai[:], in1=dtt[:], op=ALU.mult), [Ai, dtt], [dAi])
            pidx = T4("pidx", [128, 1], I32); pf = T4("pf", [128, 1])
            m1 = T4("m1", [128, 1]); m2 = T4("m2", [128, 1]); nm1 = T4("nm1", [128, 1]); nm2 = T4("nm2", [128, 1]); sg = T4("sg", [128, 1])
            s.op("pool", lambda e: e.iota(pidx[:], [[0, 1]], base=0, channel_multiplier=1), writes=[pidx.b])
            V(lambda e: e.tensor_copy(pf[:], pidx[:]), [pidx], [pf])
            V(lambda e: e.tensor_scalar(out=m1[:], in0=pf[:], scalar1=64.0, scalar2=None, op0=ALU.is_lt), [pf], [m1])
            V(lambda e: e.tensor_scalar(out=m2[:], in0=m1[:], scalar1=-1.0, scalar2=1.0, op0=ALU.mult, op1=ALU.add), [m1], [m2])
            V(lambda e: e.tensor_scalar(out=nm1[:], in0=m1[:], scalar1=-1.0, scalar2=None, op0=ALU.mult), [m1], [nm1])
            V(lambda e: e.tensor_scalar(out=nm2[:], in0=m2[:], scalar1=-1.0, scalar2=None, op0=ALU.mult), [m2], [nm2])
            V(lambda e: e.tensor_scalar(out=sg[:], in0=m2[:], scalar1=2.0, scalar2=-1.0, op0=ALU.mult, op1=ALU.add), [m2], [sg])
            LRE = T4("LRE", [128, 16, G]); LIM = T4("LIM", [128, 16, G])
            ptmp = ExitStack()
            ANG = sbt(ptmp, "ANG", [128, 16, G], F32); KF = sbt(ptmp, "KF", [128, 16, G], F32); KI = sbt(ptmp, "KI", [128, 16, G], I32)
            MAG = sbt(ptmp, "MAG", [128, 16, G], F32)
            for idx in range(16):
                k = float(idx - 7)
                V(lambda e, idx=idx, k=k: e.tensor_scalar(out=ANG[:, idx, :], in0=dAi[:], scalar1=k, scalar2=None, op0=ALU.mult), [dAi], [ANG])
                s.op("act", lambda e, idx=idx, k=k: e.activation(out=MAG[:, idx, :], in_=dAr[:], func=AF.Exp, scale=k), reads=[dAr.b], writes=[MAG.b])

            def wrap2(t, kf):
                V(lambda e: e.tensor_scalar(out=kf[:], in0=t[:], scalar1=math.pi, scalar2=-TWO_PI, op0=ALU.is_gt, op1=ALU.mult), [t], [kf])
                V(lambda e: e.tensor_tensor(out=t[:], in0=t[:], in1=kf[:], op=ALU.add), [t, kf], [t])
                V(lambda e: e.tensor_scalar(out=kf[:], in0=t[:], scalar1=-math.pi, scalar2=TWO_PI, op0=ALU.is_lt, op1=ALU.mult), [t], [kf])
                V(lambda e: e.tensor_tensor(out=t[:], in0=t[:], in1=kf[:], op=ALU.add), [t, kf], [t])

            V(lambda e: e.tensor_scalar(out=KF[:], in0=ANG[:], scalar1=1.0 / TWO_PI, scalar2=None, op0=ALU.mult), [ANG], [KF])
            V(lambda e: e.tensor_copy(KI[:], KF[:]), [KF], [KI])
            V(lambda e: e.tensor_copy(KF[:], KI[:]), [KI], [KF])
            V(lambda e: e.scalar_tensor_tensor(out=ANG[:], in0=KF[:], scalar=-CW1, in1=ANG[:], op0=ALU.mult, op1=ALU.add), [KF, ANG], [ANG])
            V(lambda e: e.scalar_tensor_tensor(out=ANG[:], in0=KF[:], scalar=-CW2, in1=ANG[:], op0=ALU.mult, op1=ALU.add), [KF, ANG], [ANG])
            wrap2(ANG, KF)
            s.op("act", lambda e: e.activation(out=LIM[:], in_=ANG[:], func=AF.Sin), reads=[ANG.b], writes=[LIM.b])
            V(lambda e: e.tensor_scalar(out=ANG[:], in0=ANG[:], scalar1=math.pi / 2, scalar2=None, op0=ALU.add), [ANG], [ANG])
            wrap2(ANG, KF)
            s.op("act", lambda e: e.activation(out=LRE[:], in_=ANG[:], func=AF.Sin), reads=[ANG.b], writes=[LRE.b])
            V(lambda e: e.tensor_tensor(out=LRE[:], in0=LRE[:], in1=MAG[:], op=ALU.mult), [LRE, MAG], [LRE])
            V(lambda e: e.tensor_tensor(out=LIM[:], in0=LIM[:], in1=MAG[:], op=ALU.mult), [LIM, MAG], [LIM])
            s.barrier()
            ptmp.close()
            aK = T4("aK", [128, LV, G]); bK = T4("bK", [128, LV, G]); bKs = T4("bKs", [128, LV, G]); tq1 = T4("tq1", [128, G]); tq2 = T4("tq2", [128, G])
            V(lambda e: e.tensor_copy(aK[:, 0, :], LRE[:, 15, :]), [LRE], [aK])
            V(lambda e: e.tensor_copy(bK[:, 0, :], LIM[:, 15, :]), [LIM], [bK])
            for l in range(1, LV):
                V(lambda e, l=l: e.tensor_tensor(out=tq1[:], in0=aK[:, l - 1, :], in1=aK[:, l - 1, :], op=ALU.mult), [aK], [tq1])
                V(lambda e, l=l: e.tensor_tensor(out=tq2[:], in0=bK[:, l - 1, :], in1=bK[:, l - 1, :], op=ALU.mult), [bK], [tq2])
                V(lambda e, l=l: e.tensor_tensor(out=aK[:, l, :], in0=tq1[:], in1=tq2[:], op=ALU.subtract), [tq1, tq2], [aK])
                V(lambda e, l=l: e.scalar_tensor_tensor(out=bK[:, l, :], in0=aK[:, l - 1, :], scalar=2.0, in1=bK[:, l - 1, :], op0=ALU.mult, op1=ALU.mult), [aK, bK], [bK])
            V(lambda e: e.tensor_scalar(out=bKs[:], in0=bK[:], scalar1=sg[:, 0:1], scalar2=None, op0=ALU.mult), [bK, sg], [bKs])
            kre = T4("kre", [128, G]); kim = T4("kim", [128, G]); den = T4("den", [128, G]); nr = T4("nr", [128, G])
            V(lambda e: e.tensor_scalar(out=nr[:], in0=LRE[:, 8, :], scalar1=-1.0, scalar2=None, op0=ALU.add), [LRE], [nr])
            V(lambda e: e.tensor_tensor(out=den[:], in0=Ar[:], in1=Ar[:], op=ALU.mult), [Ar], [den])
            V(lambda e: e.tensor_tensor(out=tq1[:], in0=Ai[:], in1=Ai[:], op=ALU.mult), [Ai], [tq1])
            V(lambda e: e.tensor_tensor(out=den[:], in0=den[:], in1=tq1[:], op=ALU.add), [den, tq1], [den])
            V(lambda e: e.reciprocal(den[:], den[:]), [den], [den])
            V(lambda e: e.tensor_tensor(out=kre[:], in0=nr[:], in1=Ar[:], op=ALU.mult), [nr, Ar], [kre])
            V(lambda e: e.tensor_tensor(out=tq1[:], in0=LIM[:, 8, :], in1=Ai[:], op=ALU.mult), [LIM, Ai], [tq1])
            V(lambda e: e.tensor_tensor(out=kre[:], in0=kre[:], in1=tq1[:], op=ALU.add), [kre, tq1], [kre])
            V(lambda e: e.tensor_tensor(out=kre[:], in0=kre[:], in1=den[:], op=ALU.mult), [kre, den], [kre])
            V(lambda e: e.tensor_tensor(out=kim[:], in0=LIM[:, 8, :], in1=Ar[:], op=ALU.mult), [LIM, Ar], [kim])
            V(lambda e: e.tensor_tensor(out=tq1[:], in0=nr[:], in1=Ai[:], op=ALU.mult), [nr, Ai], [tq1])
            V(lambda e: e.tensor_tensor(out=kim[:], in0=kim[:], in1=tq1[:], op=ALU.subtract), [kim, tq1], [kim])
            V(lambda e: e.tensor_tensor(out=kim[:], in0=kim[:], in1=den[:], op=ALU.mult), [kim, den], [kim])
            P_all = T4("P_all", [128, G, 128], BF16); T_all = T4("T_all", [128, G, 128], BF16); Q_all = T4("Q_all", [128, G, 128], BF16)
            Sw = T4("Sw", [128, 128], F32)
            pset = ExitStack()
            Bre = sbt(pset, "Bre", [128, G, 16], F32); Bim = sbt(pset, "Bim", [128, G, 16], F32)
            for half in range(2):
                s.dma("sp", lambda e, half=half: DMA(e, out=Bre[half * 64:(half + 1) * 64, :, :], in_=ssm_B_re.rearrange("g p i -> p g i")), writes=[Bre.b])
                s.dma("sp", lambda e, half=half: DMA(e, out=Bim[half * 64:(half + 1) * 64, :, :], in_=ssm_B_im.rearrange("g p i -> p g i")), writes=[Bim.b])
            Bbr = sbt(pset, "Bbr", [128, G, 16], F32); Bbi = sbt(pset, "Bbi", [128, G, 16], F32); tb = sbt(pset, "tb", [128, G, 16], F32)
            kreb = kre[:, :].unsqueeze(2).to_broadcast([128, G, 16]); kimb = kim[:, :].unsqueeze(2).to_broadcast([128, G, 16])
            V(lambda e: e.tensor_tensor(out=Bbr[:], in0=Bre[:], in1=kreb, op=ALU.mult), [Bre, kre], [Bbr])
            V(lambda e: e.tensor_tensor(out=tb[:], in0=Bim[:], in1=kimb, op=ALU.mult), [Bim, kim], [tb])
            V(lambda e: e.tensor_tensor(out=Bbr[:], in0=Bbr[:], in1=tb[:], op=ALU.subtract), [Bbr, tb], [Bbr])
            V(lambda e: e.tensor_tensor(out=Bbi[:], in0=Bim[:], in1=kreb, op=ALU.mult), [Bim, kre], [Bbi])
            V(lambda e: e.tensor_tensor(out=tb[:], in0=Bre[:], in1=kimb, op=ALU.mult), [Bre, kim], [tb])
            V(lambda e: e.tensor_tensor(out=Bbi[:], in0=Bbi[:], in1=tb[:], op=ALU.add), [Bbi, tb], [Bbi])
            X1 = Bre; X2 = Bim
            V(lambda e: e.tensor_scalar(out=X1[:], in0=Bbr[:], scalar1=m1[:, 0:1], scalar2=None, op0=ALU.mult), [Bbr, m1], [X1])
            V(lambda e: e.scalar_tensor_tensor(out=X1[:], in0=Bbi[:], scalar=m2[:, 0:1], in1=X1[:], op0=ALU.mult, op1=ALU.add), [Bbi, m2, X1], [X1])
            V(lambda e: e.tensor_scalar(out=X2[:], in0=Bbr[:], scalar1=m2[:, 0:1], scalar2=None, op0=ALU.mult), [Bbr, m2], [X2])
            V(lambda e: e.scalar_tensor_tensor(out=X2[:], in0=Bbi[:], scalar=nm1[:, 0:1], in1=X2[:], op0=ALU.mult, op1=ALU.add), [Bbi, nm1, X2], [X2])
            PS = sbt(pset, "PS", [128, G, 8, 16], F32)
            PhS = sbt(pset, "PhS", [128, G, 8, 16], F32)
            for tau in range(8):
                for (dst, idx) in ((PS, 7 - tau + 7), (PhS, -tau + 7)):
                    lre = LRE[:, idx, :].unsqueeze(2).to_broadcast([128, G, 16]); lim = LIM[:, idx, :].unsqueeze(2).to_broadcast([128, G, 16])
                    V(lambda e, dst=dst, tau=tau, lre=lre: e.tensor_tensor(out=dst[:, :, tau, :], in0=X1[:], in1=lre, op=ALU.mult), [X1, LRE], [dst])
                    V(lambda e, lim=lim: e.tensor_tensor(out=tb[:], in0=X2[:], in1=lim, op=ALU.mult), [X2, LIM], [tb])
                    V(lambda e, dst=dst, tau=tau: e.tensor_tensor(out=dst[:, :, tau, :], in0=dst[:, :, tau, :], in1=tb[:], op=ALU.add), [dst, tb], [dst])
            Cre = sbt(pset, "Cre", [128, G, 16], F32); Cim = sbt(pset, "Cim", [128, G, 16], F32)
            ctile = Ring([sbt(pset, "ctile%d" % i, [128, 128], F32) for i in range(2)])
            pS4 = Ring([pst(p4, "pS4%d" % i, [128, 512], F32) for i in range(4)])
            for (csrc, cdst) in ((ssm_C_re, Cre), (ssm_C_im, Cim)):
                cflat = csrc.rearrange("g o p -> (g o) p")
                for r in range(8):
                    ct = ctile.get()
                    s.dma("sp", lambda e, ct=ct, r=r, cflat=cflat: DMA(e, out=ct[:, 0:64], in_=cflat[r * 128:(r + 1) * 128, :]), writes=[ct.b])
                    s.dma("sp", lambda e, ct=ct, r=r, cflat=cflat: DMA(e, out=ct[:, 64:128], in_=cflat[r * 128:(r + 1) * 128, :]), writes=[ct.b])
                    pp = pS4.get()
                    s.op("pe", lambda e, pp=pp, ct=ct: e.transpose(pp[:, 0:128], ct[:], ident_f[:]), reads=[ct.b, ident_f.b], writes=[pp.b])
                    V(lambda e, pp=pp, cdst=cdst, r=r: e.tensor_copy(cdst[:, r * 8:(r + 1) * 8, :].rearrange("p g o -> p (g o)"), pp[:, 0:128]), [pp], [cdst])
            Y1 = Bbr; Y2 = Bbi
            V(lambda e: e.tensor_scalar(out=Y1[:], in0=Cre[:], scalar1=m1[:, 0:1], scalar2=None, op0=ALU.mult), [Cre, m1], [Y1])
            V(lambda e: e.scalar_tensor_tensor(out=Y1[:], in0=Cim[:], scalar=nm2[:, 0:1], in1=Y1[:], op0=ALU.mult, op1=ALU.add), [Cim, nm2, Y1], [Y1])
            V(lambda e: e.tensor_scalar(out=Y2[:], in0=Cim[:], scalar1=nm1[:, 0:1], scalar2=None, op0=ALU.mult), [Cim, nm1], [Y2])
            V(lambda e: e.scalar_tensor_tensor(out=Y2[:], in0=Cre[:], scalar=nm2[:, 0:1], in1=Y2[:], op0=ALU.mult, op1=ALU.add), [Cre, nm2, Y2], [Y2])
            QhS = sbt(pset, "QhS", [128, G, 9, 16], F32)
            for tp in range(9):
                idx = tp + 7
                lre = LRE[:, idx, :].unsqueeze(2).to_broadcast([128, G, 16]); lim = LIM[:, idx, :].unsqueeze(2).to_broadcast([128, G, 16])
                V(lambda e, tp=tp, lre=lre: e.tensor_tensor(out=QhS[:, :, tp, :], in0=Y1[:], in1=lre, op=ALU.mult), [Y1, LRE], [QhS])
                V(lambda e, lim=lim: e.tensor_tensor(out=tb[:], in0=Y2[:], in1=lim, op=ALU.mult), [Y2, LIM], [tb])
                V(lambda e, tp=tp: e.tensor_tensor(out=QhS[:, :, tp, :], in0=QhS[:, :, tp, :], in1=tb[:], op=ALU.add), [QhS, tb], [QhS])
            V(lambda e: e.tensor_copy(Q_all[:].rearrange("p g (t o) -> p g t o", o=16), QhS[:, :, 1:9, :]), [QhS], [Q_all])
            mki = sbt(pset, "mki", [128, 8, 16], I32); Mk = sbt(pset, "Mk", [128, 128], F32); Dd = sbt(pset, "Dd", [128, G], F32); tT = sbt(pset, "tT", [128, 128], F32)
            s.op("pool", lambda e: e.iota(mki[:], [[16, 8], [0, 16]], base=0, channel_multiplier=-1), writes=[mki.b])
            V(lambda e: e.tensor_copy(Mk[:].rearrange("p (t o) -> p t o", o=16), mki[:]), [mki], [Mk])
            V(lambda e: e.tensor_scalar(out=Mk[:], in0=Mk[:], scalar1=-15.0, scalar2=None, op0=ALU.is_ge), [Mk], [Mk])
            for tau in range(8):
                s.dma("sp", lambda e, tau=tau: DMA(e, out=Dd[tau * 16:(tau + 1) * 16, :], in_=ssm_D.rearrange("(g i) -> i g", i=16)), writes=[Dd.b])
            V(lambda e: e.tensor_copy(Sw[:, 0:64], ident_f[:, 64:128]), [ident_f], [Sw])
            V(lambda e: e.tensor_copy(Sw[:, 64:128], ident_f[:, 0:64]), [ident_f], [Sw])
            for g in range(G):
                pp = pS4.get()
                s.op("pe", lambda e, pp=pp, g=g: e.transpose(pp[:, 0:128], PS[:, g, :, :].rearrange("p t i -> p (t i)"), ident_f[:]), reads=[PS.b, ident_f.b], writes=[pp.b])
                V(lambda e, pp=pp, g=g: e.tensor_copy(P_all[:, g, :], pp[:, 0:128]), [pp], [P_all])
                pq = pS4.get()
                s.op("pe", lambda e, pq=pq, g=g: e.matmul(pq[:, 0:128], PhS[:, g, :, :].rearrange("p t i -> p (t i)"), QhS[:, g, 0:8, :].rearrange("p t o -> p (t o)"), start=True, stop=True),
                     reads=[PhS.b, QhS.b], writes=[pq.b])
                V(lambda e, pq=pq: e.tensor_tensor(out=tT[:], in0=pq[:, 0:128], in1=Mk[:], op=ALU.mult), [pq, Mk], [tT])
                V(lambda e, g=g: e.scalar_tensor_tensor(out=T_all[:, g, :], in0=ident_f[:], scalar=Dd[:, g:g + 1], in1=tT[:], op0=ALU.mult, op1=ALU.add), [ident_f, Dd, tT], [T_all])
            s.barrier()
            pset.close()
            Ug = Ring([T4("Ug%d" % i, [128, NCH], BF16) for i in range(2)])
            XA = T4("XA", [128, NCH], F32); XB = T4("XB", [128, NCH], F32)
            Xb = T4("Xb", [128, NCH + 2], BF16)
            Ybuf = T4("Ybuf", [128, NOB, 8, 1024], BF16)
            V(lambda e: e.memset(Xb[:, 0:2], 0.0), [], [Xb])
            cown = NCH - NOB * 128
            for g in range(G):
                u = Ug.get()
                s.dma("sp", lambda e, u=u, g=g: DMA(e, out=u[:], in_=Ud[g]), writes=[u.b])
                for n0 in range(0, NCH, 512):
                    n1 = min(NCH, n0 + 512)
                    pp = pS4.get()
                    s.op("pe", lambda e, pp=pp, u=u, g=g, n0=n0, n1=n1: e.matmul(pp[:, 0:n1 - n0], P_all[:, g, :], u[:, n0:n1], start=True, stop=True),
                         reads=[P_all.b, u.b], writes=[pp.b])
                    V(lambda e, pp=pp, n0=n0, n1=n1: e.tensor_copy(XA[:, n0:n1], pp[:, 0:n1 - n0]), [pp], [XA])
                cur, nxt = XA, XB
                for l in range(LV):
                    sh = 1 << l
                    if sh >= NCH:
                        break
                    V(lambda e, cur=cur, nxt=nxt, sh=sh: e.tensor_copy(nxt[:, 0:sh], cur[:, 0:sh]), [cur], [nxt])
                    for n0 in range(0, NCH - sh, 512):
                        n1 = min(NCH - sh, n0 + 512)
                        pp = pS4.get()
                        s.op("pe", lambda e, pp=pp, cur=cur, n0=n0, n1=n1: e.matmul(pp[:, 0:n1 - n0], Sw[:], cur[:, n0:n1], start=True, stop=True),
                             reads=[Sw.b, cur.b], writes=[pp.b])
                        V(lambda e, pp=pp, cur=cur, nxt=nxt, n0=n0, n1=n1, sh=sh, l=l, g=g: e.scalar_tensor_tensor(out=nxt[:, n0 + sh:n1 + sh], in0=pp[:, 0:n1 - n0], scalar=bKs[:, l, g:g + 1],
                                                                                                          in1=cur[:, n0 + sh:n1 + sh], op0=ALU.mult, op1=ALU.add),
                          [pp, bKs, cur], [nxt])
                        V(lambda e, cur=cur, nxt=nxt, n0=n0, n1=n1, sh=sh, l=l, g=g: e.scalar_tensor_tensor(out=nxt[:, n0 + sh:n1 + sh], in0=cur[:, n0:n1], scalar=aK[:, l, g:g + 1],
                                                                                                   in1=nxt[:, n0 + sh:n1 + sh], op0=ALU.mult, op1=ALU.add),
                          [cur, aK, nxt], [nxt])
                    cur, nxt = nxt, cur
                V(lambda e, cur=cur: e.tensor_copy(Xb[:, 1:NCH + 1], cur[:, :]), [cur], [Xb])
                for ob in range(NOB):
                    c0 = cown + ob * 128
                    pp = pS4.get()
                    s.op("pe", lambda e, pp=pp, u=u, g=g, c0=c0: e.matmul(pp[:, 0:128], u[:, c0:c0 + 128], T_all[:, g, :], start=True, stop=False), reads=[u.b, T_all.b], writes=[pp.b])
                    s.op("pe", lambda e, pp=pp, g=g, c0=c0: e.matmul(pp[:, 0:128], Xb[:, c0:c0 + 128], Q_all[:, g, :], start=False, stop=True), reads=[Xb.b, Q_all.b], writes=[pp.b])
                    s.op("act", lambda e, pp=pp, g=g, ob=ob: e.activation(out=Ybuf[:, ob, :, g * 16:(g + 1) * 16], in_=pp[:, 0:128].rearrange("p (t o) -> p t o", o=16), func=AF.Gelu),
                         reads=[pp.b], writes=[Ybuf.b])
            Gt = T4("Gt", [128, 8, 1024], BF16)
            for ob in range(NOB):
                s.dma("sp", lambda e, ob=ob: DMA(e, out=Gt[:], in_=Gd[ob]), writes=[Gt.b])
                V(lambda e, ob=ob: e.tensor_tensor(out=Ybuf[:, ob, :, :], in0=Ybuf[:, ob, :, :], in1=Gt[:], op=ALU.mult), [Ybuf, Gt], [Ybuf])
                s.dma("sp", lambda e, ob=ob: DMA(e, out=ys5_d[ob * 1024:(ob + 1) * 1024, :].rearrange("(c t) ch -> c t ch", t=8), in_=Ybuf[:, ob, :, :]), reads=[Ybuf.b])
            s.barrier()
            if stage == 3:
                s.dma("sp", lambda e: DMA(e, out=dbg["ys5"], in_=ys5_d))
                s.barrier()

        if stage >= 4:
          with ExitStack() as p5:
            def T5(name, shape, dt=F32):
                return sbt(p5, name, shape, dt)
            g1row = T5("g1row", [128, D]); sh2row = T5("sh2row", [128, D]); a2row = T5("a2row", [128, D]); g2row = T5("g2row", [128, D])
            rows = [g1row, sh2row, a2row, g2row]
            with ExitStack() as p5a:
                cT5 = sbt(p5a, "cT5", [128, 16], F32)
                cact5 = sbt(p5a, "cact5", [128, 16], BF16)
                cB = sbt(p5a, "cB", [128, 16, 128], BF16)
                gfrow = sbt(p5a, "gfrow", [128, D], F32)
                s.dma("sp", lambda e: DMA(e, out=cT5[:], in_=c_in.rearrange("(k p) -> p k", p=128)), writes=[cT5.b])
                s.op("act", lambda e: e.activation(out=cact5[:], in_=cT5[:], func=AF.Silu), reads=[cT5.b], writes=[cact5.b])
                for k in range(16):
                    s.op("dve", lambda e, k=k: e.tensor_copy(cB[:, k, :], cact5[:, k:k + 1].to_broadcast([128, 128])), reads=[cact5.b], writes=[cB.b])
                wa5 = Ring([sbt(p5a, "wa5%d" % i, [128, 16, 512], BF16) for i in range(2)])
                pr5 = Ring([pst(p5a, "pr5%d" % i, [128, 512], F32) for i in range(2)])
                for ci, row in enumerate(rows):
                    chunk = 2 + ci
                    s.dma("sp", lambda e, row=row, chunk=chunk: DMA(e, out=row[:], in_=b_ada[chunk * D:(chunk + 1) * D].partition_broadcast(128)), writes=[row.b])
                    for j in range(4):
                        w = wa5.get()
                        c0 = chunk * D + j * 512
                        s.dma("pool", lambda e, w=w, c0=c0: DMA(e, out=w[:], in_=w_ada[:, c0:c0 + 512].rearrange("(k p) n -> p k n", p=128)), writes=[w.b])
                        pr = pr5.get()
                        for k in range(16):
                            s.op("pe", lambda e, pr=pr, w=w, k=k: e.matmul(pr[:], cB[:, k, :], w[:, k, :], start=(k == 0), stop=(k == 15)), reads=[cB.b, w.b], writes=[pr.b])
                        s.op("dve", lambda e, pr=pr, row=row, j=j: e.tensor_tensor(out=row[:, j * 512:(j + 1) * 512], in0=pr[:], in1=row[:, j * 512:(j + 1) * 512], op=ALU.add),
                             reads=[pr.b, row.b], writes=[row.b])
                s.dma("sp", lambda e: DMA(e, out=gfrow[:], in_=norm_ffn_g.partition_broadcast(128)), writes=[gfrow.b])
                s.op("dve", lambda e: e.scalar_tensor_tensor(out=a2row[:], in0=a2row[:], scalar=1.0, in1=gfrow[:], op0=ALU.add, op1=ALU.mult), reads=[a2row.b, gfrow.b], writes=[a2row.b])
                s.barrier()
            with ExitStack() as p5b:
                wo = sbt(p5b, "wo", [128, 16, D], BF16)
                gsc = sbt(p5b, "gsc", [128, 16], F32)
                s.dma("pool", lambda e: DMA(e, out=wo[:], in_=w_out.rearrange("(k p) n -> p k n", p=128)), writes=[wo.b])
                s.dma("sp", lambda e: DMA(e, out=gsc[:, 0:8], in_=out_ssm_g.rearrange("(k p) -> p k", p=128)), writes=[gsc.b])
                s.dma("sp", lambda e: DMA(e, out=gsc[:, 8:16], in_=out_mla_g.rearrange("(k p) -> p k", p=128)), writes=[gsc.b])
                for k in range(16):
                    s.op("dve", lambda e, k=k: e.scalar_tensor_tensor(out=wo[:, k, :], in0=wo[:, k, :], scalar=gsc[:, k:k + 1], in1=g1row[:], op0=ALU.mult, op1=ALU.mult),
                         reads=[wo.b, gsc.b, g1row.b], writes=[wo.b])
                ymR = Ring([sbt(p5b, "ym%d" % i, [128, 2, 1024], BF16) for i in range(2)])
                junk5 = sbt(p5b, "junk5", [128, 1024], BF16)
                ss5R = Ring([sbt(p5b, "ss5%d" % i, [128, 2], F32) for i in range(2)])
                mixT = Ring([sbt(p5b, "mixT%d" % i, [128, 16, 128], BF16) for i in range(2)])
                xR = Ring([sbt(p5b, "x5%d" % i, [128, D], F32) for i in range(2)])
                x1R = Ring([sbt(p5b, "x15%d" % i, [128, D], F32) for i in range(2)])
                ptr5 = Ring([pst(p5b, "ptr5%d" % i, [128, 1024], BF16) for i in range(2)])
                pa5 = Ring([pst(p5b, "pa5%d" % i, [128, 512], F32) for i in range(4)])
                for ti in range(NOWN // 128):
                    ym = ymR.get()
                    s.dma("sp", lambda e, ym=ym, ti=ti: DMA(e, out=ym[:, 0, :], in_=ys5_d[ti * 128:(ti + 1) * 128, :]), writes=[ym.b])
                    s.dma("sp", lambda e, ym=ym, ti=ti: DMA(e, out=ym[:, 1, :], in_=ymla_d[ti * 128:(ti + 1) * 128, :]), writes=[ym.b])
                    xt = xR.get()
                    r0 = NT - NOWN + ti * 128
                    s.dma("sp", lambda e, xt=xt, r0=r0: DMA(e, out=xt[:], in_=xp[r0:r0 + 128, :]), writes=[xt.b])
                    ss = ss5R.get()
                    for hh in range(2):
                        s.op("act", lambda e, ym=ym, ss=ss, hh=hh: e.activation(out=junk5[:], in_=ym[:, hh, :], func=AF.Square, accum_out=ss[:, hh:hh + 1]),
                             reads=[ym.b], writes=[junk5.b, ss.b])
                    s.op("dve", lambda e, ss=ss: e.tensor_scalar(out=ss[:], in0=ss[:], scalar1=1.0 / 1024, scalar2=EPS, op0=ALU.mult, op1=ALU.add), reads=[ss.b], writes=[ss.b])
                    s.op("act", lambda e, ss=ss: e.activation(out=ss[:], in_=ss[:], func=AF.Sqrt), reads=[ss.b], writes=[ss.b])
                    s.op("dve", lambda e, ss=ss: e.reciprocal(ss[:], ss[:]), reads=[ss.b], writes=[ss.b])
                    mt = mixT.get()
                    for hh in range(2):
                        pt = ptr5.get()
                        for k8 in range(8):
                            s.op("pe", lambda e, pt=pt, ym=ym, hh=hh, k8=k8: e.transpose(pt[:, k8 * 128:(k8 + 1) * 128], ym[:, hh, k8 * 128:(k8 + 1) * 128], ident[:]),
                                 reads=[ym.b, ident.b], writes=[pt.b])
                        s.op("dve", lambda e, pt=pt, mt=mt, hh=hh: e.tensor_copy(mt[:, hh * 8:(hh + 1) * 8, :], pt[:].rearrange("p (a b) -> p a b", a=8)), reads=[pt.b], writes=[mt.b])
                    x1 = x1R.get()
                    for cg in range(4):
                        pS_ = pa5.get(); pM_ = pa5.get()
                        for k in range(8):
                            s.op("pe", lambda e, pS_=pS_, mt=mt, k=k, cg=cg: e.matmul(pS_[:], mt[:, k, :], wo[:, k, cg * 512:(cg + 1) * 512], start=(k == 0), stop=(k == 7)),
                                 reads=[mt.b, wo.b], writes=[pS_.b])
                        for k in range(8, 16):
                            s.op("pe", lambda e, pM_=pM_, mt=mt, k=k, cg=cg: e.matmul(pM_[:], mt[:, k, :], wo[:, k, cg * 512:(cg + 1) * 512], start=(k == 8), stop=(k == 15)),
                                 reads=[mt.b, wo.b], writes=[pM_.b])
                        s.op("dve", lambda e, pS_=pS_, ss=ss, xt=xt, x1=x1, cg=cg: e.scalar_tensor_tensor(out=x1[:, cg * 512:(cg + 1) * 512], in0=pS_[:], scalar=ss[:, 0:1],
                                                                                                       in1=xt[:, cg * 512:(cg + 1) * 512], op0=ALU.mult, op1=ALU.add),
                             reads=[pS_.b, ss.b, xt.b], writes=[x1.b])
                        s.op("dve", lambda e, pM_=pM_, ss=ss, x1=x1, cg=cg: e.scalar_tensor_tensor(out=x1[:, cg * 512:(cg + 1) * 512], in0=pM_[:], scalar=ss[:, 1:2],
                                                                                               in1=x1[:, cg * 512:(cg + 1) * 512], op0=ALU.mult, op1=ALU.add),
                             reads=[pM_.b, ss.b, x1.b], writes=[x1.b])
                    s.dma("sp", lambda e, x1=x1, ti=ti: DMA(e, out=x1_d[ti * 128:(ti + 1) * 128, :], in_=x1[:]), reads=[x1.b])
                s.barrier()
                if stage == 4:
                    s.dma("sp", lambda e: DMA(e, out=dbg["x1"], in_=x1_d))
                    s.barrier()
            if stage >= 5:
              with ExitStack() as p5c:
                wr = sbt(p5c, "wr", [128, 16, NE], BF16)
                brow = sbt(p5c, "brow", [1, NE], BF16)
                s.dma("pool", lambda e: DMA(e, out=wr[:], in_=w_router.rearrange("(k p) n -> p k n", p=128)), writes=[wr.b])
                s.dma("pool", lambda e: DMA(e, out=brow[:], in_=b_router.rearrange("(o n) -> o n", o=1)), writes=[brow.b])
                x1q = sbt(p5c, "x1q", [128, 4, D], F32)
                accq = sbt(p5c, "accq", [128, 4, D], F32)
                h2T = sbt(p5c, "h2T", [128, 16, 512], BF16)
                actT = sbt(p5c, "actT", [128, 16, 512], BF16)
                tmpf = sbt(p5c, "tmpf", [128, D], F32)
                h2b = sbt(p5c, "h2b", [128, D], BF16)
                junk6 = h2b
                Gq = sbt(p5c, "Gq", [128, 4, NE], F32)
                lg = sbt(p5c, "lg", [128, NE], F32); mx8 = sbt(p5c, "mx8", [128, 8], F32); msk = sbt(p5c, "msk", [128, NE], F32); ssm_ = sbt(p5c, "ssm_", [128, 4], F32)
                wguR = Ring([sbt(p5c, "wgu%d" % i, [128, 16, 256], BF16) for i in range(2)])
                wdT = sbt(p5c, "wdT", [128, 16, 1024], BF16)
                bgu = sbt(p5c, "bgu", [128, 16, 2], F32)
                bdr = sbt(p5c, "bdr", [1, D], BF16)
                gt_ = Ring([sbt(p5c, "gt%d" % i, [128, 512], F32) for i in range(2)])
                sg_ = Ring([sbt(p5c, "sgm%d" % i, [128, 512], F32) for i in range(1)])
                ut_ = Ring([sbt(p5c, "ut%d" % i, [128, 512], F32) for i in range(2)])
                ptr6 = Ring([pst(p5c, "ptr6%d" % i, [128, 1024], BF16) for i in range(2)])
                pgu = Ring([pst(p5c, "pgu%d" % i, [128, 512], F32) for i in range(4)])
                pdn = Ring([pst(p5c, "pdn%d" % i, [128, 512], F32) for i in range(2)])
                for q8 in range(NOWN // 512):
                    s.op("pool", lambda e: e.memset(accq[:], 0.0), writes=[accq.b])
                    for tt in range(4):
                        t0_ = q8 * 512 + tt * 128
                        s.dma("sp", lambda e, tt=tt, t0_=t0_: DMA(e, out=x1q[:, tt, :], in_=x1_d[t0_:t0_ + 128, :]), writes=[x1q.b])
                        s.op("act", lambda e, tt=tt: e.activation(out=junk6[:], in_=x1q[:, tt, :], func=AF.Square, accum_out=ssm_[:, 0:1]), reads=[x1q.b], writes=[junk6.b, ssm_.b])
                        s.op("dve", lambda e: e.tensor_scalar(out=ssm_[:, 0:1], in0=ssm_[:, 0:1], scalar1=1.0 / D, scalar2=EPS, op0=ALU.mult, op1=ALU.add), reads=[ssm_.b], writes=[ssm_.b])
                        s.op("act", lambda e: e.activation(out=ssm_[:, 0:1], in_=ssm_[:, 0:1], func=AF.Sqrt), reads=[ssm_.b], writes=[ssm_.b])
                        s.op("dve", lambda e: e.reciprocal(ssm_[:, 0:1], ssm_[:, 0:1]), reads=[ssm_.b], writes=[ssm_.b])
                        s.op("dve", lambda e, tt=tt: e.scalar_tensor_tensor(out=tmpf[:], in0=x1q[:, tt, :], scalar=ssm_[:, 0:1], in1=a2row[:], op0=ALU.mult, op1=ALU.mult),
                             reads=[x1q.b, ssm_.b, a2row.b], writes=[tmpf.b])
                        s.op("dve", lambda e: e.tensor_tensor(out=h2b[:], in0=tmpf[:], in1=sh2row[:], op=ALU.add), reads=[tmpf.b, sh2row.b], writes=[h2b.b])
                        for half in range(2):
                            pt = ptr6.get()
                            for k8 in range(8):
                                k = half * 8 + k8
                                s.op("pe", lambda e, pt=pt, k8=k8, k=k: e.transpose(pt[:, k8 * 128:(k8 + 1) * 128], h2b[:, k * 128:(k + 1) * 128], ident[:]),
                                     reads=[h2b.b, ident.b], writes=[pt.b])
                            s.op("dve", lambda e, pt=pt, half=half, tt=tt: e.tensor_copy(h2T[:, half * 8:(half + 1) * 8, tt * 128:(tt + 1) * 128], pt[:].rearrange("p (a b) -> p a b", a=8)),
                                 reads=[pt.b], writes=[h2T.b])
                        pl = pdn.get()
                        for k in range(16):
                            s.op("pe", lambda e, pl=pl, k=k, tt=tt: e.matmul(pl[:, 0:NE], h2T[:, k, tt * 128:(tt + 1) * 128], wr[:, k, :], start=(k == 0), stop=False), reads=[h2T.b, wr.b], writes=[pl.b])
                        s.op("pe", lambda e, pl=pl: e.matmul(pl[:, 0:NE], ones_r[:], brow[:], start=False, stop=True), reads=[ones_r.b, brow.b], writes=[pl.b])
                        s.op("dve", lambda e, pl=pl: e.tensor_copy(lg[:], pl[:, 0:NE]), reads=[pl.b], writes=[lg.b])
                        s.op("dve", lambda e: e.max(mx8[:], lg[:]), reads=[lg.b], writes=[mx8.b])
                        s.op("dve", lambda e: e.tensor_scalar(out=msk[:], in0=lg[:], scalar1=mx8[:, 3:4], scalar2=None, op0=ALU.is_ge), reads=[lg.b, mx8.b], writes=[msk.b])
                        s.op("dve", lambda e: e.tensor_scalar(out=lg[:], in0=lg[:], scalar1=mx8[:, 0:1], scalar2=None, op0=ALU.subtract), reads=[lg.b, mx8.b], writes=[lg.b])
                        s.op("act", lambda e: e.activation(out=lg[:], in_=lg[:], func=AF.Exp), reads=[lg.b], writes=[lg.b])
                        s.op("dve", lambda e: e.tensor_tensor(out=lg[:], in0=lg[:], in1=msk[:], op=ALU.mult), reads=[lg.b, msk.b], writes=[lg.b])
                        s.op("dve", lambda e: e.tensor_reduce(out=ssm_[:, 1:2], in_=lg[:], axis=AX.X, op=ALU.add), reads=[lg.b], writes=[ssm_.b])
                        s.op("dve", lambda e: e.reciprocal(ssm_[:, 1:2], ssm_[:, 1:2]), reads=[ssm_.b], writes=[ssm_.b])
                        s.op("dve", lambda e, tt=tt: e.tensor_scalar(out=Gq[:, tt, :], in0=lg[:], scalar1=ssm_[:, 1:2], scalar2=None, op0=ALU.mult), reads=[lg.b, ssm_.b], writes=[Gq.b])
                    for ex in range(NE):
                        s.dma("sp", lambda e, ex=ex: DMA(e, out=bgu[:], in_=b_gate_up[ex].rearrange("(ft p two) -> p ft two", p=128, two=2)), writes=[bgu.b])
                        s.dma("pool", lambda e, ex=ex: DMA(e, out=bdr[:], in_=b_down[ex].rearrange("(o n) -> o n", o=1)), writes=[bdr.b])
                        for ft in range(16):
                            w = wguR.get()
                            s.dma("pool", lambda e, ex=ex, ft=ft, w=w: DMA(e, out=w[:], in_=w_gate_up[ex, :, ft * 256:(ft + 1) * 256].rearrange("(k p) n -> p k n", p=128)), writes=[w.b])
                            pg = pgu.get(); pu = pgu.get()
                            for k in range(16):
                                s.op("pe", lambda e, pg=pg, w=w, k=k: e.matmul(pg[:], w[:, k, 0::2], h2T[:, k, :], start=(k == 0), stop=(k == 15)), reads=[w.b, h2T.b], writes=[pg.b])
                            for k in range(16):
                                s.op("pe", lambda e, pu=pu, w=w, k=k: e.matmul(pu[:], w[:, k, 1::2], h2T[:, k, :], start=(k == 0), stop=(k == 15)), reads=[w.b, h2T.b], writes=[pu.b])
                            g_ = gt_.get(); sgm = sg_.get(); u_ = ut_.get()
                            s.op("dve", lambda e, pg=pg, g_=g_, ft=ft: e.tensor_scalar(out=g_[:], in0=pg[:], scalar1=bgu[:, ft, 0:1], scalar2=7.0, op0=ALU.add, op1=ALU.min), reads=[pg.b, bgu.b], writes=[g_.b])
                            s.op("act", lambda e, g_=g_, sgm=sgm: e.activation(out=sgm[:], in_=g_[:], func=AF.Sigmoid, scale=1.702), reads=[g_.b], writes=[sgm.b])
                            s.op("dve", lambda e, pu=pu, u_=u_, ft=ft: e.tensor_scalar(out=u_[:], in0=pu[:], scalar1=bgu[:, ft, 1:2], scalar2=7.0, op0=ALU.add, op1=ALU.min), reads=[pu.b, bgu.b], writes=[u_.b])
                            s.op("pool", lambda e, u_=u_: e.tensor_scalar(out=u_[:], in0=u_[:], scalar1=-7.0, scalar2=1.0, op0=ALU.max, op1=ALU.add), reads=[u_.b], writes=[u_.b])
                            s.op("pool", lambda e, g_=g_, sgm=sgm: e.tensor_tensor(out=g_[:], in0=g_[:], in1=sgm[:], op=ALU.mult), reads=[g_.b, sgm.b], writes=[g_.b])
                            s.op("pool", lambda e, g_=g_, u_=u_, ft=ft: e.tensor_tensor(out=actT[:, ft, :], in0=g_[:], in1=u_[:], op=ALU.mult), reads=[g_.b, u_.b], writes=[actT.b])
                        for tt, cg in [(tt_, cg_) for ch in range(2) for tt_ in range(4) for cg_ in (2 * ch, 2 * ch + 1)]:
                            if True:
                                if tt == 0 and cg % 2 == 0:
                                    ch = cg // 2
                                    s.dma("pool", lambda e, ex=ex, ch=ch: DMA(e, out=wdT[:], in_=w_down[ex, :, ch * 1024:(ch + 1) * 1024].rearrange("(k p) n -> p k n", p=128)), writes=[wdT.b])
                                pd = pdn.get()
                                for ft in range(16):
                                    s.op("pe", lambda e, pd=pd, ft=ft, tt=tt, cg=cg: e.matmul(pd[:], actT[:, ft, tt * 128:(tt + 1) * 128], wdT[:, ft, (cg % 2) * 512:(cg % 2 + 1) * 512], start=(ft == 0), stop=False),
                                         reads=[actT.b, wdT.b], writes=[pd.b])
                                s.op("pe", lambda e, pd=pd, cg=cg: e.matmul(pd[:], ones_r[:], bdr[:, cg * 512:(cg + 1) * 512], start=False, stop=True), reads=[ones_r.b, bdr.b], writes=[pd.b])
                                s.op("dve", lambda e, pd=pd, tt=tt, cg=cg, ex=ex: e.scalar_tensor_tensor(out=accq[:, tt, cg * 512:(cg + 1) * 512], in0=pd[:], scalar=Gq[:, tt, ex:ex + 1],
                                                                                                     in1=accq[:, tt, cg * 512:(cg + 1) * 512], op0=ALU.mult, op1=ALU.add),
                                     reads=[pd.b, Gq.b, accq.b], writes=[accq.b])
                    for tt in range(4):
                        t0_ = q8 * 512 + tt * 128
                        s.op("dve", lambda e, tt=tt: e.tensor_tensor(out=accq[:, tt, :], in0=accq[:, tt, :], in1=g2row[:], op=ALU.mult), reads=[accq.b, g2row.b], writes=[accq.b])
                        s.op("dve", lambda e, tt=tt: e.tensor_tensor(out=accq[:, tt, :], in0=accq[:, tt, :], in1=x1q[:, tt, :], op=ALU.add), reads=[accq.b, x1q.b], writes=[accq.b])
                        s.dma("sp", lambda e, tt=tt, t0_=t0_: DMA(e, out=out[t0_:t0_ + 128, :], in_=accq[:, tt, :]), reads=[accq.b])
                s.barrier()
        s.barrier()
        s.emit()
    return nc


def make_inputs(inputs):
    x = np.asarray(inputs["x"], dtype=np.float32)
    pos = np.asarray(inputs["positions"])
    maps = []
    shared = {}
    for c in range(8):
        b, j = c // 4, c % 4
        npad = 12288 - 4096 * j
        xpad = np.zeros((NT, D), np.float32)
        xpad[npad:] = x[b, : 4096 * (j + 1)]
        pp = np.zeros((NT,), np.float32)
        pp[npad:] = pos[b, : 4096 * (j + 1)].astype(np.float32)
        valid = np.zeros((128, 16), np.float32)
        valid[:, npad // 1024:] = 1.0
        kbias = np.full((128, 128), NEG, np.float32)
        kbias[:, npad // 128:] = 0.0
        m = {"xp": xpad, "pos": pp.reshape(128, 128), "valid": valid, "kbias": kbias,
             "c": np.ascontiguousarray(inputs["c"][b]).astype(np.float32)}
        for k in ["w_ada", "b_ada", "norm_mix_g", "w_in", "q_lat_g", "kv_lat_g", "w_uq", "w_ukv", "q_nope_g", "q_rope_g", "k_nope_g", "k_rope_g",
                  "ssm_A_re", "ssm_A_im", "ssm_B_re", "ssm_B_im", "ssm_C_re", "ssm_C_im", "ssm_D", "ssm_log_dt", "out_ssm_g", "out_mla_g", "w_out",
                  "norm_ffn_g", "w_router", "b_router", "w_gate_up", "b_gate_up", "w_down", "b_down"]:
            if k not in shared:
                shared[k] = np.ascontiguousarray(np.asarray(inputs[k])[0], dtype=np.float32)
            m[k] = shared[k]
        maps.append(m)
    return maps


def kernel(**inputs):
    nc = build()
    maps = make_inputs(inputs)
    res = run_bass_kernel_spmd(nc, maps, core_ids=list(range(8)))
    outp = np.zeros((2, 16384, D), np.float32)
    for c in range(8):
        b, j = c // 4, c % 4
        outp[b, 4096 * j:4096 * (j + 1)] = res.results[c]["out"]
    return outp
```

```python
import math
import numpy as np
import ml_dtypes
from contextlib import ExitStack
import concourse.bass as bass
import concourse.mybir as mybir
from concourse.bass_utils import run_bass_kernel_spmd

F32 = mybir.dt.float32
BF16 = mybir.dt.bfloat16
I32 = mybir.dt.int32
U32 = mybir.dt.uint32
AF = mybir.ActivationFunctionType
ALU = mybir.AluOpType
AX = mybir.AxisListType

ENGS = ["pe", "act", "dve", "pool", "sp"]
D = 2048
NT = 16384
NOWN = 4096
EPS = 1e-6
NEG = -30000.0


class Buf:
    __slots__ = ("name", "last_w", "reads", "excl")

    def __init__(self, name, excl=False):
        self.name = name
        self.last_w = None
        self.reads = []
        self.excl = excl


class Ev:
    __slots__ = ("key", "val", "clock")

    def __init__(self, key, val, clock):
        self.key = key
        self.val = val
        self.clock = clock


class Sched:
    def __init__(self, nc, stack, n_dma_sems=48):
        self.nc = nc
        self.ops = {e: [] for e in ENGS}
        self.cnt = {e: 0 for e in ENGS}
        self.known = {e: {} for e in ENGS}
        self.sems = {}
        for e in ENGS:
            self.sems[e] = stack.enter_context(nc.semaphore("s_" + e))
        self.dma_sems = []
        for i in range(n_dma_sems):
            self.sems[("d", i)] = stack.enter_context(nc.semaphore("d_%d" % i))
            self.dma_sems.append({"key": ("d", i), "val": 0, "last": None})
        self.dma_rr = 0
        self.dma_rr_p = 0
        self.stopped = False

    def _need(self, eng, ev):
        if ev is None:
            return
        kn = self.known[eng]
        if kn.get(ev.key, 0) >= ev.val:
            return
        if ev.key == eng and eng == "pe":
            kn[ev.key] = ev.val
            return
        sem = self.sems[ev.key]
        val = ev.val
        self.ops[eng].append(lambda e, sem=sem, val=val: e.wait_ge(sem, val))
        for k, v in ev.clock.items():
            if kn.get(k, 0) < v:
                kn[k] = v
        kn[ev.key] = max(kn.get(ev.key, 0), ev.val)

    def _deps(self, eng, reads, writes):
        for b in reads:
            self._need(eng, b.last_w)
            if b.excl:
                for r in b.reads:
                    self._need(eng, r)
        for b in writes:
            self._need(eng, b.last_w)
            for r in b.reads:
                self._need(eng, r)

    def _commit(self, ev, reads, writes):
        for b in reads:
            b.reads.append(ev)
        for b in writes:
            b.last_w = ev
            b.reads = []

    def op(self, eng, fn, reads=(), writes=()):
        if self.stopped:
            return None
        self._deps(eng, reads, writes)
        self.cnt[eng] += 1
        idx = self.cnt[eng]
        sem = self.sems[eng]
        self.ops[eng].append(lambda e, fn=fn, sem=sem: fn(e).then_inc(sem, 1))
        clock = dict(self.known[eng])
        clock[eng] = idx
        ev = Ev(eng, idx, clock)
        self._commit(ev, reads, writes)
        return ev

    def dma(self, eng, fn, reads=(), writes=()):
        if self.stopped:
            return None
        self._deps(eng, reads, writes)
        pool_ = self.dma_sems[:8] if eng == "pool" else self.dma_sems[8:]
        rr = self.dma_rr_p if eng == "pool" else self.dma_rr
        n = len(pool_)
        pick = None
        for t in range(n):
            s_ = pool_[(rr + t) % n]
            if s_["last"] is None or self.known[eng].get(s_["key"], 0) >= s_["val"]:
                pick = s_
                rr = (rr + t + 1) % n
                break
        if pick is None:
            pick = pool_[rr % n]
            rr = (rr + 1) % n
            self._need(eng, pick["last"])
        if eng == "pool":
            self.dma_rr_p = rr
        else:
            self.dma_rr = rr
        pick["val"] += 16
        sem = self.sems[pick["key"]]
        self.ops[eng].append(lambda e, fn=fn, sem=sem: fn(e).then_inc(sem, 16))
        clock = dict(self.known[eng])
        clock[pick["key"]] = pick["val"]
        ev = Ev(pick["key"], pick["val"], clock)
        pick["last"] = ev
        self._commit(ev, reads, writes)
        return ev

    def barrier(self):
        evs = []
        for e in ENGS:
            if self.cnt[e] > 0:
                clock = dict(self.known[e])
                clock[e] = self.cnt[e]
                evs.append(Ev(e, self.cnt[e], clock))
        for s_ in self.dma_sems:
            if s_["last"] is not None:
                evs.append(s_["last"])
        for e in ENGS:
            for ev in evs:
                kn = self.known[e]
                if kn.get(ev.key, 0) >= ev.val:
                    continue
                sem = self.sems[ev.key]
                val = ev.val
                self.ops[e].append(lambda en, sem=sem, val=val: en.wait_ge(sem, val))
                for k, v in ev.clock.items():
                    if kn.get(k, 0) < v:
                        kn[k] = v
                kn[ev.key] = max(kn.get(ev.key, 0), ev.val)

    def emit(self):
        nc = self.nc
        with nc.Block() as block:
            @block.tensor
            def _(e):
                for t in self.ops["pe"]:
                    t(e)

            @block.scalar
            def _(e):
                for t in self.ops["act"]:
                    t(e)

            @block.vector
            def _(e):
                for t in self.ops["dve"]:
                    t(e)

            @block.gpsimd
            def _(e):
                for t in self.ops["pool"]:
                    t(e)

            @block.sync
            def _(e):
                for t in self.ops["sp"]:
                    t(e)


class TT:
    __slots__ = ("t", "b")

    def __init__(self, t, name, excl=False):
        self.t = t
        self.b = Buf(name, excl)

    def __getitem__(self, k):
        return self.t[k]


class Ring:
    def __init__(self, items):
        self.items = items
        self.i = 0

    def get(self):
        it = self.items[self.i % len(self.items)]
        self.i += 1
        return it


INV_FREQ = [float(np.float32(10000.0) ** np.float32(-(2.0 * i) / 64.0)) for i in range(32)]
TWO_PI = 2.0 * math.pi
CW1 = 6.28125
CW2 = TWO_PI - 6.28125


def DMA(e, **kw):
    return e.dma_start(allow_slow_non_contiguous=True, **kw)


class Cut(Exception):
    pass


def build(stage=99, nblk=16, nown=4, cut=0, NE=32, moe_cap=2048):
    NT = nblk * 1024
    NOWN = nown * 1024
    nc = bass.Bass("TRN2", target_bir_lowering=False)
    din = lambda n, sh, dt=F32: nc.dram_tensor(n, sh, dt, kind="ExternalInput").ap()
    xp = din("xp", [NT, D])
    pos_in = din("pos", [128, 128])
    valid_in = din("valid", [128, 16])
    kbias_in = din("kbias", [128, 128])
    c_in = din("c", [D])
    w_ada = din("w_ada", [D, 6 * D])
    b_ada = din("b_ada", [6 * D])
    g_mix = din("norm_mix_g", [D])
    w_in = din("w_in", [D, 2880])
    q_lat_g = din("q_lat_g", [512])
    kv_lat_g = din("kv_lat_g", [256])
    w_uq = din("w_uq", [512, 1536])
    w_ukv = din("w_ukv", [256, 2048])
    q_nope_g = din("q_nope_g", [128])
    q_rope_g = din("q_rope_g", [64])
    k_nope_g = din("k_nope_g", [128])
    k_rope_g = din("k_rope_g", [64])
    ssm_A_re = din("ssm_A_re", [64, 64])
    ssm_A_im = din("ssm_A_im", [64, 64])
    ssm_B_re = din("ssm_B_re", [64, 64, 16])
    ssm_B_im = din("ssm_B_im", [64, 64, 16])
    ssm_C_re = din("ssm_C_re", [64, 16, 64])
    ssm_C_im = din("ssm_C_im", [64, 16, 64])
    ssm_D = din("ssm_D", [1024])
    ssm_log_dt = din("ssm_log_dt", [64])
    out_ssm_g = din("out_ssm_g", [1024])
    out_mla_g = din("out_mla_g", [1024])
    w_out = din("w_out", [2048, 2048])
    norm_ffn_g = din("norm_ffn_g", [2048])
    w_router = din("w_router", [2048, NE])
    b_router = din("b_router", [NE])
    w_gate_up = din("w_gate_up", [NE, 2048, 4096])
    b_gate_up = din("b_gate_up", [NE, 4096])
    w_down = din("w_down", [NE, 2048, 2048])
    b_down = din("b_down", [NE, 2048])
    out = nc.dram_tensor("out", [NOWN, D], F32, kind="ExternalOutput").ap()
    x1_d = nc.dram_tensor("x1_d", [NOWN, D], F32).ap()
    NCH = NT // 8
    Ud = nc.dram_tensor("Ud", [64, 128, NCH], BF16).ap()
    Gd = nc.dram_tensor("Gd", [NOWN // 1024, 128, 8, 1024], BF16).ap()
    ys5_d = nc.dram_tensor("ys5_d", [NOWN, 1024], BF16).ap()

    kT_d = nc.dram_tensor("kT_d", [8, 128, NT], BF16).ap()
    krT_d = nc.dram_tensor("krT_d", [64, NT], BF16).ap()
    V_d = nc.dram_tensor("V_d", [NT, 8, 128], BF16).ap()
    qTn_d = nc.dram_tensor("qTn_d", [8, 128, NOWN], BF16).ap()
    qTr_d = nc.dram_tensor("qTr_d", [4, 128, NOWN], BF16).ap()
    ymla_d = nc.dram_tensor("ymla_d", [NOWN, 1024], BF16).ap()
    dbg = {}
    if stage == 0:
        dbg["mod"] = nc.dram_tensor("dbg_mod", [128, 32], F32, kind="ExternalOutput").ap()
    if stage == 1:
        dbg["kT"] = nc.dram_tensor("dbg_kT", [8, 128, NT], BF16, kind="ExternalOutput").ap()
        dbg["krT"] = nc.dram_tensor("dbg_krT", [64, NT], BF16, kind="ExternalOutput").ap()
        dbg["V"] = nc.dram_tensor("dbg_V", [NT, 8, 128], BF16, kind="ExternalOutput").ap()
        dbg["mod"] = nc.dram_tensor("dbg_mod", [128, 32], F32, kind="ExternalOutput").ap()
        dbg["qTn"] = nc.dram_tensor("dbg_qTn", [8, 128, NOWN], BF16, kind="ExternalOutput").ap()
        dbg["qTr"] = nc.dram_tensor("dbg_qTr", [4, 128, NOWN], BF16, kind="ExternalOutput").ap()
    if stage == 4:
        dbg["x1"] = nc.dram_tensor("dbg_x1", [NOWN, D], F32, kind="ExternalOutput").ap()
    if stage == 3:
        dbg["ys5"] = nc.dram_tensor("dbg_ys5", [NOWN, 1024], BF16, kind="ExternalOutput").ap()
    if stage == 2:
        dbg["ymla"] = nc.dram_tensor("dbg_ymla", [NOWN, 1024], BF16, kind="ExternalOutput").ap()

    with ExitStack() as root:
        s = Sched(nc, root)

        def sbt(st, name, shape, dt):
            return TT(st.enter_context(nc.sbuf_tensor("sb_" + name, shape, dt)), name)

        def pst(st, name, shape, dt):
            return TT(st.enter_context(nc.psum_tensor("ps_" + name, shape, dt)), name, True)

        ident_i = sbt(root, "ident_i", [128, 128], I32)
        ident_f = sbt(root, "ident_f", [128, 128], F32)
        ident = sbt(root, "ident", [128, 128], BF16)
        ones_r = sbt(root, "ones_r", [1, 128], BF16)
        s.op("pool", lambda e: e.iota(ident_i[:], [[1, 128]], base=0, channel_multiplier=-1), writes=[ident_i.b])
        s.op("dve", lambda e: e.tensor_scalar(out=ident_f[:], in0=ident_i[:], scalar1=0, scalar2=None, op0=ALU.is_equal),
             reads=[ident_i.b], writes=[ident_f.b])
        s.op("dve", lambda e: e.tensor_copy(ident[:], ident_f[:]), reads=[ident_f.b], writes=[ident.b])
        s.op("dve", lambda e: e.memset(ones_r[:], 1.0), writes=[ones_r.b])

        sh1c = sbt(root, "sh1c", [128, 16], F32)
        a1c = sbt(root, "a1c", [128, 16], F32)
        sh1c16 = sbt(root, "sh1c16", [128, 16], BF16)

        with ExitStack() as p0:
            cT = sbt(p0, "cT", [128, 16], F32)
            cact = sbt(p0, "cact", [128, 16], BF16)
            s.dma("sp", lambda e: DMA(e, out=cT[:], in_=c_in.rearrange("(k p) -> p k", p=128)), writes=[cT.b])
            s.op("act", lambda e: e.activation(out=cact[:], in_=cT[:], func=AF.Silu), reads=[cT.b], writes=[cact.b])
            wa = Ring([sbt(p0, "wa%d" % i, [128, 16, 512], BF16) for i in range(2)])
            pacc = Ring([pst(p0, "pacc%d" % i, [128, 512], F32) for i in range(2)])
            modc = sbt(p0, "modc", [128, 32], F32)
            badc = sbt(p0, "badc", [128, 32], F32)
            gmc = sbt(p0, "gmc", [128, 16], F32)
            s.dma("sp", lambda e: DMA(e, out=badc[:], in_=b_ada[0:4096].rearrange("(k p) -> p k", p=128)), writes=[badc.b])
            s.dma("sp", lambda e: DMA(e, out=gmc[:], in_=g_mix.rearrange("(k p) -> p k", p=128)), writes=[gmc.b])
            for j in range(8):
                w = wa.get()
                s.dma("pool", lambda e, w=w, j=j: DMA(e, out=w[:], in_=w_ada[:, j * 512:(j + 1) * 512].rearrange("(k p) n -> p k n", p=128)),
                      writes=[w.b])
                pa = pacc.get()
                for sub in range(4):
                    for k in range(16):
                        s.op("pe", lambda e, w=w, pa=pa, sub=sub, k=k: e.matmul(pa[:, sub:sub + 1], w[:, k, sub * 128:(sub + 1) * 128], cact[:, k:k + 1],
                                                                               start=(k == 0), stop=(k == 15)),
                             reads=[w.b, cact.b], writes=[pa.b])
                s.op("dve", lambda e, pa=pa, j=j: e.tensor_tensor(out=modc[:, j * 4:(j + 1) * 4], in0=pa[:, 0:4], in1=badc[:, j * 4:(j + 1) * 4], op=ALU.add),
                     reads=[pa.b, badc.b], writes=[modc.b])
            s.op("dve", lambda e: e.tensor_copy(sh1c[:], modc[:, 0:16]), reads=[modc.b], writes=[sh1c.b])
            s.op("dve", lambda e: e.tensor_copy(sh1c16[:], modc[:, 0:16]), reads=[modc.b], writes=[sh1c16.b])
            s.op("dve", lambda e: e.scalar_tensor_tensor(out=a1c[:], in0=modc[:, 16:32], scalar=1.0, in1=gmc[:], op0=ALU.add, op1=ALU.mult),
                 reads=[modc.b, gmc.b], writes=[a1c.b])
            if stage <= 1:
                s.dma("sp", lambda e: DMA(e, out=dbg["mod"], in_=modc[:]), reads=[modc.b])
            s.barrier()
        if stage == 0:
            s.barrier()
            s.emit()
            return nc

        def cp(n):
            if cut == n:
                s.stopped = True
        try:
          with ExitStack() as p1:
              NCA = 320
              wA = sbt(p1, "wA", [128, 16, NCA], BF16)
              biasA = sbt(p1, "biasA", [1, NCA], BF16)
              wukv = sbt(p1, "wukv", [128, 2, 2048], BF16)
              kvg = sbt(p1, "kvg", [128, 2], F32)
              gk = sbt(p1, "gk", [128, 1], F32)
              gq_ = sbt(p1, "gq_", [128, 1], F32)
              gkr = sbt(p1, "gkr", [128, 64], F32)
              posT = sbt(p1, "posT", [128, 128], F32)
              cosT = sbt(p1, "cosT", [128, 128, 32], BF16)
              sinT = sbt(p1, "sinT", [128, 128, 32], BF16)
              kbias = sbt(p1, "kbias", [128, 128], F32)
              s.dma("pool", lambda e: DMA(e, out=wA[:], in_=w_in[:, 2560:2880].rearrange("(k p) n -> p k n", p=128)), writes=[wA.b])
              s.dma("pool", lambda e: DMA(e, out=wukv[:], in_=w_ukv.rearrange("(k p) n -> p k n", p=128)), writes=[wukv.b])
              s.dma("sp", lambda e: DMA(e, out=kvg[:], in_=kv_lat_g.rearrange("(k p) -> p k", p=128)), writes=[kvg.b])
              s.dma("sp", lambda e: DMA(e, out=gk[:], in_=k_nope_g.rearrange("(p o) -> p o", o=1)), writes=[gk.b])
              s.dma("sp", lambda e: DMA(e, out=gq_[:], in_=q_nope_g.rearrange("(p o) -> p o", o=1)), writes=[gq_.b])
              s.dma("sp", lambda e: DMA(e, out=gkr[:], in_=k_rope_g.partition_broadcast(128)), writes=[gkr.b])
              s.op("dve", lambda e: e.tensor_tensor(out=gk[:], in0=gk[:], in1=gq_[:], op=ALU.mult), reads=[gk.b, gq_.b], writes=[gk.b])
              pb = pst(p1, "pb", [1, 512], F32)
              for k in range(16):
                  s.op("pe", lambda e, k=k: e.matmul(pb[:, 0:NCA], sh1c16[:, k:k + 1], wA[:, k, :], start=(k == 0), stop=(k == 15)),
                       reads=[sh1c16.b, wA.b], writes=[pb.b])
              s.op("dve", lambda e: e.tensor_copy(biasA[:], pb[:, 0:NCA]), reads=[pb.b], writes=[biasA.b])
              for k in range(16):
                  s.op("dve", lambda e, k=k: e.tensor_scalar(out=wA[:, k, :], in0=wA[:, k, :], scalar1=a1c[:, k:k + 1], scalar2=None, op0=ALU.mult),
                       reads=[wA.b, a1c.b], writes=[wA.b])
              for k in range(2):
                  s.op("dve", lambda e, k=k: e.tensor_scalar(out=wukv[:, k, :], in0=wukv[:, k, :], scalar1=kvg[:, k:k + 1], scalar2=None, op0=ALU.mult),
                       reads=[wukv.b, kvg.b], writes=[wukv.b])
              cp(1)
              ptile = sbt(p1, "ptile", [128, 128], F32)
              ppos = pst(p1, "ppos", [128, 512], F32)
              s.dma("sp", lambda e: DMA(e, out=ptile[:], in_=pos_in), writes=[ptile.b])
              s.dma("sp", lambda e: DMA(e, out=kbias[:], in_=kbias_in), writes=[kbias.b])
              s.op("pe", lambda e: e.transpose(ppos[:, 0:128], ptile[:], ident_f[:]), reads=[ptile.b, ident_f.b], writes=[ppos.b])
              s.op("dve", lambda e: e.tensor_copy(posT[:], ppos[:, 0:128]), reads=[ppos.b], writes=[posT.b])
              ptrig = ExitStack()
              ang = sbt(ptrig, "ang", [128, 128, 32], F32)
              kf = sbt(ptrig, "kf", [128, 128, 32], F32)
              ki = sbt(ptrig, "ki", [128, 128, 32], I32)
              for i in range(32):
                  s.op("dve", lambda e, i=i: e.tensor_scalar(out=ang[:, :, i], in0=posT[:], scalar1=INV_FREQ[i], scalar2=None, op0=ALU.mult),
                       reads=[posT.b], writes=[ang.b])

              def wrap(t):
                  s.op("dve", lambda e: e.tensor_scalar(out=kf[:], in0=t[:], scalar1=math.pi, scalar2=-TWO_PI, op0=ALU.is_gt, op1=ALU.mult),
                       reads=[t.b], writes=[kf.b])
                  s.op("dve", lambda e: e.tensor_tensor(out=t[:], in0=t[:], in1=kf[:], op=ALU.add), reads=[t.b, kf.b], writes=[t.b])
                  s.op("dve", lambda e: e.tensor_scalar(out=kf[:], in0=t[:], scalar1=-math.pi, scalar2=TWO_PI, op0=ALU.is_lt, op1=ALU.mult),
                       reads=[t.b], writes=[kf.b])
                  s.op("dve", lambda e: e.tensor_tensor(out=t[:], in0=t[:], in1=kf[:], op=ALU.add), reads=[t.b, kf.b], writes=[t.b])

              s.op("dve", lambda e: e.tensor_scalar(out=kf[:], in0=ang[:], scalar1=1.0 / TWO_PI, scalar2=None, op0=ALU.mult), reads=[ang.b], writes=[kf.b])
              s.op("dve", lambda e: e.tensor_copy(ki[:], kf[:]), reads=[kf.b], writes=[ki.b])
              s.op("dve", lambda e: e.tensor_copy(kf[:], ki[:]), reads=[ki.b], writes=[kf.b])
              s.op("dve", lambda e: e.scalar_tensor_tensor(out=ang[:], in0=kf[:], scalar=-CW1, in1=ang[:], op0=ALU.mult, op1=ALU.add),
                   reads=[kf.b, ang.b], writes=[ang.b])
              s.op("dve", lambda e: e.scalar_tensor_tensor(out=ang[:], in0=kf[:], scalar=-CW2, in1=ang[:], op0=ALU.mult, op1=ALU.add),
                   reads=[kf.b, ang.b], writes=[ang.b])
              wrap(ang)
              s.op("act", lambda e: e.activation(out=sinT[:], in_=ang[:], func=AF.Sin), reads=[ang.b], writes=[sinT.b])
              s.op("dve", lambda e: e.tensor_scalar(out=ang[:], in0=ang[:], scalar1=math.pi / 2, scalar2=None, op0=ALU.add), reads=[ang.b], writes=[ang.b])
              wrap(ang)
              s.op("act", lambda e: e.activation(out=cosT[:], in_=ang[:], func=AF.Sin), reads=[ang.b], writes=[cosT.b])

              s.barrier()
              ptrig.close()
              cp(2)
              wQ = sbt(p1, "wQ", [128, 16, 512], BF16)
              biasQ = sbt(p1, "biasQ", [1, 512], BF16)
              wuq = sbt(p1, "wuq", [128, 4, 1536], BF16)
              qlg = sbt(p1, "qlg", [128, 4], F32)
              gqr = sbt(p1, "gqr", [128, 64], F32)
              s.dma("pool", lambda e: DMA(e, out=wQ[:], in_=w_in[:, 2048:2560].rearrange("(k p) n -> p k n", p=128)), writes=[wQ.b])
              s.dma("pool", lambda e: DMA(e, out=wuq[:], in_=w_uq.rearrange("(k p) n -> p k n", p=128)), writes=[wuq.b])
              s.dma("sp", lambda e: DMA(e, out=qlg[:], in_=q_lat_g.rearrange("(k p) -> p k", p=128)), writes=[qlg.b])
              s.dma("sp", lambda e: DMA(e, out=gqr[:], in_=q_rope_g.partition_broadcast(128)), writes=[gqr.b])
              for k in range(16):
                  s.op("pe", lambda e, k=k: e.matmul(pb[:, 0:512], sh1c16[:, k:k + 1], wQ[:, k, :], start=(k == 0), stop=(k == 15)),
                       reads=[sh1c16.b, wQ.b], writes=[pb.b])
              s.op("dve", lambda e: e.tensor_copy(biasQ[:], pb[:, 0:512]), reads=[pb.b], writes=[biasQ.b])
              for k in range(16):
                  s.op("dve", lambda e, k=k: e.tensor_scalar(out=wQ[:, k, :], in0=wQ[:, k, :], scalar1=a1c[:, k:k + 1], scalar2=None, op0=ALU.mult),
                       reads=[wQ.b, a1c.b], writes=[wQ.b])
              for k in range(4):
                  s.op("dve", lambda e, k=k: e.tensor_scalar(out=wuq[:, k, :], in0=wuq[:, k, :], scalar1=qlg[:, k:k + 1], scalar2=None, op0=ALU.mult),
                       reads=[wuq.b, qlg.b], writes=[wuq.b])
              qn16 = Ring([sbt(p1, "qn16%d" % i, [128, 512], BF16) for i in range(2)])
              qnT = Ring([sbt(p1, "qnT%d" % i, [128, 4, 128], BF16) for i in range(2)])
              sqq = sbt(p1, "sqq", [128, 8, 192], F32)
              ssq = Ring([sbt(p1, "ssq%d" % i, [128, 16], F32) for i in range(2)])
              qno16 = Ring([sbt(p1, "qno%d" % i, [128, 8, 128], BF16) for i in range(2)])
              qrA = sbt(p1, "qrA", [128, 8, 64], F32)
              qrB = sbt(p1, "qrB", [128, 8, 64], F32)
              qrC = sbt(p1, "qrC", [128, 8, 64], F32)
              qr16 = Ring([sbt(p1, "qr16%d" % i, [128, 8, 64], BF16) for i in range(2)])
              qnstR = Ring([sbt(p1, "qnst%d" % i, [128, 8, 128], BF16) for i in range(2)])
              qrstR = Ring([sbt(p1, "qrst%d" % i, [128, 4, 128], BF16) for i in range(2)])
              xpool = Ring([sbt(p1, "xt%d" % i, [128, D], F32) for i in range(2)])
              junk = sbt(p1, "junk", [128, D], BF16)
              xh = Ring([sbt(p1, "xh%d" % i, [128, D], BF16) for i in range(2)])
              xhT = sbt(p1, "xhT", [128, 16, 1024], BF16)
              ptr = Ring([pst(p1, "ptr%d" % i, [128, 1024], BF16) for i in range(2)])
              pacc = Ring([pst(p1, "pa%d" % i, [128, 512], F32) for i in range(4)])
              ssr = Ring([sbt(p1, "ss%d" % i, [128, 4], F32) for i in range(4)])
              kvn = Ring([sbt(p1, "kvn%d" % i, [128, 256], BF16) for i in range(2)])
              kvnT = Ring([sbt(p1, "kvnT%d" % i, [128, 2, 128], BF16) for i in range(2)])
              kr = Ring([sbt(p1, "kr%d" % i, [128, 64], F32) for i in range(2)])
              kr2 = Ring([sbt(p1, "krb%d" % i, [128, 64], F32) for i in range(2)])
              kr16 = Ring([sbt(p1, "krc%d" % i, [128, 128], BF16) for i in range(2)])
              for t_ in kr16.items:
                  s.op("dve", lambda e, t_=t_: e.memset(t_[:], 0.0), writes=[t_.b])
              sq = Ring([sbt(p1, "sq%d" % i, [128, 8, 128], F32) for i in range(2)])
              kn16 = Ring([sbt(p1, "kn16%d" % i, [128, 8, 128], BF16) for i in range(2)])
              v16 = Ring([sbt(p1, "v16%d" % i, [128, 8, 128], BF16) for i in range(2)])
              kstR = Ring([sbt(p1, "kst%d" % i, [128, 8, 128], BF16) for i in range(2)])
              krst = sbt(p1, "krst", [128, 1024], BF16)
              ev_alt = [0]

              def evac(out_ap, in_ap, reads, writes):
                  ev_alt[0] += 1
                  if ev_alt[0] % 2:
                      s.op("dve", lambda e: e.tensor_copy(out_ap, in_ap), reads=reads, writes=writes)
                  else:
                      s.op("act", lambda e: e.activation(out=out_ap, in_=in_ap, func=AF.Copy), reads=reads, writes=writes)

              def rstd_from_ss(ss, col, n):
                  s.op("dve", lambda e: e.tensor_scalar(out=ss[:, col:col + 1], in0=ss[:, col:col + 1], scalar1=1.0 / n, scalar2=EPS, op0=ALU.mult, op1=ALU.add),
                       reads=[ss.b], writes=[ss.b])
                  s.op("act", lambda e: e.activation(out=ss[:, col:col + 1], in_=ss[:, col:col + 1], func=AF.Sqrt), reads=[ss.b], writes=[ss.b])
                  s.op("dve", lambda e: e.reciprocal(ss[:, col:col + 1], ss[:, col:col + 1]), reads=[ss.b], writes=[ss.b])

              def make_xhT(blk):
                  for tt in range(8):
                      r0 = blk * 1024 + tt * 128
                      xt = xpool.get()
                      s.dma("sp", lambda e, xt=xt, r0=r0: DMA(e, out=xt[:], in_=xp[r0:r0 + 128, :]), writes=[xt.b])
                      ss = ssr.get()
                      s.op("act", lambda e, xt=xt, ss=ss: e.activation(out=junk[:], in_=xt[:], func=AF.Square, accum_out=ss[:, 0:1]),
                           reads=[xt.b], writes=[junk.b, ss.b])
                      rstd_from_ss(ss, 0, D)
                      xb = xh.get()
                      s.op("dve", lambda e, xt=xt, ss=ss, xb=xb: e.tensor_scalar(out=xb[:], in0=xt[:], scalar1=ss[:, 0:1], scalar2=None, op0=ALU.mult),
                           reads=[xt.b, ss.b], writes=[xb.b])
                      for half in range(2):
                          pt = ptr.get()
                          for k8 in range(8):
                              k = half * 8 + k8
                              s.op("pe", lambda e, pt=pt, xb=xb, k8=k8, k=k: e.transpose(pt[:, k8 * 128:(k8 + 1) * 128], xb[:, k * 128:(k + 1) * 128], ident[:]),
                                   reads=[xb.b, ident.b], writes=[pt.b])
                          evac(xhT[:, half * 8:(half + 1) * 8, tt * 128:(tt + 1) * 128], pt[:].rearrange("p (a b) -> p a b", a=8), [pt.b], [xhT.b])

              for blk in range(nblk):
                  make_xhT(blk)
                  cp(3)
                  for tt in range(8):
                      tile_i = blk * 8 + tt
                      r0 = tile_i * 128
                      pa = pacc.get()
                      for k in range(16):
                          s.op("pe", lambda e, pa=pa, k=k, tt=tt: e.matmul(pa[:, 0:NCA], xhT[:, k, tt * 128:(tt + 1) * 128], wA[:, k, :], start=(k == 0), stop=False),
                               reads=[xhT.b, wA.b], writes=[pa.b])
                      s.op("pe", lambda e, pa=pa: e.matmul(pa[:, 0:NCA], ones_r[:], biasA[:], start=False, stop=True),
                           reads=[ones_r.b, biasA.b], writes=[pa.b])
                      ss = ssr.get()
                      s.op("act", lambda e, pa=pa, ss=ss: e.activation(out=junk[:, 0:256], in_=pa[:, 0:256], func=AF.Square, accum_out=ss[:, 0:1]),
                           reads=[pa.b], writes=[junk.b, ss.b])
                      s.op("act", lambda e, pa=pa, ss=ss: e.activation(out=junk[:, 256:320], in_=pa[:, 256:320], func=AF.Square, accum_out=ss[:, 1:2]),
                           reads=[pa.b], writes=[junk.b, ss.b])
                      rstd_from_ss(ss, 0, 256)
                      rstd_from_ss(ss, 1, 64)
                      kn_ = kvn.get()
                      s.op("dve", lambda e, pa=pa, ss=ss, kn_=kn_: e.tensor_scalar(out=kn_[:], in0=pa[:, 0:256], scalar1=ss[:, 0:1], scalar2=None, op0=ALU.mult),
                           reads=[pa.b, ss.b], writes=[kn_.b])
                      cp(4)
                      k1 = kr.get()
                      s.op("dve", lambda e, pa=pa, ss=ss, k1=k1: e.scalar_tensor_tensor(out=k1[:], in0=pa[:, 256:320], scalar=ss[:, 1:2], in1=gkr[:], op0=ALU.mult, op1=ALU.mult),
                           reads=[pa.b, ss.b, gkr.b], writes=[k1.b])
                      k2 = kr2.get()
                      k3 = kr16.get()
                      cs = cosT[:, tile_i, :]
                      sn = sinT[:, tile_i, :]
                      s.op("pool", lambda e, k1=k1, k2=k2, cs=cs: e.tensor_tensor(out=k2[:, 0:32], in0=k1[:, 0:32], in1=cs, op=ALU.mult), reads=[k1.b, cosT.b], writes=[k2.b])
                      s.op("pool", lambda e, k1=k1, k2=k2, cs=cs: e.tensor_tensor(out=k2[:, 32:64], in0=k1[:, 32:64], in1=cs, op=ALU.mult), reads=[k1.b, cosT.b], writes=[k2.b])
                      tmpa = kr.get()
                      s.op("pool", lambda e, k1=k1, tmpa=tmpa, sn=sn: e.tensor_tensor(out=tmpa[:, 0:32], in0=k1[:, 32:64], in1=sn, op=ALU.mult), reads=[k1.b, sinT.b], writes=[tmpa.b])
                      s.op("pool", lambda e, k1=k1, tmpa=tmpa, sn=sn: e.tensor_tensor(out=tmpa[:, 32:64], in0=k1[:, 0:32], in1=sn, op=ALU.mult), reads=[k1.b, sinT.b], writes=[tmpa.b])
                      s.op("pool", lambda e, k2=k2, k3=k3, tmpa=tmpa: e.tensor_tensor(out=k3[:, 0:32], in0=k2[:, 0:32], in1=tmpa[:, 0:32], op=ALU.subtract), reads=[k2.b, tmpa.b], writes=[k3.b])
                      s.op("pool", lambda e, k2=k2, k3=k3, tmpa=tmpa: e.tensor_tensor(out=k3[:, 32:64], in0=k2[:, 32:64], in1=tmpa[:, 32:64], op=ALU.add), reads=[k2.b, tmpa.b], writes=[k3.b])
                      cp(5)
                      pt = ptr.get()
                      for k in range(2):
                          s.op("pe", lambda e, pt=pt, kn_=kn_, k=k: e.transpose(pt[:, k * 128:(k + 1) * 128], kn_[:, k * 128:(k + 1) * 128], ident[:]),
                               reads=[kn_.b, ident.b], writes=[pt.b])
                      s.op("pe", lambda e, pt=pt, k3=k3: e.transpose(pt[:, 256:384], k3[:, :], ident[:]), reads=[k3.b, ident.b], writes=[pt.b])
                      cp(8)
                      kT_ = kvnT.get()
                      evac(kT_[:], pt[:, 0:256].rearrange("p (a b) -> p a b", a=2), [pt.b], [kT_.b])
                      evac(krst[:, tt * 128:(tt + 1) * 128], pt[:, 256:384], [pt.b], [krst.b])
                      cp(6)
                      sq_ = sq.get()
                      k16 = kn16.get()
                      v_ = v16.get()
                      pus = []
                      for cg in range(4):
                          pu = pacc.get()
                          pus.append(pu)
                          for k in range(2):
                              s.op("pe", lambda e, pu=pu, kT_=kT_, k=k, cg=cg: e.matmul(pu[:], kT_[:, k, :], wukv[:, k, cg * 512:(cg + 1) * 512], start=(k == 0), stop=(k == 1)),
                                   reads=[kT_.b, wukv.b], writes=[pu.b])
                          puv = pu[:].rearrange("p (h d) -> p h d", h=2)
                          s.op("act", lambda e, puv=puv, sq_=sq_, cg=cg: e.activation(out=sq_[:, 2 * cg:2 * cg + 2, :], in_=puv[:, :, 0:128], func=AF.Square),
                               reads=[pu.b], writes=[sq_.b])
                          s.op("act", lambda e, puv=puv, v_=v_, cg=cg: e.activation(out=v_[:, 2 * cg:2 * cg + 2, :], in_=puv[:, :, 128:256], func=AF.Copy),
                               reads=[pu.b], writes=[v_.b])
                      ss2 = ssr.get()
                      ss8 = sq.get()
                      s.op("dve", lambda e, sq_=sq_, ss8=ss8: e.tensor_reduce(out=ss8[:, :, 0], in_=sq_[:], axis=AX.X, op=ALU.add), reads=[sq_.b], writes=[ss8.b])
                      s.op("dve", lambda e, ss8=ss8: e.tensor_scalar(out=ss8[:, :, 0], in0=ss8[:, :, 0], scalar1=1.0 / 128, scalar2=EPS, op0=ALU.mult, op1=ALU.add),
                           reads=[ss8.b], writes=[ss8.b])
                      s.op("act", lambda e, ss8=ss8: e.activation(out=ss8[:, :, 0], in_=ss8[:, :, 0], func=AF.Sqrt), reads=[ss8.b], writes=[ss8.b])
                      s.op("dve", lambda e, ss8=ss8: e.reciprocal(ss8[:, :, 0], ss8[:, :, 0]), reads=[ss8.b], writes=[ss8.b])
                      for cg in range(4):
                          pu = pus[cg]
                          puv = pu[:].rearrange("p (h d) -> p h d", h=2)
                          s.op("dve", lambda e, puv=puv, k16=k16, ss8=ss8, cg=cg: e.tensor_tensor(out=k16[:, 2 * cg:2 * cg + 2, :], in0=puv[:, :, 0:128],
                                                                                                    in1=ss8[:, 2 * cg:2 * cg + 2, 0:1].to_broadcast([128, 2, 128]), op=ALU.mult),
                               reads=[pu.b, ss8.b], writes=[k16.b])
                      cp(7)
                      s.dma("sp", lambda e, v_=v_, r0=r0: DMA(e, out=V_d[r0:r0 + 128, :, :], in_=v_[:]), reads=[v_.b])
                      pt2 = ptr.get()
                      for h in range(8):
                          s.op("pe", lambda e, pt2=pt2, k16=k16, h=h: e.transpose(pt2[:, h * 128:(h + 1) * 128], k16[:, h, :], ident[:]),
                               reads=[k16.b, ident.b], writes=[pt2.b])
                      kst = kstR.get()
                      s.op("dve", lambda e, pt2=pt2, kst=kst: e.tensor_scalar(out=kst[:], in0=pt2[:].rearrange("p (a b) -> p a b", a=8),
                                                                           scalar1=gk[:, 0:1], scalar2=None, op0=ALU.mult),
                           reads=[pt2.b, gk.b], writes=[kst.b])
                      s.dma("sp", lambda e, kst=kst, r0=r0: DMA(e, out=kT_d[:, :, r0:r0 + 128].rearrange("h d t -> d h t"), in_=kst[:]), reads=[kst.b])

                      if blk >= nblk - nown:
                          pq = pacc.get()
                          for k in range(16):
                              s.op("pe", lambda e, pq=pq, k=k, tt=tt: e.matmul(pq[:], xhT[:, k, tt * 128:(tt + 1) * 128], wQ[:, k, :], start=(k == 0), stop=False),
                                   reads=[xhT.b, wQ.b], writes=[pq.b])
                          s.op("pe", lambda e, pq=pq: e.matmul(pq[:], ones_r[:], biasQ[:], start=False, stop=True), reads=[ones_r.b, biasQ.b], writes=[pq.b])
                          ssA = ssr.get()
                          s.op("act", lambda e, pq=pq, ssA=ssA: e.activation(out=junk[:, 0:512], in_=pq[:], func=AF.Square, accum_out=ssA[:, 0:1]),
                               reads=[pq.b], writes=[junk.b, ssA.b])
                          rstd_from_ss(ssA, 0, 512)
                          qn_ = qn16.get()
                          s.op("dve", lambda e, pq=pq, ssA=ssA, qn_=qn_: e.tensor_scalar(out=qn_[:], in0=pq[:], scalar1=ssA[:, 0:1], scalar2=None, op0=ALU.mult),
                               reads=[pq.b, ssA.b], writes=[qn_.b])
                          ptq = ptr.get()
                          for k in range(4):
                              s.op("pe", lambda e, ptq=ptq, qn_=qn_, k=k: e.transpose(ptq[:, k * 128:(k + 1) * 128], qn_[:, k * 128:(k + 1) * 128], ident[:]),
                                   reads=[qn_.b, ident.b], writes=[ptq.b])
                          qT_ = qnT.get()
                          evac(qT_[:], ptq[:, 0:512].rearrange("p (a b) -> p a b", a=4), [ptq.b], [qT_.b])
                          pqs = []
                          for cg in range(4):
                              pu = pacc.get()
                              pqs.append(pu)
                              for k in range(4):
                                  s.op("pe", lambda e, pu=pu, qT_=qT_, k=k, cg=cg: e.matmul(pu[:, 0:384], qT_[:, k, :], wuq[:, k, cg * 384:(cg + 1) * 384], start=(k == 0), stop=(k == 3)),
                                       reads=[qT_.b, wuq.b], writes=[pu.b])
                              s.op("act", lambda e, pu=pu, cg=cg: e.activation(out=sqq[:, 2 * cg:2 * cg + 2, :], in_=pu[:, 0:384].rearrange("p (h d) -> p h d", h=2), func=AF.Square),
                                   reads=[pu.b], writes=[sqq.b])
                          s8 = ssq.get()
                          s.op("dve", lambda e, s8=s8: e.tensor_reduce(out=s8[:, 0:8], in_=sqq[:, :, 0:128], axis=AX.X, op=ALU.add), reads=[sqq.b], writes=[s8.b])
                          s.op("dve", lambda e, s8=s8: e.tensor_reduce(out=s8[:, 8:16], in_=sqq[:, :, 128:192], axis=AX.X, op=ALU.add), reads=[sqq.b], writes=[s8.b])
                          s.op("dve", lambda e, s8=s8: e.tensor_scalar(out=s8[:, 0:8], in0=s8[:, 0:8], scalar1=1.0 / 128, scalar2=EPS, op0=ALU.mult, op1=ALU.add), reads=[s8.b], writes=[s8.b])
                          s.op("dve", lambda e, s8=s8: e.tensor_scalar(out=s8[:, 8:16], in0=s8[:, 8:16], scalar1=1.0 / 64, scalar2=EPS, op0=ALU.mult, op1=ALU.add), reads=[s8.b], writes=[s8.b])
                          s.op("act", lambda e, s8=s8: e.activation(out=s8[:], in_=s8[:], func=AF.Sqrt), reads=[s8.b], writes=[s8.b])
                          s.op("dve", lambda e, s8=s8: e.reciprocal(s8[:], s8[:]), reads=[s8.b], writes=[s8.b])
                          qno = qno16.get()
                          for cg in range(4):
                              pu = pqs[cg]
                              puv = pu[:, 0:384].rearrange("p (h d) -> p h d", h=2)
                              s.op("dve", lambda e, puv=puv, qno=qno, s8=s8, cg=cg: e.tensor_tensor(out=qno[:, 2 * cg:2 * cg + 2, :], in0=puv[:, :, 0:128],
                                                                                                    in1=s8[:, 2 * cg:2 * cg + 2].unsqueeze(2).to_broadcast([128, 2, 128]), op=ALU.mult),
                                   reads=[pu.b, s8.b], writes=[qno.b])
                              s.op("dve", lambda e, puv=puv, s8=s8, cg=cg: e.tensor_tensor(out=qrA[:, 2 * cg:2 * cg + 2, :], in0=puv[:, :, 128:192],
                                                                                           in1=s8[:, 8 + 2 * cg:10 + 2 * cg].unsqueeze(2).to_broadcast([128, 2, 64]), op=ALU.mult),
                                   reads=[pu.b, s8.b], writes=[qrA.b])
                          s.op("pool", lambda e: e.tensor_tensor(out=qrA[:], in0=qrA[:], in1=gqr[:].unsqueeze(1).to_broadcast([128, 8, 64]), op=ALU.mult),
                               reads=[qrA.b, gqr.b], writes=[qrA.b])
                          csb = cosT[:, tile_i, :].unsqueeze(1).to_broadcast([128, 8, 32])
                          snb = sinT[:, tile_i, :].unsqueeze(1).to_broadcast([128, 8, 32])
                          s.op("pool", lambda e, csb=csb: e.tensor_tensor(out=qrB[:, :, 0:32], in0=qrA[:, :, 0:32], in1=csb, op=ALU.mult), reads=[qrA.b, cosT.b], writes=[qrB.b])
                          s.op("pool", lambda e, csb=csb: e.tensor_tensor(out=qrB[:, :, 32:64], in0=qrA[:, :, 32:64], in1=csb, op=ALU.mult), reads=[qrA.b, cosT.b], writes=[qrB.b])
                          s.op("pool", lambda e, snb=snb: e.tensor_tensor(out=qrC[:, :, 0:32], in0=qrA[:, :, 32:64], in1=snb, op=ALU.mult), reads=[qrA.b, sinT.b], writes=[qrC.b])
                          s.op("pool", lambda e, snb=snb: e.tensor_tensor(out=qrC[:, :, 32:64], in0=qrA[:, :, 0:32], in1=snb, op=ALU.mult), reads=[qrA.b, sinT.b], writes=[qrC.b])
                          q16 = qr16.get()
                          s.op("pool", lambda e, q16=q16: e.tensor_tensor(out=q16[:, :, 0:32], in0=qrB[:, :, 0:32], in1=qrC[:, :, 0:32], op=ALU.subtract), reads=[qrB.b, qrC.b], writes=[q16.b])
                          s.op("pool", lambda e, q16=q16: e.tensor_tensor(out=q16[:, :, 32:64], in0=qrB[:, :, 32:64], in1=qrC[:, :, 32:64], op=ALU.add), reads=[qrB.b, qrC.b], writes=[q16.b])
                          pt3 = ptr.get()
                          for h in range(8):
                              s.op("pe", lambda e, pt3=pt3, qno=qno, h=h: e.transpose(pt3[:, h * 128:(h + 1) * 128], qno[:, h, :], ident[:]),
                                   reads=[qno.b, ident.b], writes=[pt3.b])
                          qnst = qnstR.get()
                          evac(qnst[:], pt3[:].rearrange("p (a b) -> p a b", a=8), [pt3.b], [qnst.b])
                          q0 = (blk - (nblk - nown)) * 1024 + tt * 128
                          s.dma("sp", lambda e, q0=q0, qnst=qnst: DMA(e, out=qTn_d[:, :, q0:q0 + 128].rearrange("h d t -> d h t"), in_=qnst[:]), reads=[qnst.b])
                          pt4 = ptr.get()
                          for hp in range(4):
                              s.op("pe", lambda e, pt4=pt4, q16=q16, hp=hp: e.transpose(pt4[:, hp * 128:(hp + 1) * 128], q16[:, 2 * hp:2 * hp + 2, :].rearrange("p h d -> p (h d)"), ident[:]),
                                   reads=[q16.b, ident.b], writes=[pt4.b])
                          qrst = qrstR.get()
                          evac(qrst[:], pt4[:, 0:512].rearrange("p (a b) -> p a b", a=4), [pt4.b], [qrst.b])
                          s.dma("sp", lambda e, q0=q0, qrst=qrst: DMA(e, out=qTr_d[:, :, q0:q0 + 128].rearrange("h d t -> d h t"), in_=qrst[:]), reads=[qrst.b])
                  c0 = blk * 1024
                  s.dma("sp", lambda e, c0=c0: DMA(e, out=krT_d[:, c0:c0 + 1024], in_=krst[0:64, :]), reads=[krst.b])
              s.barrier()
              if stage == 1:
                  s.dma("sp", lambda e: DMA(e, out=dbg["kT"], in_=kT_d))
                  s.dma("sp", lambda e: DMA(e, out=dbg["krT"], in_=krT_d))
                  s.dma("sp", lambda e: DMA(e, out=dbg["V"], in_=V_d))
                  s.dma("sp", lambda e: DMA(e, out=dbg["qTn"], in_=qTn_d))
                  s.dma("sp", lambda e: DMA(e, out=dbg["qTr"], in_=qTr_d))
                  s.barrier()
        except Cut:
            pass

        if stage >= 2:
          with ExitStack() as p2:
            NKT = NT // 128
            base0 = (NT - NOWN) // 128
            SCALE = 192.0 ** -0.5
            kbias2 = sbt(p2, "kbias2", [128, 128], F32)
            krT2 = sbt(p2, "krT2", [128, NT], BF16)
            kTh = sbt(p2, "kTh", [128, NT], BF16)
            Vh = sbt(p2, "Vh", [128, NKT, 130], BF16)
            s.dma("sp", lambda e: DMA(e, out=kbias2[:], in_=kbias_in), writes=[kbias2.b])
            s.dma("sp", lambda e: DMA(e, out=krT2[0:64, :], in_=krT_d), writes=[krT2.b])
            s.dma("sp", lambda e: DMA(e, out=krT2[64:128, :], in_=krT_d), writes=[krT2.b])
            s.op("dve", lambda e: e.memset(Vh[:, :, 128:130], 1.0), writes=[Vh.b])
            qnR = Ring([sbt(p2, "qn_t%d" % i, [128, 512], BF16) for i in range(2)])
            qrR = Ring([sbt(p2, "qr_t%d" % i, [128, 512], BF16) for i in range(2)])
            pTR = Ring([sbt(p2, "pT%d" % i, [128, 512], BF16) for i in range(3)])
            pSR = Ring([pst(p2, "pS%d" % i, [128, 512], F32) for i in range(2)])
            pO = [pst(p2, "pO%d" % i, [128, 512], F32) for i in range(4)]
            ostR = Ring([sbt(p2, "ost%d" % i, [128, 128], BF16) for i in range(2)])
            rlR = Ring([sbt(p2, "rl%d" % i, [128, 1], F32) for i in range(2)])
            for h in range(8):
                hb = (h % 2) * 64
                s.dma("sp", lambda e, h=h: DMA(e, out=kTh[:], in_=kT_d[h]), writes=[kTh.b])
                for n0 in range(0, NKT, 16):
                    n1 = min(NKT, n0 + 16)
                    s.dma("sp", lambda e, h=h, n0=n0, n1=n1: DMA(e, out=Vh[:, n0:n1, 0:128], in_=V_d[n0 * 128:n1 * 128, h, :].rearrange("(n p) d -> p n d", p=128)),
                          writes=[Vh.b])
                for qg in range(NOWN // 512):
                    qn = qnR.get()
                    qr = qrR.get()
                    s.dma("sp", lambda e, h=h, qg=qg, qn=qn: DMA(e, out=qn[:], in_=qTn_d[h, :, qg * 512:(qg + 1) * 512]), writes=[qn.b])
                    s.dma("sp", lambda e, h=h, qg=qg, qr=qr: DMA(e, out=qr[:], in_=qTr_d[h // 2, :, qg * 512:(qg + 1) * 512]), writes=[qr.b])
                    dbase = base0 + 4 * qg
                    nk = dbase + 4
                    for kt in range(nk):
                        i = kt - dbase
                        c0 = 128 * i if i > 0 else 0
                        ps = pSR.get()
                        s.op("pe", lambda e, ps=ps, kt=kt, qn=qn, c0=c0: e.matmul(ps[:, c0:512], kTh[:, kt * 128:(kt + 1) * 128], qn[:, c0:512], start=True, stop=False),
                             reads=[kTh.b, qn.b], writes=[ps.b])
                        s.op("pe", lambda e, ps=ps, kt=kt, qr=qr, c0=c0, hb=hb: e.matmul(ps[:, c0:512], krT2[hb:hb + 64, kt * 128:(kt + 1) * 128], qr[hb:hb + 64, c0:512], start=False, stop=True),
                             reads=[krT2.b, qr.b], writes=[ps.b])
                        p = pTR.get()
                        s.op("act", lambda e, ps=ps, p=p, kt=kt, c0=c0: e.activation(out=p[:, c0:512], in_=ps[:, c0:512], func=AF.Exp, bias=kbias2[:, kt:kt + 1], scale=SCALE),
                             reads=[ps.b, kbias2.b], writes=[p.b])
                        if i >= 0:
                            s.op("dve", lambda e, p=p, c0=c0: e.memset(p[64:128, c0:c0 + 64], 0.0), writes=[p.b])
                        for qt in range(max(i, 0), 4):
                            s.op("pe", lambda e, p=p, qt=qt, kt=kt, dbase=dbase: e.matmul(pO[qt][:, 0:129], p[:, qt * 128:(qt + 1) * 128], Vh[:, kt, 0:129],
                                                                                           start=(kt == 0), stop=(kt == dbase + qt)),
                                 reads=[p.b, Vh.b], writes=[pO[qt].b])
                    for qt in range(4):
                        rl = rlR.get()
                        s.op("dve", lambda e, rl=rl, qt=qt: e.reciprocal(rl[:], pO[qt][:, 128:129]), reads=[pO[qt].b], writes=[rl.b])
                        o = ostR.get()
                        s.op("dve", lambda e, rl=rl, qt=qt, o=o: e.tensor_scalar(out=o[:], in0=pO[qt][:, 0:128], scalar1=rl[:, 0:1], scalar2=None, op0=ALU.mult),
                             reads=[pO[qt].b, rl.b], writes=[o.b])
                        t0_ = qg * 512 + qt * 128
                        s.dma("sp", lambda e, o=o, t0_=t0_, h=h: DMA(e, out=ymla_d[t0_:t0_ + 128, h * 128:(h + 1) * 128], in_=o[:]), reads=[o.b])
            s.barrier()
            if stage == 2:
                s.dma("sp", lambda e: DMA(e, out=dbg["ymla"], in_=ymla_d))
                s.barrier()

        if stage >= 3:
          with ExitStack() as p3:
            wU = sbt(p3, "wU", [128, 16, 1024], BF16)
            wG = sbt(p3, "wG", [128, 16, 1024], BF16)
            biasU = sbt(p3, "biasU", [1, 1024], BF16)
            biasG = sbt(p3, "biasG", [1, 1024], BF16)
            validS = sbt(p3, "validS", [128, 16], F32)
            s.dma("sp", lambda e: DMA(e, out=validS[:], in_=valid_in), writes=[validS.b])
            s.dma("pool", lambda e: DMA(e, out=wU[:], in_=w_in[:, 0:1024].rearrange("(k p) n -> p k n", p=128)), writes=[wU.b])
            s.dma("pool", lambda e: DMA(e, out=wG[:], in_=w_in[:, 1024:2048].rearrange("(k p) n -> p k n", p=128)), writes=[wG.b])
            pb3 = pst(p3, "pb3", [1, 512], F32)
            for (w_, b_) in ((wU, biasU), (wG, biasG)):
                for cg in range(2):
                    for k in range(16):
                        s.op("pe", lambda e, k=k, w_=w_, cg=cg: e.matmul(pb3[:, :], sh1c16[:, k:k + 1], w_[:, k, cg * 512:(cg + 1) * 512], start=(k == 0), stop=(k == 15)),
                             reads=[sh1c16.b, w_.b], writes=[pb3.b])
                    s.op("dve", lambda e, b_=b_, cg=cg: e.tensor_copy(b_[:, cg * 512:(cg + 1) * 512], pb3[:, :]), reads=[pb3.b], writes=[b_.b])
                for k in range(16):
                    s.op("dve", lambda e, k=k, w_=w_: e.tensor_scalar(out=w_[:, k, :], in0=w_[:, k, :], scalar1=a1c[:, k:k + 1], scalar2=None, op0=ALU.mult),
                         reads=[w_.b, a1c.b], writes=[w_.b])
            xpool_3 = Ring([sbt(p3, "xt3%d" % i, [128, D], F32) for i in range(2)])
            junk_3 = sbt(p3, "junk3", [128, D], BF16)
            xh_3 = Ring([sbt(p3, "xh3%d" % i, [128, D], BF16) for i in range(2)])
            xhT_3 = sbt(p3, "xhT3", [128, 16, 1024], BF16)
            ptr_3 = Ring([pst(p3, "ptr3%d" % i, [128, 1024], BF16) for i in range(2)])
            pacc_3 = Ring([pst(p3, "pa3%d" % i, [128, 512], F32) for i in range(4)])
            ssr_3 = Ring([sbt(p3, "ss3%d" % i, [128, 4], F32) for i in range(4)])
            Utok = sbt(p3, "Utok", [128, 64, 8, 16], BF16)
            Gblk = sbt(p3, "Gblk", [128, 8, 1024], BF16)
            UTs = Ring([sbt(p3, "UTs%d" % i, [128, 8, 128], BF16) for i in range(2)])
            ev3 = [0]

            def evac3(out_ap, in_ap, reads, writes):
                ev3[0] += 1
                if ev3[0] % 2:
                    s.op("dve", lambda e: e.tensor_copy(out_ap, in_ap), reads=reads, writes=writes)
                else:
                    s.op("act", lambda e: e.activation(out=out_ap, in_=in_ap, func=AF.Copy), reads=reads, writes=writes)

            for blk in range(nblk):
                for tt in range(8):
                    r0 = blk * 1024 + tt * 128
                    xt = xpool_3.get()
                    s.dma("sp", lambda e, xt=xt, r0=r0: DMA(e, out=xt[:], in_=xp[r0:r0 + 128, :]), writes=[xt.b])
                    ss = ssr_3.get()
                    s.op("act", lambda e, xt=xt, ss=ss: e.activation(out=junk_3[:], in_=xt[:], func=AF.Square, accum_out=ss[:, 0:1]), reads=[xt.b], writes=[junk_3.b, ss.b])
                    s.op("dve", lambda e, ss=ss: e.tensor_scalar(out=ss[:, 0:1], in0=ss[:, 0:1], scalar1=1.0 / D, scalar2=EPS, op0=ALU.mult, op1=ALU.add), reads=[ss.b], writes=[ss.b])
                    s.op("act", lambda e, ss=ss: e.activation(out=ss[:, 0:1], in_=ss[:, 0:1], func=AF.Sqrt), reads=[ss.b], writes=[ss.b])
                    s.op("dve", lambda e, ss=ss: e.reciprocal(ss[:, 0:1], ss[:, 0:1]), reads=[ss.b], writes=[ss.b])
                    xb = xh_3.get()
                    s.op("dve", lambda e, xt=xt, ss=ss, xb=xb: e.tensor_scalar(out=xb[:], in0=xt[:], scalar1=ss[:, 0:1], scalar2=None, op0=ALU.mult), reads=[xt.b, ss.b], writes=[xb.b])
                    for half in range(2):
                        pt = ptr_3.get()
                        for k8 in range(8):
                            k = half * 8 + k8
                            s.op("pe", lambda e, pt=pt, xb=xb, k8=k8, k=k: e.transpose(pt[:, k8 * 128:(k8 + 1) * 128], xb[:, k * 128:(k + 1) * 128], ident[:]),
                                 reads=[xb.b, ident.b], writes=[pt.b])
                        evac3(xhT_3[:, half * 8:(half + 1) * 8, tt * 128:(tt + 1) * 128], pt[:].rearrange("p (a b) -> p a b", a=8), [pt.b], [xhT_3.b])
                own = blk >= nblk - nown
                for tau in range(8):
                    for cg in range(2):
                        pa = pacc_3.get()
                        for k in range(16):
                            s.op("pe", lambda e, pa=pa, k=k, tau=tau, cg=cg: e.matmul(pa[:], xhT_3[:, k, tau::8], wU[:, k, cg * 512:(cg + 1) * 512], start=(k == 0), stop=False),
                                 reads=[xhT_3.b, wU.b], writes=[pa.b])
                        s.op("pe", lambda e, pa=pa, cg=cg: e.matmul(pa[:], ones_r[:], biasU[:, cg * 512:(cg + 1) * 512], start=False, stop=True), reads=[ones_r.b, biasU.b], writes=[pa.b])
                        s.op("dve", lambda e, pa=pa, tau=tau, cg=cg, blk=blk: e.tensor_scalar(out=Utok[:, cg * 32:(cg + 1) * 32, tau, :], in0=pa[:].rearrange("p (g i) -> p g i", i=16),
                                                                                         scalar1=validS[:, blk:blk + 1], scalar2=None, op0=ALU.mult),
                             reads=[pa.b, validS.b], writes=[Utok.b])
                        if own:
                            pg = pacc_3.get()
                            for k in range(16):
                                s.op("pe", lambda e, pg=pg, k=k, tau=tau, cg=cg: e.matmul(pg[:], xhT_3[:, k, tau::8], wG[:, k, cg * 512:(cg + 1) * 512], start=(k == 0), stop=False),
                                     reads=[xhT_3.b, wG.b], writes=[pg.b])
                            s.op("pe", lambda e, pg=pg, cg=cg: e.matmul(pg[:], ones_r[:], biasG[:, cg * 512:(cg + 1) * 512], start=False, stop=True), reads=[ones_r.b, biasG.b], writes=[pg.b])
                            s.op("act", lambda e, pg=pg, tau=tau, cg=cg: e.activation(out=Gblk[:, tau, cg * 512:(cg + 1) * 512], in_=pg[:], func=AF.Sigmoid), reads=[pg.b], writes=[Gblk.b])
                if own:
                    ob = blk - (nblk - nown)
                    s.dma("sp", lambda e, ob=ob: DMA(e, out=Gd[ob], in_=Gblk[:]), reads=[Gblk.b])
                for g8 in range(8):
                    pt = ptr_3.get()
                    for gg in range(8):
                        g = g8 * 8 + gg
                        s.op("pe", lambda e, pt=pt, gg=gg, g=g: e.transpose(pt[:, gg * 128:(gg + 1) * 128], Utok[:, g, :, :].rearrange("p t i -> p (t i)"), ident[:]),
                             reads=[Utok.b, ident.b], writes=[pt.b])
                    ut = UTs.get()
                    evac3(ut[:], pt[:].rearrange("p (a b) -> p a b", a=8), [pt.b], [ut.b])
                    s.dma("sp", lambda e, ut=ut, g8=g8, blk=blk: DMA(e, out=Ud[g8 * 8:(g8 + 1) * 8, :, blk * 128:(blk + 1) * 128].rearrange("g s c -> s g c"), in_=ut[:]), reads=[ut.b])
            s.barrier()

        if stage >= 3:
          with ExitStack() as p4:
            G = 64
            NOB = NOWN // 1024
            LV = max(1, int(math.ceil(math.log2(NCH))))

            def T4(name, shape, dt=F32):
                return sbt(p4, name, shape, dt)

            def V(fn, reads, writes):
                s.op("dve", fn, reads=[r.b for r in reads], writes=[w.b for w in writes])

            Ar = T4("Ar", [128, G]); Ai = T4("Ai", [128, G]); dtt = T4("dtt", [128, G])
            for half in range(2):
                s.dma("sp", lambda e, half=half: DMA(e, out=Ar[half * 64:(half + 1) * 64, :], in_=ssm_A_re.rearrange("g p -> p g")), writes=[Ar.b])
                s.dma("sp", lambda e, half=half: DMA(e, out=Ai[half * 64:(half + 1) * 64, :], in_=ssm_A_im.rearrange("g p -> p g")), writes=[Ai.b])
            s.dma("sp", lambda e: DMA(e, out=dtt[:], in_=ssm_log_dt.partition_broadcast(128)), writes=[dtt.b])
            s.op("act", lambda e: e.activation(out=dtt[:], in_=dtt[:], func=AF.Exp), reads=[dtt.b], writes=[dtt.b])
            dAr = T4("dAr", [128, G]); dAi = T4("dAi", [128, G])
            V(lambda e: e.tensor_tensor(out=dAr[:], in0=Ar[:], in1=dtt[:], op=ALU.mult), [Ar, dtt], [dAr])
            V(lambda e: e.tensor_tensor(out=dAi[:], in0=Ai[:], in1=dtt[:], op=ALU.mult), [Ai, dtt], [dAi])
            pidx = T4("pidx", [128, 1], I32); pf = T4("pf", [128, 1])
            m1 = T4("m1", [128, 1]); m2 = T4("m2", [128, 1]); nm1 = T4("nm1", [128, 1]); nm2 = T4("nm2", [128, 1]); sg = T4("sg", [128, 1])
            s.op("pool", lambda e: e.iota(pidx[:], [[0, 1]], base=0, channel_multiplier=1), writes=[pidx.b])
            V(lambda e: e.tensor_copy(pf[:], pidx[:]), [pidx], [pf])
            V(lambda e: e.tensor_scalar(out=m1[:], in0=pf[:], scalar1=64.0, scalar2=None, op0=ALU.is_lt), [pf], [m1])
            V(lambda e: e.tensor_scalar(out=m2[:], in0=m1[:], scalar1=-1.0, scalar2=1.0, op0=ALU.mult, op1=ALU.add), [m1], [m2])
            V(lambda e: e.tensor_scalar(out=nm1[:], in0=m1[:], scalar1=-1.0, scalar2=None, op0=ALU.mult), [m1], [nm1])
            V(lambda e: e.tensor_scalar(out=nm2[:], in0=m2[:], scalar1=-1.0, scalar2=None, op0=ALU.mult), [m2], [nm2])
            V(lambda e: e.tensor_scalar(out=sg[:], in0=m2[:], scalar1=2.0, scalar2=-1.0, op0=ALU.mult, op1=ALU.add), [m2], [sg])
            LRE = T4("LRE", [128, 16, G]); LIM = T4("LIM", [128, 16, G])
            ptmp = ExitStack()
            ANG = sbt(ptmp, "ANG", [128, 16, G], F32); KF = sbt(ptmp, "KF", [128, 16, G], F32); KI = sbt(ptmp, "KI", [128, 16, G], I32)
            MAG = sbt(ptmp, "MAG", [128, 16, G], F32)
            for idx in range(16):
                k = float(idx - 7)
                V(lambda e, idx=idx, k=k: e.tensor_scalar(out=ANG[:, idx, :], in0=dAi[:], scalar1=k, scalar2=None, op0=ALU.mult), [dAi], [ANG])
                s.op("act", lambda e, idx=idx, k=k: e.activation(out=MAG[:, idx, :], in_=dAr[:], func=AF.Exp, scale=k), reads=[dAr.b], writes=[MAG.b])

            def wrap2(t, kf):
                V(lambda e: e.tensor_scalar(out=kf[:], in0=t[:], scalar1=math.pi, scalar2=-TWO_PI, op0=ALU.is_gt, op1=ALU.mult), [t], [kf])
                V(lambda e: e.tensor_tensor(out=t[:], in0=t[:], in1=kf[:], op=ALU.add), [t, kf], [t])
                V(lambda e: e.tensor_scalar(out=kf[:], in0=t[:], scalar1=-math.pi, scalar2=TWO_PI, op0=ALU.is_lt, op1=ALU.mult), [t], [kf])
                V(lambda e: e.tensor_tensor(out=t[:], in0=t[:], in1=kf[:], op=ALU.add), [t, kf], [t])

            V(lambda e: e.tensor_scalar(out=KF[:], in0=ANG[:], scalar1=1.0 / TWO_PI, scalar2=None, op0=ALU.mult), [ANG], [KF])
            V(lambda e: e.tensor_copy(KI[:], KF[:]), [KF], [KI])
            V(lambda e: e.tensor_copy(KF[:], KI[:]), [KI], [KF])
            V(lambda e: e.scalar_tensor_tensor(out=ANG[:], in0=KF[:], scalar=-CW1, in1=ANG[:], op0=ALU.mult, op1=ALU.add), [KF, ANG], [ANG])
            V(lambda e: e.scalar_tensor_tensor(out=ANG[:], in0=KF[:], scalar=-CW2, in1=ANG[:], op0=ALU.mult, op1=ALU.add), [KF, ANG], [ANG])
            wrap2(ANG, KF)
            s.op("act", lambda e: e.activation(out=LIM[:], in_=ANG[:], func=AF.Sin), reads=[ANG.b], writes=[LIM.b])
            V(lambda e: e.tensor_scalar(out=ANG[:], in0=ANG[:], scalar1=math.pi / 2, scalar2=None, op0=ALU.add), [ANG], [ANG])
            wrap2(ANG, KF)
            s.op("act", lambda e: e.activation(out=LRE[:], in_=ANG[:], func=AF.Sin), reads=[ANG.b], writes=[LRE.b])
            V(lambda e: e.tensor_tensor(out=LRE[:], in0=LRE[:], in1=MAG[:], op=ALU.mult), [LRE, MAG], [LRE])
            V(lambda e: e.tensor_tensor(out=LIM[:], in0=LIM[:], in1=MAG[:], op=ALU.mult), [LIM, MAG], [LIM])
            s.barrier()
            ptmp.close()
            aK = T4("aK", [128, LV, G]); bK = T4("bK", [128, LV, G]); bKs = T4("bKs", [128, LV, G]); tq1 = T4("tq1", [128, G]); tq2 = T4("tq2", [128, G])
            V(lambda e: e.tensor_copy(aK[:, 0, :], LRE[:, 15, :]), [LRE], [aK])
            V(lambda e: e.tensor_copy(bK[:, 0, :], LIM[:, 15, :]), [LIM], [bK])
            for l in range(1, LV):
                V(lambda e, l=l: e.tensor_tensor(out=tq1[:], in0=aK[:, l - 1, :], in1=aK[:, l - 1, :], op=ALU.mult), [aK], [tq1])
                V(lambda e, l=l: e.tensor_tensor(out=tq2[:], in0=bK[:, l - 1, :], in1=bK[:, l - 1, :], op=ALU.mult), [bK], [tq2])
                V(lambda e, l=l: e.tensor_tensor(out=aK[:, l, :], in0=tq1[:], in1=tq2[:], op=ALU.subtract), [tq1, tq2], [aK])
                V(lambda e, l=l: e.scalar_tensor_tensor(out=bK[:, l, :], in0=aK[:, l - 1, :], scalar=2.0, in1=bK[:, l - 1, :], op0=ALU.mult, op1=ALU.mult), [aK, bK], [bK])
            V(lambda e: e.tensor_scalar(out=bKs[:], in0=bK[:], scalar1=sg[:, 0:1], scalar2=None, op0=ALU.mult), [bK, sg], [bKs])
            kre = T4("kre", [128, G]); kim = T4("kim", [128, G]); den = T4("den", [128, G]); nr = T4("nr", [128, G])
            V(lambda e: e.tensor_scalar(out=nr[:], in0=LRE[:, 8, :], scalar1=-1.0, scalar2=None, op0=ALU.add), [LRE], [nr])
            V(lambda e: e.tensor_tensor(out=den[:], in0=Ar[:], in1=Ar[:], op=ALU.mult), [Ar], [den])
            V(lambda e: e.tensor_tensor(out=tq1[:], in0=Ai[:], in1=Ai[:], op=ALU.mult), [Ai], [tq1])
            V(lambda e: e.tensor_tensor(out=den[:], in0=den[:], in1=tq1[:], op=ALU.add), [den, tq1], [den])
            V(lambda e: e.reciprocal(den[:], den[:]), [den], [den])
            V(lambda e: e.tensor_tensor(out=kre[:], in0=nr[:], in1=Ar[:], op=ALU.mult), [nr, Ar], [kre])
            V(lambda e: e.tensor_tensor(out=tq1[:], in0=LIM[:, 8, :], in1=Ai[:], op=ALU.mult), [LIM, Ai], [tq1])
            V(lambda e: e.tensor_tensor(out=kre[:], in0=kre[:], in1=tq1[:], op=ALU.add), [kre, tq1], [kre])
            V(lambda e: e.tensor_tensor(out=kre[:], in0=kre[:], in1=den[:], op=ALU.mult), [kre, den], [kre])
            V(lambda e: e.tensor_tensor(out=kim[:], in0=LIM[:, 8, :], in1=Ar[:], op=ALU.mult), [LIM, Ar], [kim])
            V(lambda e: e.tensor_tensor(out=tq1[:], in0=nr[:], in1=Ai[:], op=ALU.mult), [nr, Ai], [tq1])
            V(lambda e: e.tensor_tensor(out=kim[:], in0=kim[:], in1=tq1[:], op=ALU.subtract), [kim, tq1], [kim])
            V(lambda e: e.tensor_tensor(out=kim[:], in0=kim[:], in1=den[:], op=ALU.mult), [kim, den], [kim])
            P_all = T4("P_all", [128, G, 128], BF16); T_all = T4("T_all", [128, G, 128], BF16); Q_all = T4("Q_all", [128, G, 128], BF16)
            Sw = T4("Sw", [128, 128], F32)
            pset = ExitStack()
            Bre = sbt(pset, "Bre", [128, G, 16], F32); Bim = sbt(pset, "Bim", [128, G, 16], F32)
            for half in range(2):
                s.dma("sp", lambda e, half=half: DMA(e, out=Bre[half * 64:(half + 1) * 64, :, :], in_=ssm_B_re.rearrange("g p i -> p g i")), writes=[Bre.b])
                s.dma("sp", lambda e, half=half: DMA(e, out=Bim[half * 64:(half + 1) * 64, :, :], in_=ssm_B_im.rearrange("g p i -> p g i")), writes=[Bim.b])
            Bbr = sbt(pset, "Bbr", [128, G, 16], F32); Bbi = sbt(pset, "Bbi", [128, G, 16], F32); tb = sbt(pset, "tb", [128, G, 16], F32)
            kreb = kre[:, :].unsqueeze(2).to_broadcast([128, G, 16]); kimb = kim[:, :].unsqueeze(2).to_broadcast([128, G, 16])
            V(lambda e: e.tensor_tensor(out=Bbr[:], in0=Bre[:], in1=kreb, op=ALU.mult), [Bre, kre], [Bbr])
            V(lambda e: e.tensor_tensor(out=tb[:], in0=Bim[:], in1=kimb, op=ALU.mult), [Bim, kim], [tb])
            V(lambda e: e.tensor_tensor(out=Bbr[:], in0=Bbr[:], in1=tb[:], op=ALU.subtract), [Bbr, tb], [Bbr])
            V(lambda e: e.tensor_tensor(out=Bbi[:], in0=Bim[:], in1=kreb, op=ALU.mult), [Bim, kre], [Bbi])
            V(lambda e: e.tensor_tensor(out=tb[:], in0=Bre[:], in1=kimb, op=ALU.mult), [Bre, kim], [tb])
            V(lambda e: e.tensor_tensor(out=Bbi[:], in0=Bbi[:], in1=tb[:], op=ALU.add), [Bbi, tb], [Bbi])
            X1 = Bre; X2 = Bim
            V(lambda e: e.tensor_scalar(out=X1[:], in0=Bbr[:], scalar1=m1[:, 0:1], scalar2=None, op0=ALU.mult), [Bbr, m1], [X1])
            V(lambda e: e.scalar_tensor_tensor(out=X1[:], in0=Bbi[:], scalar=m2[:, 0:1], in1=X1[:], op0=ALU.mult, op1=ALU.add), [Bbi, m2, X1], [X1])
            V(lambda e: e.tensor_scalar(out=X2[:], in0=Bbr[:], scalar1=m2[:, 0:1], scalar2=None, op0=ALU.mult), [Bbr, m2], [X2])
            V(lambda e: e.scalar_tensor_tensor(out=X2[:], in0=Bbi[:], scalar=nm1[:, 0:1], in1=X2[:], op0=ALU.mult, op1=ALU.add), [Bbi, nm1, X2], [X2])
            PS = sbt(pset, "PS", [128, G, 8, 16], F32)
            PhS = sbt(pset, "PhS", [128, G, 8, 16], F32)
            for tau in range(8):
                for (dst, idx) in ((PS, 7 - tau + 7), (PhS, -tau + 7)):
                    lre = LRE[:, idx, :].unsqueeze(2).to_broadcast([128, G, 16]); lim = LIM[:, idx, :].unsqueeze(2).to_broadcast([128, G, 16])
                    V(lambda e, dst=dst, tau=tau, lre=lre: e.tensor_tensor(out=dst[:, :, tau, :], in0=X1[:], in1=lre, op=ALU.mult), [X1, LRE], [dst])
                    V(lambda e, lim=lim: e.tensor_tensor(out=tb[:], in0=X2[:], in1=lim, op=ALU.mult), [X2, LIM], [tb])
                    V(lambda e, dst=dst, tau=tau: e.tensor_tensor(out=dst[:, :, tau, :], in0=dst[:, :, tau, :], in1=tb[:], op=ALU.add), [dst, tb], [dst])
            Cre = sbt(pset, "Cre", [128, G, 16], F32); Cim = sbt(pset, "Cim", [128, G, 16], F32)
            ctile = Ring([sbt(pset, "ctile%d" % i, [128, 128], F32) for i in range(2)])
            pS4 = Ring([pst(p4, "pS4%d" % i, [128, 512], F32) for i in range(4)])
            for (csrc, cdst) in ((ssm_C_re, Cre), (ssm_C_im, Cim)):
                cflat = csrc.rearrange("g o p -> (g o) p")
                for r in range(8):
                    ct = ctile.get()
                    s.dma("sp", lambda e, ct=ct, r=r, cflat=cflat: DMA(e, out=ct[:, 0:64], in_=cflat[r * 128:(r + 1) * 128, :]), writes=[ct.b])
                    s.dma("sp", lambda e, ct=ct, r=r, cflat=cflat: DMA(e, out=ct[:, 64:128], in_=cflat[r * 128:(r + 1) * 128, :]), writes=[ct.b])
                    pp = pS4.get()
                    s.op("pe", lambda e, pp=pp, ct=ct: e.transpose(pp[:, 0:128], ct[:], ident_f[:]), reads=[ct.b, ident_f.b], writes=[pp.b])
                    V(lambda e, pp=pp, cdst=cdst, r=r: e.tensor_copy(cdst[:, r * 8:(r + 1) * 8, :].rearrange("p g o -> p (g o)"), pp[:, 0:128]), [pp], [cdst])
            Y1 = Bbr; Y2 = Bbi
            V(lambda e: e.tensor_scalar(out=Y1[:], in0=Cre[:], scalar1=m1[:, 0:1], scalar2=None, op0=ALU.mult), [Cre, m1], [Y1])
            V(lambda e: e.scalar_tensor_tensor(out=Y1[:], in0=Cim[:], scalar=nm2[:, 0:1], in1=Y1[:], op0=ALU.mult, op1=ALU.add), [Cim, nm2, Y1], [Y1])
            V(lambda e: e.tensor_scalar(out=Y2[:], in0=Cim[:], scalar1=nm1[:, 0:1], scalar2=None, op0=ALU.mult), [Cim, nm1], [Y2])
            V(lambda e: e.scalar_tensor_tensor(out=Y2[:], in0=Cre[:], scalar=nm2[:, 0:1], in1=Y2[:], op0=ALU.mult, op1=ALU.add), [Cre, nm2, Y2], [Y2])
            QhS = sbt(pset, "QhS", [128, G, 9, 16], F32)
            for tp in range(9):
                idx = tp + 7
                lre = LRE[:, idx, :].unsqueeze(2).to_broadcast([128, G, 16]); lim = LIM[:, idx, :].unsqueeze(2).to_broadcast([128, G, 16])
                V(lambda e, tp=tp, lre=lre: e.tensor_tensor(out=QhS[:, :, tp, :], in0=Y1[:], in1=lre, op=ALU.mult), [Y1, LRE], [QhS])
                V(lambda e, lim=lim: e.tensor_tensor(out=tb[:], in0=Y2[:], in1=lim, op=ALU.mult), [Y2, LIM], [tb])
                V(lambda e, tp=tp: e.tensor_tensor(out=QhS[:, :, tp, :], in0=QhS[:, :, tp, :], in1=tb[:], op=ALU.add), [QhS, tb], [QhS])
            V(lambda e: e.tensor_copy(Q_all[:].rearrange("p g (t o) -> p g t o", o=16), QhS[:, :, 1:9, :]), [QhS], [Q_all])
            mki = sbt(pset, "mki", [128, 8, 16], I32); Mk = sbt(pset, "Mk", [128, 128], F32); Dd = sbt(pset, "Dd", [128, G], F32); tT = sbt(pset, "tT", [128, 128], F32)
            s.op("pool", lambda e: e.iota(mki[:], [[16, 8], [0, 16]], base=0, channel_multiplier=-1), writes=[mki.b])
            V(lambda e: e.tensor_copy(Mk[:].rearrange("p (t o) -> p t o", o=16), mki[:]), [mki], [Mk])
            V(lambda e: e.tensor_scalar(out=Mk[:], in0=Mk[:], scalar1=-15.0, scalar2=None, op0=ALU.is_ge), [Mk], [Mk])
            for tau in range(8):
                s.dma("sp", lambda e, tau=tau: DMA(e, out=Dd[tau * 16:(tau + 1) * 16, :], in_=ssm_D.rearrange("(g i) -> i g", i=16)), writes=[Dd.b])
            V(lambda e: e.tensor_copy(Sw[:, 0:64], ident_f[:, 64:128]), [ident_f], [Sw])
            V(lambda e: e.tensor_copy(Sw[:, 64:128], ident_f[:, 0:64]), [ident_f], [Sw])
            for g in range(G):
                pp = pS4.get()
                s.op("pe", lambda e, pp=pp, g=g: e.transpose(pp[:, 0:128], PS[:, g, :, :].rearrange("p t i -> p (t i)"), ident_f[:]), reads=[PS.b, ident_f.b], writes=[pp.b])
                V(lambda e, pp=pp, g=g: e.tensor_copy(P_all[:, g, :], pp[:, 0:128]), [pp], [P_all])
                pq = pS4.get()
                s.op("pe", lambda e, pq=pq, g=g: e.matmul(pq[:, 0:128], PhS[:, g, :, :].rearrange("p t i -> p (t i)"), QhS[:, g, 0:8, :].rearrange("p t o -> p (t o)"), start=True, stop=True),
                     reads=[PhS.b, QhS.b], writes=[pq.b])
                V(lambda e, pq=pq: e.tensor_tensor(out=tT[:], in0=pq[:, 0:128], in1=Mk[:], op=ALU.mult), [pq, Mk], [tT])
                V(lambda e, g=g: e.scalar_tensor_tensor(out=T_all[:, g, :], in0=ident_f[:], scalar=Dd[:, g:g + 1], in1=tT[:], op0=ALU.mult, op1=ALU.add), [ident_f, Dd, tT], [T_all])
            s.barrier()
            pset.close()
            Ug = Ring([T4("Ug%d" % i, [128, NCH], BF16) for i in range(2)])
            XA = T4("XA", [128, NCH], F32); XB = T4("XB", [128, NCH], F32)
            Xb = T4("Xb", [128, NCH + 2], BF16)
            Ybuf = T4("Ybuf", [128, NOB, 8, 1024], BF16)
            V(lambda e: e.memset(Xb[:, 0:2], 0.0), [], [Xb])
            cown = NCH - NOB * 128
            for g in range(G):
                u = Ug.get()
                s.dma("sp", lambda e, u=u, g=g: DMA(e, out=u[:], in_=Ud[g]), writes=[u.b])
                for n0 in range(0, NCH, 512):
                    n1 = min(NCH, n0 + 512)
                    pp = pS4.get()
                    s.op("pe", lambda e, pp=pp, u=u, g=g, n0=n0, n1=n1: e.matmul(pp[:, 0:n1 - n0], P_all[:, g, :], u[:, n0:n1], start=True, stop=True),
                         reads=[P_all.b, u.b], writes=[pp.b])
                    V(lambda e, pp=pp, n0=n0, n1=n1: e.tensor_copy(XA[:, n0:n1], pp[:, 0:n1 - n0]), [pp], [XA])
                cur, nxt = XA, XB
                for l in range(LV):
                    sh = 1 << l
                    if sh >= NCH:
                        break
                    V(lambda e, cur=cur, nxt=nxt, sh=sh: e.tensor_copy(nxt[:, 0:sh], cur[:, 0:sh]), [cur], [nxt])
                    for n0 in range(0, NCH - sh, 512):
                        n1 = min(NCH - sh, n0 + 512)
                        pp = pS4.get()
                        s.op("pe", lambda e, pp=pp, cur=cur, n0=n0, n1=n1: e.matmul(pp[:, 0:n1 - n0], Sw[:], cur[:, n0:n1], start=True, stop=True),
                             reads=[Sw.b, cur.b], writes=[pp.b])
                        V(lambda e, pp=pp, cur=cur, nxt=nxt, n0=n0, n1=n1, sh=sh, l=l, g=g: e.scalar_tensor_tensor(out=nxt[:, n0 + sh:n1 + sh], in0=pp[:, 0:n1 - n0], scalar=bKs[:, l, g:g + 1],
                                                                                                          in1=cur[:, n0 + sh:n1 + sh], op0=ALU.mult, op1=ALU.add),
                          [pp, bKs, cur], [nxt])
                        V(lambda e, cur=cur, nxt=nxt, n0=n0, n1=n1, sh=sh, l=l, g=g: e.scalar_tensor_tensor(out=nxt[:, n0 + sh:n1 + sh], in0=cur[:, n0:n1], scalar=aK[:, l, g:g + 1],
                                                                                                   in1=nxt[:, n0 + sh:n1 + sh], op0=ALU.mult, op1=ALU.add),
                          [cur, aK, nxt], [nxt])
                    cur, nxt = nxt, cur
                V(lambda e, cur=cur: e.tensor_copy(Xb[:, 1:NCH + 1], cur[:, :]), [cur], [Xb])
                for ob in range(NOB):
                    c0 = cown + ob * 128
                    pp = pS4.get()
                    s.op("pe", lambda e, pp=pp, u=u, g=g, c0=c0: e.matmul(pp[:, 0:128], u[:, c0:c0 + 128], T_all[:, g, :], start=True, stop=False), reads=[u.b, T_all.b], writes=[pp.b])
                    s.op("pe", lambda e, pp=pp, g=g, c0=c0: e.matmul(pp[:, 0:128], Xb[:, c0:c0 + 128], Q_all[:, g, :], start=False, stop=True), reads=[Xb.b, Q_all.b], writes=[pp.b])
                    s.op("act", lambda e, pp=pp, g=g, ob=ob: e.activation(out=Ybuf[:, ob, :, g * 16:(g + 1) * 16], in_=pp[:, 0:128].rearrange("p (t o) -> p t o", o=16), func=AF.Gelu),
                         reads=[pp.b], writes=[Ybuf.b])
            Gt = T4("Gt", [128, 8, 1024], BF16)
            for ob in range(NOB):
                s.dma("sp", lambda e, ob=ob: DMA(e, out=Gt[:], in_=Gd[ob]), writes=[Gt.b])
                V(lambda e, ob=ob: e.tensor_tensor(out=Ybuf[:, ob, :, :], in0=Ybuf[:, ob, :, :], in1=Gt[:], op=ALU.mult), [Ybuf, Gt], [Ybuf])
                s.dma("sp", lambda e, ob=ob: DMA(e, out=ys5_d[ob * 1024:(ob + 1) * 1024, :].rearrange("(c t) ch -> c t ch", t=8), in_=Ybuf[:, ob, :, :]), reads=[Ybuf.b])
            s.barrier()
            if stage == 3:
                s.dma("sp", lambda e: DMA(e, out=dbg["ys5"], in_=ys5_d))
                s.barrier()

        if stage >= 4:
          with ExitStack() as p5:
            def T5(name, shape, dt=F32):
                return sbt(p5, name, shape, dt)
            g1row = T5("g1row", [128, D]); sh2row = T5("sh2row", [128, D]); a2row = T5("a2row", [128, D]); g2row = T5("g2row", [128, D])
            rows = [g1row, sh2row, a2row, g2row]
            with ExitStack() as p5a:
                cT5 = sbt(p5a, "cT5", [128, 16], F32)
                cact5 = sbt(p5a, "cact5", [128, 16], BF16)
                cB = sbt(p5a, "cB", [128, 16, 128], BF16)
                gfrow = sbt(p5a, "gfrow", [128, D], F32)
                s.dma("sp", lambda e: DMA(e, out=cT5[:], in_=c_in.rearrange("(k p) -> p k", p=128)), writes=[cT5.b])
                s.op("act", lambda e: e.activation(out=cact5[:], in_=cT5[:], func=AF.Silu), reads=[cT5.b], writes=[cact5.b])
                for k in range(16):
                    s.op("dve", lambda e, k=k: e.tensor_copy(cB[:, k, :], cact5[:, k:k + 1].to_broadcast([128, 128])), reads=[cact5.b], writes=[cB.b])
                wa5 = Ring([sbt(p5a, "wa5%d" % i, [128, 16, 512], BF16) for i in range(2)])
                pr5 = Ring([pst(p5a, "pr5%d" % i, [128, 512], F32) for i in range(2)])
                for ci, row in enumerate(rows):
                    chunk = 2 + ci
                    s.dma("sp", lambda e, row=row, chunk=chunk: DMA(e, out=row[:], in_=b_ada[chunk * D:(chunk + 1) * D].partition_broadcast(128)), writes=[row.b])
                    for j in range(4):
                        w = wa5.get()
                        c0 = chunk * D + j * 512
                        s.dma("pool", lambda e, w=w, c0=c0: DMA(e, out=w[:], in_=w_ada[:, c0:c0 + 512].rearrange("(k p) n -> p k n", p=128)), writes=[w.b])
                        pr = pr5.get()
                        for k in range(16):
                            s.op("pe", lambda e, pr=pr, w=w, k=k: e.matmul(pr[:], cB[:, k, :], w[:, k, :], start=(k == 0), stop=(k == 15)), reads=[cB.b, w.b], writes=[pr.b])
                        s.op("dve", lambda e, pr=pr, row=row, j=j: e.tensor_tensor(out=row[:, j * 512:(j + 1) * 512], in0=pr[:], in1=row[:, j * 512:(j + 1) * 512], op=ALU.add),
                             reads=[pr.b, row.b], writes=[row.b])
                s.dma("sp", lambda e: DMA(e, out=gfrow[:], in_=norm_ffn_g.partition_broadcast(128)), writes=[gfrow.b])
                s.op("dve", lambda e: e.scalar_tensor_tensor(out=a2row[:], in0=a2row[:], scalar=1.0, in1=gfrow[:], op0=ALU.add, op1=ALU.mult), reads=[a2row.b, gfrow.b], writes=[a2row.b])
                s.barrier()
            with ExitStack() as p5b:
                wo = sbt(p5b, "wo", [128, 16, D], BF16)
                gsc = sbt(p5b, "gsc", [128, 16], F32)
                s.dma("pool", lambda e: DMA(e, out=wo[:], in_=w_out.rearrange("(k p) n -> p k n", p=128)), writes=[wo.b])
                s.dma("sp", lambda e: DMA(e, out=gsc[:, 0:8], in_=out_ssm_g.rearrange("(k p) -> p k", p=128)), writes=[gsc.b])
                s.dma("sp", lambda e: DMA(e, out=gsc[:, 8:16], in_=out_mla_g.rearrange("(k p) -> p k", p=128)), writes=[gsc.b])
                for k in range(16):
                    s.op("dve", lambda e, k=k: e.scalar_tensor_tensor(out=wo[:, k, :], in0=wo[:, k, :], scalar=gsc[:, k:k + 1], in1=g1row[:], op0=ALU.mult, op1=ALU.mult),
                         reads=[wo.b, gsc.b, g1row.b], writes=[wo.b])
                ymR = Ring([sbt(p5b, "ym%d" % i, [128, 2, 1024], BF16) for i in range(2)])
                junk5 = sbt(p5b, "junk5", [128, 1024], BF16)
                ss5R = Ring([sbt(p5b, "ss5%d" % i, [128, 2], F32) for i in range(2)])
                mixT = Ring([sbt(p5b, "mixT%d" % i, [128, 16, 128], BF16) for i in range(2)])
                xR = Ring([sbt(p5b, "x5%d" % i, [128, D], F32) for i in range(2)])
                x1R = Ring([sbt(p5b, "x15%d" % i, [128, D], F32) for i in range(2)])
                ptr5 = Ring([pst(p5b, "ptr5%d" % i, [128, 1024], BF16) for i in range(2)])
                pa5 = Ring([pst(p5b, "pa5%d" % i, [128, 512], F32) for i in range(4)])
                for ti in range(NOWN // 128):
                    ym = ymR.get()
                    s.dma("sp", lambda e, ym=ym, ti=ti: DMA(e, out=ym[:, 0, :], in_=ys5_d[ti * 128:(ti + 1) * 128, :]), writes=[ym.b])
                    s.dma("sp", lambda e, ym=ym, ti=ti: DMA(e, out=ym[:, 1, :], in_=ymla_d[ti * 128:(ti + 1) * 128, :]), writes=[ym.b])
                    xt = xR.get()
                    r0 = NT - NOWN + ti * 128
                    s.dma("sp", lambda e, xt=xt, r0=r0: DMA(e, out=xt[:], in_=xp[r0:r0 + 128, :]), writes=[xt.b])
                    ss = ss5R.get()
                    for hh in range(2):
                        s.op("act", lambda e, ym=ym, ss=ss, hh=hh: e.activation(out=junk5[:], in_=ym[:, hh, :], func=AF.Square, accum_out=ss[:, hh:hh + 1]),
                             reads=[ym.b], writes=[junk5.b, ss.b])
                    s.op("dve", lambda e, ss=ss: e.tensor_scalar(out=ss[:], in0=ss[:], scalar1=1.0 / 1024, scalar2=EPS, op0=ALU.mult, op1=ALU.add), reads=[ss.b], writes=[ss.b])
                    s.op("act", lambda e, ss=ss: e.activation(out=ss[:], in_=ss[:], func=AF.Sqrt), reads=[ss.b], writes=[ss.b])
                    s.op("dve", lambda e, ss=ss: e.reciprocal(ss[:], ss[:]), reads=[ss.b], writes=[ss.b])
                    mt = mixT.get()
                    for hh in range(2):
                        pt = ptr5.get()
                        for k8 in range(8):
                            s.op("pe", lambda e, pt=pt, ym=ym, hh=hh, k8=k8: e.transpose(pt[:, k8 * 128:(k8 + 1) * 128], ym[:, hh, k8 * 128:(k8 + 1) * 128], ident[:]),
                                 reads=[ym.b, ident.b], writes=[pt.b])
                        s.op("dve", lambda e, pt=pt, mt=mt, hh=hh: e.tensor_copy(mt[:, hh * 8:(hh + 1) * 8, :], pt[:].rearrange("p (a b) -> p a b", a=8)), reads=[pt.b], writes=[mt.b])
                    x1 = x1R.get()
                    for cg in range(4):
                        pS_ = pa5.get(); pM_ = pa5.get()
                        for k in range(8):
                            s.op("pe", lambda e, pS_=pS_, mt=mt, k=k, cg=cg: e.matmul(pS_[:], mt[:, k, :], wo[:, k, cg * 512:(cg + 1) * 512], start=(k == 0), stop=(k == 7)),
                                 reads=[mt.b, wo.b], writes=[pS_.b])
                        for k in range(8, 16):
                            s.op("pe", lambda e, pM_=pM_, mt=mt, k=k, cg=cg: e.matmul(pM_[:], mt[:, k, :], wo[:, k, cg * 512:(cg + 1) * 512], start=(k == 8), stop=(k == 15)),
                                 reads=[mt.b, wo.b], writes=[pM_.b])
                        s.op("dve", lambda e, pS_=pS_, ss=ss, xt=xt, x1=x1, cg=cg: e.scalar_tensor_tensor(out=x1[:, cg * 512:(cg + 1) * 512], in0=pS_[:], scalar=ss[:, 0:1],
                                                                                                       in1=xt[:, cg * 512:(cg + 1) * 512], op0=ALU.mult, op1=ALU.add),
                             reads=[pS_.b, ss.b, xt.b], writes=[x1.b])
                        s.op("dve", lambda e, pM_=pM_, ss=ss, x1=x1, cg=cg: e.scalar_tensor_tensor(out=x1[:, cg * 512:(cg + 1) * 512], in0=pM_[:], scalar=ss[:, 1:2],
                                                                                               in1=x1[:, cg * 512:(cg + 1) * 512], op0=ALU.mult, op1=ALU.add),
                             reads=[pM_.b, ss.b, x1.b], writes=[x1.b])
                    s.dma("sp", lambda e, x1=x1, ti=ti: DMA(e, out=x1_d[ti * 128:(ti + 1) * 128, :], in_=x1[:]), reads=[x1.b])
                s.barrier()
                if stage == 4:
                    s.dma("sp", lambda e: DMA(e, out=dbg["x1"], in_=x1_d))
                    s.barrier()
            if stage >= 5:
              CAP = moe_cap
              HC = min(CAP, 1024)
              NHF = CAP // HC
              NTL = HC // 128
              xg_d = nc.dram_tensor("xg_d", [NE * CAP, D], BF16).ap()
              yg_d = nc.dram_tensor("yg_d", [NE * CAP, D], BF16).ap()
              NTI = NOWN // 128
              with ExitStack() as p5c:
                destA = sbt(p5c, "destA", [128, NTI, 4], I32)
                gateA = sbt(p5c, "gateA", [128, NTI, 4], F32)
                with ExitStack() as p5r:
                    wr = sbt(p5r, "wr", [128, 16, NE], BF16)
                    brow = sbt(p5r, "brow", [1, NE], BF16)
                    s.dma("pool", lambda e: DMA(e, out=wr[:], in_=w_router.rearrange("(k p) n -> p k n", p=128)), writes=[wr.b])
                    s.dma("pool", lambda e: DMA(e, out=brow[:], in_=b_router.rearrange("(o n) -> o n", o=1)), writes=[brow.b])
                    zt = sbt(p5r, "zt", [128, D], BF16)
                    s.op("pool", lambda e: e.memset(zt[:], 0.0), writes=[zt.b])
                    XG = Buf("xg_d")
                    for r in range(NE * CAP // 128):
                        s.dma("sp", lambda e, r=r: DMA(e, out=xg_d[r * 128:(r + 1) * 128, :], in_=zt[:]), reads=[zt.b], writes=[XG])
                    ltri_i = sbt(p5r, "ltri_i", [128, 128], I32); ltri_f = sbt(p5r, "ltri_f", [128, 128], F32); ltri = sbt(p5r, "ltri", [128, 128], BF16)
                    ones_m = sbt(p5r, "ones_m", [128, 128], BF16)
                    eoff = sbt(p5r, "eoff", [128, NE], F32); eoff_i = sbt(p5r, "eoff_i", [128, NE], I32)
                    cnt = sbt(p5r, "cnt", [128, NE], F32)
                    s.op("pool", lambda e: e.iota(ltri_i[:], [[1, 128]], base=0, channel_multiplier=-1), writes=[ltri_i.b])
                    s.op("dve", lambda e: e.tensor_copy(ltri_f[:], ltri_i[:]), reads=[ltri_i.b], writes=[ltri_f.b])
                    s.op("dve", lambda e: e.tensor_scalar(out=ltri[:], in0=ltri_f[:], scalar1=0.0, scalar2=None, op0=ALU.is_gt), reads=[ltri_f.b], writes=[ltri.b])
                    s.op("dve", lambda e: e.memset(ones_m[:], 1.0), writes=[ones_m.b])
                    s.op("pool", lambda e: e.iota(eoff_i[:], [[CAP, NE]], base=0, channel_multiplier=0), writes=[eoff_i.b])
                    s.op("dve", lambda e: e.tensor_copy(eoff[:], eoff_i[:]), reads=[eoff_i.b], writes=[eoff.b])
                    s.op("dve", lambda e: e.memset(cnt[:], 0.0), writes=[cnt.b])
                    x1R = Ring([sbt(p5r, "x1r%d" % i, [128, D], F32) for i in range(2)])
                    tmpf = sbt(p5r, "tmpf", [128, D], F32)
                    h2R = Ring([sbt(p5r, "h2r%d" % i, [128, D], BF16) for i in range(2)])
                    h2Tt = Ring([sbt(p5r, "h2Tt%d" % i, [128, 16, 128], BF16) for i in range(2)])
                    lg = sbt(p5r, "lg", [128, NE], F32); mx8 = sbt(p5r, "mx8", [128, 8], F32); msk = sbt(p5r, "msk", [128, NE], F32); msk16 = sbt(p5r, "msk16", [128, NE], BF16)
                    ssm_ = sbt(p5r, "ssm_", [128, 4], F32); ex4 = sbt(p5r, "ex4", [128, 4], F32); dst = sbt(p5r, "dst", [128, NE], F32); oh = sbt(p5r, "oh", [128, NE], F32)
                    dk = sbt(p5r, "dk", [128, 4], F32)
                    ptr6 = Ring([pst(p5r, "ptr6%d" % i, [128, 1024], BF16) for i in range(2)])
                    pl6 = Ring([pst(p5r, "pl6%d" % i, [128, 512], F32) for i in range(3)])
                    for ti in range(NTI):
                        x1 = x1R.get()
                        s.dma("sp", lambda e, x1=x1, ti=ti: DMA(e, out=x1[:], in_=x1_d[ti * 128:(ti + 1) * 128, :]), writes=[x1.b])
                        h2b = h2R.get()
                        s.op("act", lambda e, x1=x1, h2b=h2b: e.activation(out=h2b[:], in_=x1[:], func=AF.Square, accum_out=ssm_[:, 0:1]), reads=[x1.b], writes=[h2b.b, ssm_.b])
                        s.op("dve", lambda e: e.tensor_scalar(out=ssm_[:, 0:1], in0=ssm_[:, 0:1], scalar1=1.0 / D, scalar2=EPS, op0=ALU.mult, op1=ALU.add), reads=[ssm_.b], writes=[ssm_.b])
                        s.op("act", lambda e: e.activation(out=ssm_[:, 0:1], in_=ssm_[:, 0:1], func=AF.Sqrt), reads=[ssm_.b], writes=[ssm_.b])
                        s.op("dve", lambda e: e.reciprocal(ssm_[:, 0:1], ssm_[:, 0:1]), reads=[ssm_.b], writes=[ssm_.b])
                        s.op("dve", lambda e, x1=x1: e.scalar_tensor_tensor(out=tmpf[:], in0=x1[:], scalar=ssm_[:, 0:1], in1=a2row[:], op0=ALU.mult, op1=ALU.mult),
                             reads=[x1.b, ssm_.b, a2row.b], writes=[tmpf.b])
                        s.op("dve", lambda e, h2b=h2b: e.tensor_tensor(out=h2b[:], in0=tmpf[:], in1=sh2row[:], op=ALU.add), reads=[tmpf.b, sh2row.b], writes=[h2b.b])
                        hT = h2Tt.get()
                        for half in range(2):
                            pt = ptr6.get()
                            for k8 in range(8):
                                k = half * 8 + k8
                                s.op("pe", lambda e, pt=pt, k8=k8, k=k, h2b=h2b: e.transpose(pt[:, k8 * 128:(k8 + 1) * 128], h2b[:, k * 128:(k + 1) * 128], ident[:]),
                                     reads=[h2b.b, ident.b], writes=[pt.b])
                            s.op("dve", lambda e, pt=pt, half=half, hT=hT: e.tensor_copy(hT[:, half * 8:(half + 1) * 8, :], pt[:].rearrange("p (a b) -> p a b", a=8)), reads=[pt.b], writes=[hT.b])
                        pl = pl6.get()
                        for k in range(16):
                            s.op("pe", lambda e, pl=pl, k=k, hT=hT: e.matmul(pl[:, 0:NE], hT[:, k, :], wr[:, k, :], start=(k == 0), stop=False), reads=[hT.b, wr.b], writes=[pl.b])
                        s.op("pe", lambda e, pl=pl: e.matmul(pl[:, 0:NE], ones_r[:], brow[:], start=False, stop=True), reads=[ones_r.b, brow.b], writes=[pl.b])
                        s.op("dve", lambda e, pl=pl: e.tensor_copy(lg[:], pl[:, 0:NE]), reads=[pl.b], writes=[lg.b])
                        s.op("dve", lambda e: e.max(mx8[:], lg[:]), reads=[lg.b], writes=[mx8.b])
                        s.op("dve", lambda e: e.tensor_scalar(out=msk[:], in0=lg[:], scalar1=mx8[:, 3:4], scalar2=None, op0=ALU.is_ge), reads=[lg.b, mx8.b], writes=[msk.b])
                        s.op("dve", lambda e: e.tensor_copy(msk16[:], msk[:]), reads=[msk.b], writes=[msk16.b])
                        s.op("dve", lambda e: e.tensor_scalar(out=ex4[:], in0=mx8[:, 0:4], scalar1=mx8[:, 0:1], scalar2=None, op0=ALU.subtract), reads=[mx8.b], writes=[ex4.b])
                        s.op("act", lambda e: e.activation(out=ex4[:], in_=ex4[:], func=AF.Exp, accum_out=ssm_[:, 1:2]), reads=[ex4.b], writes=[ex4.b, ssm_.b])
                        s.op("dve", lambda e: e.reciprocal(ssm_[:, 1:2], ssm_[:, 1:2]), reads=[ssm_.b], writes=[ssm_.b])
                        s.op("dve", lambda e, ti=ti: e.tensor_scalar(out=gateA[:, ti, :], in0=ex4[:], scalar1=ssm_[:, 1:2], scalar2=None, op0=ALU.mult), reads=[ex4.b, ssm_.b], writes=[gateA.b])
                        pp = pl6.get()
                        s.op("pe", lambda e, pp=pp: e.matmul(pp[:, 0:NE], ltri[:], msk16[:], start=True, stop=True), reads=[ltri.b, msk16.b], writes=[pp.b])
                        s.op("dve", lambda e, pp=pp: e.tensor_tensor(out=dst[:], in0=pp[:, 0:NE], in1=cnt[:], op=ALU.add), reads=[pp.b, cnt.b], writes=[dst.b])
                        s.op("dve", lambda e: e.tensor_scalar(out=dst[:], in0=dst[:], scalar1=float(CAP - 1), scalar2=None, op0=ALU.min), reads=[dst.b], writes=[dst.b])
                        s.op("dve", lambda e: e.tensor_tensor(out=dst[:], in0=dst[:], in1=eoff[:], op=ALU.add), reads=[dst.b, eoff.b], writes=[dst.b])
                        pc = pl6.get()
                        s.op("pe", lambda e, pc=pc: e.matmul(pc[:, 0:NE], ones_m[:], msk16[:], start=True, stop=True), reads=[ones_m.b, msk16.b], writes=[pc.b])
                        s.op("dve", lambda e, pc=pc: e.tensor_tensor(out=cnt[:], in0=cnt[:], in1=pc[:, 0:NE], op=ALU.add), reads=[pc.b, cnt.b], writes=[cnt.b])
                        for kk in range(4):
                            s.op("dve", lambda e, kk=kk: e.tensor_scalar(out=oh[:], in0=lg[:], scalar1=mx8[:, kk:kk + 1], scalar2=None, op0=ALU.is_equal), reads=[lg.b, mx8.b], writes=[oh.b])
                            s.op("dve", lambda e: e.tensor_tensor(out=oh[:], in0=oh[:], in1=dst[:], op=ALU.mult), reads=[oh.b, dst.b], writes=[oh.b])
                            s.op("dve", lambda e, kk=kk: e.tensor_reduce(out=dk[:, kk:kk + 1], in_=oh[:], axis=AX.X, op=ALU.add), reads=[oh.b], writes=[dk.b])
                        s.op("dve", lambda e, ti=ti: e.tensor_copy(destA[:, ti, :], dk[:]), reads=[dk.b], writes=[destA.b])
                        for kk in range(4):
                            s.dma("pool", lambda e, h2b=h2b, ti=ti, kk=kk: e.indirect_dma_start(out=xg_d, out_offset=bass.IndirectOffsetOnAxis(ap=destA[:, ti, kk:kk + 1], axis=0),
                                                                                             in_=h2b[:], in_offset=None),
                                  reads=[h2b.b, destA.b], writes=[XG])
                    s.barrier()
                YG = Buf("yg_d")
                with ExitStack() as p5e:
                    xa = sbt(p5e, "xa", [128, NTL * D], BF16)
                    xgT = sbt(p5e, "xgT", [128, 16, HC], BF16)
                    wguR = Ring([sbt(p5e, "wgu%d" % i, [128, 16, 256], BF16) for i in range(2)])
                    wdR = Ring([sbt(p5e, "wdT%d" % i, [128, 16, 1024], BF16) for i in range(2)])
                    bguR = Ring([sbt(p5e, "bgu%d" % i, [128, 16, 2], F32) for i in range(2)])
                    bdrR = Ring([sbt(p5e, "bdr%d" % i, [1, D], BF16) for i in range(2)])
                    NG = 2
                    GW = HC // NG
                    gt_ = Ring([sbt(p5e, "gt%d" % i, [128, GW], F32) for i in range(2)])
                    sg_ = Ring([sbt(p5e, "sgm%d" % i, [128, GW], F32) for i in range(2)])
                    ut_ = Ring([sbt(p5e, "ut%d" % i, [128, GW], F32) for i in range(2)])
                    ybR = Ring([sbt(p5e, "yb%d" % i, [128, D], BF16) for i in range(2)])
                    ptr7 = Ring([pst(p5e, "ptr7%d" % i, [128, 1024], BF16) for i in range(2)])
                    pgu = Ring([pst(p5e, "pgu%d" % i, [128, 512], F32) for i in range(4)])
                    pdn = Ring([pst(p5e, "pdn%d" % i, [128, 512], F32) for i in range(2)])
                    for ex, hf in [(ex_, hf_) for ex_ in range(NE) for hf_ in range(NHF)]:
                        rbase = ex * CAP + hf * HC
                        bgu = bguR.get(); bdr = bdrR.get()
                        s.dma("sp", lambda e, ex=ex, bgu=bgu: DMA(e, out=bgu[:], in_=b_gate_up[ex].rearrange("(ft p two) -> p ft two", p=128, two=2)), writes=[bgu.b])
                        s.dma("pool", lambda e, ex=ex, bdr=bdr: DMA(e, out=bdr[:], in_=b_down[ex].rearrange("(o n) -> o n", o=1)), writes=[bdr.b])
                        s.dma("sp", lambda e, rbase=rbase: DMA(e, out=xa[:].rearrange("p (n d) -> p n d", d=D), in_=xg_d[rbase:rbase + HC, :].rearrange("(n p) d -> p n d", p=128)), reads=[XG], writes=[xa.b])
                        for tl in range(NTL):
                            for half in range(2):
                                pt = ptr7.get()
                                for k8 in range(8):
                                    k = half * 8 + k8
                                    s.op("pe", lambda e, pt=pt, k8=k8, k=k, tl=tl: e.transpose(pt[:, k8 * 128:(k8 + 1) * 128], xa[:, tl * D + k * 128:tl * D + (k + 1) * 128], ident[:]),
                                         reads=[xa.b, ident.b], writes=[pt.b])
                                s.op("act" if half else "dve", (lambda e, pt=pt, half=half, tl=tl: e.activation(out=xgT[:, half * 8:(half + 1) * 8, tl * 128:(tl + 1) * 128], in_=pt[:].rearrange("p (a b) -> p a b", a=8), func=AF.Copy)) if half else
                                     (lambda e, pt=pt, half=half, tl=tl: e.tensor_copy(xgT[:, half * 8:(half + 1) * 8, tl * 128:(tl + 1) * 128], pt[:].rearrange("p (a b) -> p a b", a=8))),
                                     reads=[pt.b], writes=[xgT.b])
                        for ft in range(16):
                            w = wguR.get()
                            s.dma("pool", lambda e, ex=ex, ft=ft, w=w: DMA(e, out=w[:], in_=w_gate_up[ex, :, ft * 256:(ft + 1) * 256].rearrange("(k p) n -> p k n", p=128)), writes=[w.b])
                            for gi in range(NG):
                                c0 = gi * GW
                                pg = pgu.get(); pu = pgu.get()
                                for k in range(16):
                                    s.op("pe", lambda e, pg=pg, w=w, k=k, c0=c0: e.matmul(pg[:, 0:GW], w[:, k, 0::2], xgT[:, k, c0:c0 + GW], start=(k == 0), stop=(k == 15)), reads=[w.b, xgT.b], writes=[pg.b])
                                for k in range(16):
                                    s.op("pe", lambda e, pu=pu, w=w, k=k, c0=c0: e.matmul(pu[:, 0:GW], w[:, k, 1::2], xgT[:, k, c0:c0 + GW], start=(k == 0), stop=(k == 15)), reads=[w.b, xgT.b], writes=[pu.b])
                                g_ = gt_.get(); sgm = sg_.get(); u_ = ut_.get()
                                s.op("dve", lambda e, pg=pg, g_=g_, ft=ft, bgu=bgu: e.tensor_scalar(out=g_[:], in0=pg[:, 0:GW], scalar1=bgu[:, ft, 0:1], scalar2=7.0, op0=ALU.add, op1=ALU.min), reads=[pg.b, bgu.b], writes=[g_.b])
                                s.op("act", lambda e, g_=g_, sgm=sgm: e.activation(out=sgm[:], in_=g_[:], func=AF.Sigmoid, scale=1.702), reads=[g_.b], writes=[sgm.b])
                                s.op("dve", lambda e, pu=pu, u_=u_, ft=ft, bgu=bgu: e.tensor_scalar(out=u_[:], in0=pu[:, 0:GW], scalar1=bgu[:, ft, 1:2], scalar2=7.0, op0=ALU.add, op1=ALU.min), reads=[pu.b, bgu.b], writes=[u_.b])
                                s.op("pool", lambda e, u_=u_: e.tensor_scalar(out=u_[:], in0=u_[:], scalar1=-7.0, scalar2=1.0, op0=ALU.max, op1=ALU.add), reads=[u_.b], writes=[u_.b])
                                s.op("pool", lambda e, g_=g_, sgm=sgm: e.tensor_tensor(out=g_[:], in0=g_[:], in1=sgm[:], op=ALU.mult), reads=[g_.b, sgm.b], writes=[g_.b])
                                s.op("pool", lambda e, g_=g_, u_=u_, ft=ft, c0=c0: e.tensor_tensor(out=xa[:, ft * HC + c0:ft * HC + c0 + GW], in0=g_[:], in1=u_[:], op=ALU.mult), reads=[g_.b, u_.b], writes=[xa.b])
                        wds = []
                        for ch in range(2):
                            wd = wdR.get()
                            wds.append(wd)
                            s.dma("pool", lambda e, ex=ex, ch=ch, wd=wd: DMA(e, out=wd[:], in_=w_down[ex, :, ch * 1024:(ch + 1) * 1024].rearrange("(k p) n -> p k n", p=128)), writes=[wd.b])
                        for tl in range(NTL):
                            yb = ybR.get()
                            for cg in range(4):
                                wd = wds[cg // 2]
                                pd = pdn.get()
                                for ft in range(16):
                                    s.op("pe", lambda e, pd=pd, ft=ft, tl=tl, cg=cg, wd=wd: e.matmul(pd[:], xa[:, ft * HC + tl * 128:ft * HC + (tl + 1) * 128], wd[:, ft, (cg % 2) * 512:(cg % 2 + 1) * 512], start=(ft == 0), stop=False),
                                         reads=[xa.b, wd.b], writes=[pd.b])
                                s.op("pe", lambda e, pd=pd, cg=cg, bdr=bdr: e.matmul(pd[:], ones_r[:], bdr[:, cg * 512:(cg + 1) * 512], start=False, stop=True), reads=[ones_r.b, bdr.b], writes=[pd.b])
                                if cg % 2:
                                    s.op("act", lambda e, pd=pd, yb=yb, cg=cg: e.activation(out=yb[:, cg * 512:(cg + 1) * 512], in_=pd[:], func=AF.Copy), reads=[pd.b], writes=[yb.b])
                                else:
                                    s.op("dve", lambda e, pd=pd, yb=yb, cg=cg: e.tensor_copy(yb[:, cg * 512:(cg + 1) * 512], pd[:]), reads=[pd.b], writes=[yb.b])
                            r0 = rbase + tl * 128
                            s.dma("sp", lambda e, yb=yb, r0=r0: DMA(e, out=yg_d[r0:r0 + 128, :], in_=yb[:]), reads=[yb.b], writes=[YG])
                    s.barrier()
                with ExitStack() as p5f:
                    x1R2 = Ring([sbt(p5f, "x1f%d" % i, [128, D], F32) for i in range(2)])
                    ygR = Ring([sbt(p5f, "ygk%d" % i, [128, D], BF16) for i in range(4)])
                    accR = Ring([sbt(p5f, "accf%d" % i, [128, D], F32) for i in range(2)])
                    for ti in range(NTI):
                        x1 = x1R2.get()
                        s.dma("sp", lambda e, x1=x1, ti=ti: DMA(e, out=x1[:], in_=x1_d[ti * 128:(ti + 1) * 128, :]), writes=[x1.b])
                        acc = accR.get()
                        for kk in range(4):
                            yk = ygR.get()
                            s.dma("pool", lambda e, yk=yk, ti=ti, kk=kk: e.indirect_dma_start(out=yk[:], out_offset=None, in_=yg_d,
                                                                                           in_offset=bass.IndirectOffsetOnAxis(ap=destA[:, ti, kk:kk + 1], axis=0)),
                                  reads=[YG, destA.b], writes=[yk.b])
                            if kk == 0:
                                s.op("dve", lambda e, yk=yk, acc=acc, ti=ti: e.tensor_scalar(out=acc[:], in0=yk[:], scalar1=gateA[:, ti, 0:1], scalar2=None, op0=ALU.mult), reads=[yk.b, gateA.b], writes=[acc.b])
                            else:
                                s.op("dve", lambda e, yk=yk, acc=acc, ti=ti, kk=kk: e.scalar_tensor_tensor(out=acc[:], in0=yk[:], scalar=gateA[:, ti, kk:kk + 1], in1=acc[:], op0=ALU.mult, op1=ALU.add),
                                     reads=[yk.b, gateA.b, acc.b], writes=[acc.b])
                        s.op("pool", lambda e, acc=acc: e.tensor_tensor(out=acc[:], in0=acc[:], in1=g2row[:], op=ALU.mult), reads=[acc.b, g2row.b], writes=[acc.b])
                        s.op("pool", lambda e, acc=acc, x1=x1: e.tensor_tensor(out=acc[:], in0=acc[:], in1=x1[:], op=ALU.add), reads=[acc.b, x1.b], writes=[acc.b])
                        s.dma("sp", lambda e, acc=acc, ti=ti: DMA(e, out=out[ti * 128:(ti + 1) * 128, :], in_=acc[:]), reads=[acc.b])
                    s.barrier()
        s.barrier()
        s.emit()
    return nc


def make_inputs(inputs):
    x = np.asarray(inputs["x"], dtype=np.float32)
    pos = np.asarray(inputs["positions"])
    maps = []
    shared = {}
    for c in range(8):
        b, j = c // 4, c % 4
        npad = 12288 - 4096 * j
        xpad = np.zeros((NT, D), np.float32)
        xpad[npad:] = x[b, : 4096 * (j + 1)]
        pp = np.zeros((NT,), np.float32)
        pp[npad:] = pos[b, : 4096 * (j + 1)].astype(np.float32)
        valid = np.zeros((128, 16), np.float32)
        valid[:, npad // 1024:] = 1.0
        kbias = np.full((128, 128), NEG, np.float32)
        kbias[:, npad // 128:] = 0.0
        m = {"xp": xpad, "pos": pp.reshape(128, 128), "valid": valid, "kbias": kbias,
             "c": np.ascontiguousarray(inputs["c"][b]).astype(np.float32)}
        for k in ["w_ada", "b_ada", "norm_mix_g", "w_in", "q_lat_g", "kv_lat_g", "w_uq", "w_ukv", "q_nope_g", "q_rope_g", "k_nope_g", "k_rope_g",
                  "ssm_A_re", "ssm_A_im", "ssm_B_re", "ssm_B_im", "ssm_C_re", "ssm_C_im", "ssm_D", "ssm_log_dt", "out_ssm_g", "out_mla_g", "w_out",
                  "norm_ffn_g", "w_router", "b_router", "w_gate_up", "b_gate_up", "w_down", "b_down"]:
            if k not in shared:
                shared[k] = np.ascontiguousarray(np.asarray(inputs[k])[0], dtype=np.float32)
            m[k] = shared[k]
        maps.append(m)
    return maps


def kernel(**inputs):
    nc = build()
    maps = make_inputs(inputs)
    res = run_bass_kernel_spmd(nc, maps, core_ids=list(range(8)))
    outp = np.zeros((2, 16384, D), np.float32)
    for c in range(8):
        b, j = c // 4, c % 4
        outp[b, 4096 * j:4096 * (j + 1)] = res.results[c]["out"]
    return outp
```
